# Optimizing a Trainium2 kernel written in Bass

```python
import jax
import jax.numpy as jnp
from jax import lax
import numpy as np

D_MODEL = 1024
BATCH = 8
SEQ = 4096
DEPTH = 4

GRID_W = 64
CTX_LEN = 256
EPS = 1e-6
DEEPNORM_ALPHA = (2 * DEPTH) ** 0.25
DEEPNORM_BETA = (8 * DEPTH) ** -0.25

RET_HEADS = 8
RET_DK = 64
RET_DV = 128
RET_CHUNK = 128
RET_W = RET_HEADS * RET_DV

ATT_HEADS = 16
ATT_KV_HEADS = 4
ATT_DH = 64
ATT_W = ATT_HEADS * ATT_DH
Q_BLOCK = 128
ROPE_THETA = 10000.0

CONV_CH = 1024
CONV_K = 31

N_EXPERTS = 64
TOP_K = 8
N_GROUPS = 8
TOPK_GROUPS = 4
D_EXPERT = 256
D_SHARED = 256
ROUTED_SCALE = 2.5
MOE_BLOCK = 128

N_BRANCHES = 3
SPLIT_SIZES = (RET_HEADS * RET_DK, RET_HEADS * RET_DK, RET_W, RET_W,
               ATT_W, ATT_KV_HEADS * ATT_DH, ATT_KV_HEADS * ATT_DH,
               2 * CONV_CH, N_BRANCHES * D_MODEL)
D_IN = sum(SPLIT_SIZES)

kernel_name = 'hybrid_dit_retention_gqa_conformer_moe'


def layer_norm(x, g, b):
    xf = x.astype(jnp.float32)
    mu = jnp.mean(xf, axis=-1, keepdims=True)
    var = jnp.mean(jnp.square(xf - mu), axis=-1, keepdims=True)
    return ((xf - mu) * lax.rsqrt(var + EPS) * g + b).astype(x.dtype)


def rms_norm(x, g):
    xf = x.astype(jnp.float32)
    return (xf * lax.rsqrt(jnp.mean(xf * xf, axis=-1, keepdims=True) + EPS) * g).astype(x.dtype)


def split_heads(a, n_heads):
    return a.reshape(a.shape[0], a.shape[1], n_heads, -1)


def split_projection(z):
    idx = np.cumsum(SPLIT_SIZES)[:-1].tolist()
    return jnp.split(z, idx, axis=-1)


def axial_rope_tables(row, col, head_dim):
    n_freq = head_dim // 4
    inv_freq = ROPE_THETA ** (-jnp.arange(n_freq, dtype=jnp.float32) / n_freq)
    ang = jnp.concatenate([row[:, None] * inv_freq, col[:, None] * inv_freq], axis=-1)
    return jnp.cos(ang), jnp.sin(ang)


def apply_rope(x, cos, sin):
    half = x.shape[-1] // 2
    x1, x2 = x[..., :half], x[..., half:]
    cs, sn = cos[None, :, None, :], sin[None, :, None, :]
    return jnp.concatenate([x1 * cs - x2 * sn, x1 * sn + x2 * cs], axis=-1).astype(x.dtype)


def retention_scan(q, k, v, log_gamma, s0):
    b, t, h, dk = q.shape
    dv = v.shape[-1]
    nc = t // RET_CHUNK
    qc = q.astype(jnp.float32).reshape(b, nc, RET_CHUNK, h, dk)
    kc = k.astype(jnp.float32).reshape(b, nc, RET_CHUNK, h, dk)
    vc = v.astype(jnp.float32).reshape(b, nc, RET_CHUNK, h, dv)
    pos = jnp.arange(RET_CHUNK, dtype=jnp.float32)
    rel = pos[:, None] - pos[None, :]
    decay_in = jnp.where(rel >= 0, jnp.exp(log_gamma[:, None, None] * jnp.maximum(rel, 0.0)), 0.0)
    scores = jnp.einsum('bnihd,bnjhd->bnhij', qc, kc) * decay_in
    y_inner = jnp.einsum('bnhij,bnjhe->bnihe', scores, vc)
    k_w = jnp.exp(log_gamma[None, :] * (RET_CHUNK - 1 - pos)[:, None])
    kv = jnp.einsum('bnjhd,jh,bnjhe->bnhde', kc, k_w, vc)
    g_chunk = jnp.exp(log_gamma * RET_CHUNK)[None, :, None, None]

    def step(s, kv_c):
        return g_chunk * s + kv_c, s

    s_final, s_prev = lax.scan(step, s0, jnp.moveaxis(kv, 1, 0))
    s_prev = jnp.moveaxis(s_prev, 0, 1)
    q_w = jnp.exp(log_gamma[None, :] * (pos + 1.0)[:, None])
    y_cross = jnp.einsum('bnihd,ih,bnhde->bnihe', qc, q_w, s_prev)
    return (y_inner + y_cross).reshape(b, t, h, dv), s_final


def bidirectional_retention(q_l, k_l, v_l, q_c, k_c, v_c, log_gamma):
    b = q_l.shape[0]
    s0 = jnp.zeros((b, RET_HEADS, RET_DK, RET_DV), jnp.float32)
    rev = lambda a: jnp.flip(a, axis=1)
    yc_f, sc_f = retention_scan(q_c, k_c, v_c, log_gamma[0], s0)
    yl_f, _ = retention_scan(q_l, k_l, v_l, log_gamma[0], sc_f)
    yc_b, sc_b = retention_scan(rev(q_c), rev(k_c), rev(v_c), log_gamma[1], s0)
    yl_b, _ = retention_scan(rev(q_l), rev(k_l), rev(v_l), log_gamma[1], sc_b)
    return yl_f + rev(yl_b), yc_f + rev(yc_b)


def retention_output(y, g, w_o):
    mu = jnp.mean(y, axis=-1, keepdims=True)
    var = jnp.mean(jnp.square(y - mu), axis=-1, keepdims=True)
    yn = ((y - mu) * lax.rsqrt(var + EPS)).reshape(y.shape[0], y.shape[1], RET_W).astype(g.dtype)
    return (jax.nn.silu(g) * yn) @ w_o


def block_attention(q, k, v):
    b, t, h, d = q.shape
    kvh = k.shape[2]
    grp = h // kvh
    nq = t // Q_BLOCK
    qb = jnp.moveaxis(q.reshape(b, nq, Q_BLOCK, kvh, grp, d), 1, 0)
    scale = d ** -0.5

    def one_block(qblk):
        s = jnp.einsum('bqkgd,bskd->bkgqs', qblk, k, preferred_element_type=jnp.float32) * scale
        p = jax.nn.softmax(s, axis=-1).astype(v.dtype)
        return jnp.einsum('bkgqs,bskd->bqkgd', p, v)

    o = lax.map(one_block, qb)
    return jnp.moveaxis(o, 0, 1).reshape(b, t, h * d)


def conformer_conv(u, w_dw, b_dw, ln_g, ln_b):
    a, gte = jnp.split(u, 2, axis=-1)
    glu = a * jax.nn.sigmoid(gte)
    y = lax.conv_general_dilated(glu, w_dw[:, None, :].astype(glu.dtype), window_strides=(1,),
                                 padding=[(CONV_K // 2, CONV_K // 2)],
                                 dimension_numbers=('NWC', 'WIO', 'NWC'),
                                 feature_group_count=CONV_CH) + b_dw
    return jax.nn.silu(layer_norm(y, ln_g, ln_b))


def merge_branches(gates, br_ret, br_att, br_conv):
    g_ret, g_att, g_conv = jnp.split(gates, N_BRANCHES, axis=-1)
    return jax.nn.sigmoid(g_ret) * br_ret + jax.nn.sigmoid(g_att) * br_att + jax.nn.sigmoid(g_conv) * br_conv


def token_mixers(h_l, h_c, cos, sin, need_ctx, w_in, decay_logit, q_norm, k_norm,
                 conv_dw, conv_db, conv_ln_g, conv_ln_b, w_ret_o, w_att_o, w_conv_o, w_out):
    rq_l, rk_l, rv_l, rg_l, aq_l, ak_l, av_l, cu_l, gt_l = split_projection(h_l @ w_in)
    rq_c, rk_c, rv_c, rg_c, aq_c, ak_c, av_c, cu_c, gt_c = split_projection(h_c @ w_in)

    rscale = RET_DK ** -0.5
    rq_l = apply_rope(split_heads(rq_l, RET_HEADS), cos, sin)
    rk_l = apply_rope(split_heads(rk_l, RET_HEADS), cos, sin) * rscale
    rq_c = split_heads(rq_c, RET_HEADS)
    rk_c = split_heads(rk_c, RET_HEADS) * rscale
    log_gamma = jax.nn.log_sigmoid(decay_logit.astype(jnp.float32))
    ret_l, ret_c = bidirectional_retention(rq_l, rk_l, split_heads(rv_l, RET_HEADS),
                                           rq_c, rk_c, split_heads(rv_c, RET_HEADS), log_gamma)
    br_ret_l = retention_output(ret_l, rg_l, w_ret_o)

    aq_l = apply_rope(rms_norm(split_heads(aq_l, ATT_HEADS), q_norm), cos, sin)
    ak_l = apply_rope(rms_norm(split_heads(ak_l, ATT_KV_HEADS), k_norm), cos, sin)
    av_l = split_heads(av_l, ATT_KV_HEADS)
    ak_c = rms_norm(split_heads(ak_c, ATT_KV_HEADS), k_norm)
    av_c = split_heads(av_c, ATT_KV_HEADS)
    br_att_l = block_attention(aq_l, jnp.concatenate([ak_c, ak_l], axis=1),
                               jnp.concatenate([av_c, av_l], axis=1)) @ w_att_o

    br_conv_l = conformer_conv(cu_l, conv_dw, conv_db, conv_ln_g, conv_ln_b) @ w_conv_o

    out_l = merge_branches(gt_l, br_ret_l, br_att_l, br_conv_l) @ w_out
    if not need_ctx:
        return out_l, None

    br_ret_c = retention_output(ret_c, rg_c, w_ret_o)
    aq_c = rms_norm(split_heads(aq_c, ATT_HEADS), q_norm)
    br_att_c = block_attention(aq_c, ak_c, av_c) @ w_att_o
    br_conv_c = conformer_conv(cu_c, conv_dw, conv_db, conv_ln_g, conv_ln_b) @ w_conv_o
    out_c = merge_branches(gt_c, br_ret_c, br_att_c, br_conv_c) @ w_out
    return out_l, out_c


def moe_ffn(h, w_router, router_bias, w_exp_gate, w_exp_up, w_exp_down, w_sh_gate, w_sh_up, w_sh_down):
    n, d = h.shape
    scores = jax.nn.sigmoid(jnp.dot(h, w_router, preferred_element_type=jnp.float32))
    sel = scores + router_bias.astype(jnp.float32)
    per_group = N_EXPERTS // N_GROUPS
    grp_score = lax.top_k(sel.reshape(n, N_GROUPS, per_group), 2)[0].sum(-1)
    _, top_grp = lax.top_k(grp_score, TOPK_GROUPS)
    grp_keep = jnp.any(top_grp[:, :, None] == jnp.arange(N_GROUPS)[None, None, :], axis=1)
    sel = jnp.where(jnp.repeat(grp_keep, per_group, axis=1), sel, -jnp.inf)
    _, top_e = lax.top_k(sel, TOP_K)
    gate_w = jnp.take_along_axis(scores, top_e, axis=1)
    gate_w = ROUTED_SCALE * gate_w / jnp.sum(gate_w, axis=-1, keepdims=True)

    n_assign = n * TOP_K
    n_blocks = (n_assign + N_EXPERTS * (MOE_BLOCK - 1) + MOE_BLOCK - 1) // MOE_BLOCK
    flat_e = top_e.reshape(-1).astype(jnp.int32)
    flat_tok = jnp.repeat(jnp.arange(n, dtype=jnp.int32), TOP_K)
    flat_w = gate_w.reshape(-1)
    order = jnp.argsort(flat_e, stable=True)
    se = flat_e[order]
    counts = jnp.bincount(flat_e, length=N_EXPERTS).astype(jnp.int32)
    padded = (counts + MOE_BLOCK - 1) // MOE_BLOCK * MOE_BLOCK
    start_unp = jnp.cumsum(counts) - counts
    end_pad = jnp.cumsum(padded)
    start_pad = end_pad - padded
    dest = start_pad[se] + (jnp.arange(n_assign, dtype=jnp.int32) - start_unp[se])
    buf_tok = jnp.full((n_blocks * MOE_BLOCK,), n, jnp.int32).at[dest].set(flat_tok[order])
    buf_w = jnp.zeros((n_blocks * MOE_BLOCK,), jnp.float32).at[dest].set(flat_w[order])
    block_start = jnp.arange(n_blocks, dtype=jnp.int32) * MOE_BLOCK
    block_e = jnp.minimum(jnp.searchsorted(end_pad, block_start, side='right'), N_EXPERTS - 1)
    h_pad = jnp.concatenate([h, jnp.zeros((1, d), h.dtype)], axis=0)

    def expert_block(acc, blk):
        tok, wt, e = blk
        xb = h_pad[tok]
        hid = jax.nn.silu(xb @ w_exp_gate[e]) * (xb @ w_exp_up[e])
        yb = (hid @ w_exp_down[e]) * wt[:, None].astype(h.dtype)
        return acc.at[tok].add(yb), None

    routed, _ = lax.scan(expert_block, jnp.zeros((n + 1, d), h.dtype),
                         (buf_tok.reshape(n_blocks, MOE_BLOCK), buf_w.reshape(n_blocks, MOE_BLOCK), block_e))
    shared = (jax.nn.silu(h @ w_sh_gate) * (h @ w_sh_up)) @ w_sh_down
    return routed[:n] + shared


def setup_inputs(seed: int = 0) -> dict:
    key = jax.random.key(seed)
    ks = jax.random.split(key, 32)
    f32 = jnp.float32
    nrm = lambda k, shape, s: jax.random.normal(k, shape, f32) * s
    L = DEPTH
    base_logit = jnp.log(2.0 ** (5.0 + jnp.arange(RET_HEADS, dtype=f32)) - 1.0)
    beta = DEEPNORM_BETA
    return {
        'x': nrm(ks[0], (BATCH, SEQ, D_MODEL), 1.0),
        'c': nrm(ks[1], (BATCH, D_MODEL), 1.0),
        'ctx': nrm(ks[2], (BATCH, CTX_LEN, D_MODEL), 1.0),
        'c_ctx': nrm(ks[3], (D_MODEL,), 1.0),
        'w_ada': nrm(ks[4], (L, D_MODEL, 6 * D_MODEL), 0.5 * D_MODEL ** -0.5),
        'b_ada': nrm(ks[5], (L, 6 * D_MODEL), 0.02),
        'w_in': nrm(ks[6], (L, D_MODEL, D_IN), D_MODEL ** -0.5),
        'ret_decay_logit': base_logit[None, None, :] + nrm(ks[7], (L, 2, RET_HEADS), 0.1),
        'att_q_norm': 1.0 + nrm(ks[8], (L, ATT_DH), 0.02),
        'att_k_norm': 1.0 + nrm(ks[9], (L, ATT_DH), 0.02),
        'conv_dw': nrm(ks[10], (L, CONV_K, CONV_CH), CONV_K ** -0.5),
        'conv_db': nrm(ks[11], (L, CONV_CH), 0.02),
        'conv_ln_g': 1.0 + nrm(ks[12], (L, CONV_CH), 0.02),
        'conv_ln_b': nrm(ks[13], (L, CONV_CH), 0.02),
        'w_ret_o': nrm(ks[14], (L, RET_W, D_MODEL), beta * RET_W ** -0.5),
        'w_att_o': nrm(ks[15], (L, ATT_W, D_MODEL), beta * ATT_W ** -0.5),
        'w_conv_o': nrm(ks[16], (L, CONV_CH, D_MODEL), beta * CONV_CH ** -0.5),
        'w_out': nrm(ks[17], (L, D_MODEL, D_MODEL), beta * D_MODEL ** -0.5),
        'ln1_g': 1.0 + nrm(ks[18], (L, D_MODEL), 0.02),
        'ln1_b': nrm(ks[19], (L, D_MODEL), 0.02),
        'w_router': nrm(ks[20], (L, D_MODEL, N_EXPERTS), D_MODEL ** -0.5),
        'router_bias': nrm(ks[21], (L, N_EXPERTS), 0.01),
        'w_exp_gate': nrm(ks[22], (L, N_EXPERTS, D_MODEL, D_EXPERT), D_MODEL ** -0.5),
        'w_exp_up': nrm(ks[23], (L, N_EXPERTS, D_MODEL, D_EXPERT), D_MODEL ** -0.5),
        'w_exp_down': nrm(ks[24], (L, N_EXPERTS, D_EXPERT, D_MODEL), beta * D_EXPERT ** -0.5),
        'w_sh_gate': nrm(ks[25], (L, D_MODEL, D_SHARED), D_MODEL ** -0.5),
        'w_sh_up': nrm(ks[26], (L, D_MODEL, D_SHARED), D_MODEL ** -0.5),
        'w_sh_down': nrm(ks[27], (L, D_SHARED, D_MODEL), beta * D_SHARED ** -0.5),
        'ln2_g': 1.0 + nrm(ks[28], (L, D_MODEL), 0.02),
        'ln2_b': nrm(ks[29], (L, D_MODEL), 0.02),
    }


def reference(x, c, ctx, c_ctx, w_ada, b_ada, w_in, ret_decay_logit, att_q_norm, att_k_norm,
              conv_dw, conv_db, conv_ln_g, conv_ln_b, w_ret_o, w_att_o, w_conv_o, w_out, ln1_g, ln1_b,
              w_router, router_bias, w_exp_gate, w_exp_up, w_exp_down, w_sh_gate, w_sh_up, w_sh_down,
              ln2_g, ln2_b):
    b, n_lat, _ = x.shape
    n_ctx = ctx.shape[1]
    ROWS = n_lat // GRID_W
    row = jnp.repeat(jnp.arange(ROWS, dtype=jnp.float32), GRID_W)
    col = jnp.tile(jnp.arange(GRID_W, dtype=jnp.float32), ROWS)
    cos, sin = axial_rope_tables(row, col, ATT_DH)
    s_lat = jax.nn.silu(c)
    s_ctx = jax.nn.silu(c_ctx)[None]
    x_l, x_c = x, ctx
    for l in range(DEPTH):
        need_ctx = l < DEPTH - 1
        mod_l = jnp.split((s_lat @ w_ada[l] + b_ada[l])[:, None, :], 6, axis=-1)
        mod_c = jnp.split((s_ctx @ w_ada[l] + b_ada[l])[:, None, :], 6, axis=-1)
        h_l = x_l * (1.0 + mod_l[1]) + mod_l[0]
        h_c = x_c * (1.0 + mod_c[1]) + mod_c[0]
        y_l, y_c = token_mixers(h_l, h_c, cos, sin, need_ctx, w_in[l], ret_decay_logit[l],
                                att_q_norm[l], att_k_norm[l], conv_dw[l], conv_db[l], conv_ln_g[l],
                                conv_ln_b[l], w_ret_o[l], w_att_o[l], w_conv_o[l], w_out[l])
        x_l = layer_norm(DEEPNORM_ALPHA * x_l + mod_l[2] * y_l, ln1_g[l], ln1_b[l])
        h2_l = x_l * (1.0 + mod_l[4]) + mod_l[3]
        if need_ctx:
            x_c = layer_norm(DEEPNORM_ALPHA * x_c + mod_c[2] * y_c, ln1_g[l], ln1_b[l])
            h2_c = x_c * (1.0 + mod_c[4]) + mod_c[3]
            tokens = jnp.concatenate([h2_l.reshape(-1, D_MODEL), h2_c.reshape(-1, D_MODEL)], axis=0)
            f = moe_ffn(tokens, w_router[l], router_bias[l], w_exp_gate[l], w_exp_up[l], w_exp_down[l],
                        w_sh_gate[l], w_sh_up[l], w_sh_down[l])
            f_l = f[: b * n_lat].reshape(b, n_lat, D_MODEL)
            f_c = f[b * n_lat:].reshape(b, n_ctx, D_MODEL)
            x_c = layer_norm(DEEPNORM_ALPHA * x_c + mod_c[5] * f_c, ln2_g[l], ln2_b[l])
        else:
            f_l = moe_ffn(h2_l.reshape(-1, D_MODEL), w_router[l], router_bias[l], w_exp_gate[l], w_exp_up[l],
                          w_exp_down[l], w_sh_gate[l], w_sh_up[l], w_sh_down[l]).reshape(b, n_lat, D_MODEL)
        x_l = layer_norm(DEEPNORM_ALPHA * x_l + mod_l[5] * f_l, ln2_g[l], ln2_b[l])
    return x_l
```

```python
import numpy as np
from contextlib import ExitStack
import concourse.bass as bass
import concourse.mybir as mybir
from concourse.bass_utils import run_bass_kernel_spmd

F32 = mybir.dt.float32
BF16 = mybir.dt.bfloat16
AF = mybir.ActivationFunctionType
ALU = mybir.AluOpType
AX = mybir.AxisListType

D = 1024
NT = 34
T = NT * 128
DIN = 9728
ZC = 7680
EPS = 1e-6
ALPHA = 8.0 ** 0.25
NE = 64
ENGS = ("pe", "act", "dve", "pool", "sp")


class Buf:
    __slots__ = ("name", "w", "r", "sem", "semcnt")

    def __init__(self, name=""):
        self.name = name
        self.w = {}
        self.r = {}
        self.sem = None
        self.semcnt = 0


class Op:
    __slots__ = ("eng", "fn", "deps", "idx", "sig", "dma", "sigval")

    def __init__(self, eng, fn, deps, idx, dma=None):
        self.eng = eng
        self.fn = fn
        self.deps = deps
        self.idx = idx
        self.sig = False
        self.dma = dma
        self.sigval = 0


class Sched:
    def __init__(self, nc, es):
        self.nc = nc
        self.es = es
        self.ops = {e: [] for e in ENGS}
        self.esem = {e: es.enter_context(nc.semaphore("es_" + e)) for e in ENGS}
        self.dsems = []
        self.dcnt = []
        self.free = []
        self.allbufs = []

    def buf(self, name=""):
        b = Buf(name)
        self.allbufs.append(b)
        return b

    def _collect(self, eng, reads, writes):
        deps = {}
        for b in reads:
            for k, v in b.w.items():
                if deps.get(k, -1) < v:
                    deps[k] = v
        for b in writes:
            for k, v in b.w.items():
                if deps.get(k, -1) < v:
                    deps[k] = v
            for k, v in b.r.items():
                if deps.get(k, -1) < v:
                    deps[k] = v
        out = []
        for k, v in deps.items():
            if k[0] == "e":
                if k[1] == eng and eng in ("pe", "sp"):
                    continue
                out.append(("e", k[1], v))
            else:
                out.append(("d", k[1], v))
        return out

    def _commit(self, key, val, reads, writes):
        for b in reads:
            if b.r.get(key, -1) < val:
                b.r[key] = val
        for b in writes:
            b.w = {key: val}
            b.r = {}

    def op(self, eng, fn, reads=(), writes=()):
        deps = self._collect(eng, reads, writes)
        idx = len(self.ops[eng])
        o = Op(eng, fn, deps, idx)
        self.ops[eng].append(o)
        self._commit(("e", eng), idx, reads, writes)
        return o

    def _getsem(self, buf):
        if buf.sem is None:
            if self.free:
                buf.sem = self.free.pop()
            else:
                buf.sem = len(self.dsems)
                self.dsems.append(self.es.enter_context(self.nc.semaphore("ds%d" % buf.sem)))
                self.dcnt.append(0)
        return buf.sem

    def dma(self, eng, out_ap, in_ap, reads, writes, sembuf):
        deps = self._collect(eng, reads, writes)
        si = self._getsem(sembuf)
        self.dcnt[si] += 16
        val = self.dcnt[si]
        idx = len(self.ops[eng])
        o = Op(eng, (out_ap, in_ap), deps, idx, dma=si)
        o.sigval = val
        self.ops[eng].append(o)
        self._commit(("d", si), val, reads, writes)
        return o

    def barrier(self):
        bb = Buf("bar")
        deps = {}
        for b in self.allbufs:
            for dct in (b.w, b.r):
                for k, v in dct.items():
                    if deps.get(k, -1) < v:
                        deps[k] = v
        dl = []
        for k, v in deps.items():
            if k[0] == "e":
                if k[1] != "sp":
                    dl.append(("e", k[1], v))
            else:
                dl.append(("d", k[1], v))
        idx = len(self.ops["sp"])
        o = Op("sp", lambda e: e.nop(), dl, idx)
        self.ops["sp"].append(o)
        bb.w = {("e", "sp"): idx}
        for e in ENGS:
            if e != "sp":
                self.op(e, None, [bb], [])
        for b in self.allbufs:
            b.w = {}
            b.r = {}
            b.sem = None
        self.free = list(range(len(self.dsems)))

    def emit(self):
        for e in ENGS:
            for o in self.ops[e]:
                for d in o.deps:
                    if d[0] == "e":
                        self.ops[d[1]][d[2]].sig = True
        for e in ENGS:
            c = 0
            for o in self.ops[e]:
                if o.dma is None and o.sig:
                    c += 1
                    o.sigval = c
        with self.nc.Block() as block:
            deco = {"pe": block.tensor, "act": block.scalar, "dve": block.vector,
                    "pool": block.gpsimd, "sp": block.sync}
            for e in ENGS:
                ops = self.ops[e]
                if not ops:
                    continue

                def body(engine, ops=ops, e=e):
                    waited = {}
                    for o in ops:
                        for d in o.deps:
                            if d[0] == "e":
                                key = ("e", d[1])
                                val = self.ops[d[1]][d[2]].sigval
                                sem = self.esem[d[1]]
                            else:
                                key = ("d", d[1])
                                val = d[2]
                                sem = self.dsems[d[1]]
                            if waited.get(key, 0) < val:
                                engine.wait_ge(sem, val)
                                waited[key] = val
                        if o.fn is None:
                            continue
                        if o.dma is not None:
                            if callable(o.fn):
                                o.fn(engine).then_inc(self.dsems[o.dma], 1)
                            else:
                                engine.dma_start(out=o.fn[0], in_=o.fn[1]).then_inc(self.dsems[o.dma], 16)
                        else:
                            ins = o.fn(engine)
                            if o.sig:
                                ins.then_inc(self.esem[e], 1)
                deco[e](body)


def fv(ap, pairs):
    return bass.AP(ap.tensor, ap.offset, [list(ap.ap[0])] + [list(p) for p in pairs])


def pbc(ap, nparts):
    return bass.AP(ap.tensor, ap.offset, [[0, nparts]] + [list(p) for p in ap.ap[1:]])


def build(L=4, dbg=None, NLW=4):
    nc = bass.Bass("TRN2", target_bir_lowering=False)
    dt_in = lambda name, shape: nc.dram_tensor(name, shape, F32, kind="ExternalInput").ap()
    xb_in = dt_in("xb", [4096, D])
    ctxb_in = dt_in("ctxb", [256, D])
    cvec_in = dt_in("cvec", [128, 16])
    w_ada = dt_in("w_ada", [NLW, D, 6 * D])
    b_ada = dt_in("b_ada", [NLW, 6 * D])
    w_in = dt_in("w_in", [NLW, D, DIN])
    dlog = dt_in("ret_decay_logit", [NLW, 16])
    qng = dt_in("att_q_norm", [NLW, 64])
    kng = dt_in("att_k_norm", [NLW, 64])
    conv_dw = dt_in("conv_dw", [NLW, 31, D])
    cvp = dt_in("conv_vecs", [NLW, 24, 128])
    w_ret_o = dt_in("w_ret_o", [NLW, D, D])
    w_att_o = dt_in("w_att_o", [NLW, D, D])
    w_conv_o = dt_in("w_conv_o", [NLW, D, D])
    w_out = dt_in("w_out", [NLW, D, D])
    ln1_g = dt_in("ln1_g", [NLW, D])
    ln1_b = dt_in("ln1_b", [NLW, D])
    w_router = dt_in("w_router", [NLW, D, NE])
    rbias = dt_in("router_bias", [NLW, NE])
    w_eg = dt_in("w_exp_gate", [NLW, NE, D, 256])
    w_eu = dt_in("w_exp_up", [NLW, NE, D, 256])
    w_ed = dt_in("w_exp_down", [NLW, NE, 256, D])
    w_sg = dt_in("w_sh_gate", [NLW, D, 256])
    w_su = dt_in("w_sh_up", [NLW, D, 256])
    w_sd = dt_in("w_sh_down", [NLW, 256, D])
    ln2_g = dt_in("ln2_g", [NLW, D])
    ln2_b = dt_in("ln2_b", [NLW, D])
    ident_in = dt_in("c_ident", [128, 128])
    rope_in = dt_in("c_rope", [T, 64])
    relt_in = dt_in("c_relt", [128, 384])
    posc_in = dt_in("c_posc", [128, 258])
    out = nc.dram_tensor("out", [4096, D], F32, kind="ExternalOutput").ap()

    def scratch(name, shape, dt):
        if dbg and name in dbg:
            return nc.dram_tensor(name, shape, dt, kind="ExternalOutput").ap()
        return nc.dram_tensor(name, shape, dt, kind="Internal").ap()
    modv = scratch("modv", [4, 2, 6 * D], F32)
    z_tok = scratch("z_tok", [T, ZC], BF16)
    gluT = scratch("gluT", [8, 128, T], BF16)
    retT = scratch("retT", [128, 8, T], BF16)
    attT = scratch("attT", [128, 8, T], BF16)
    convT = scratch("convT", [128, 8, T], BF16)
    mrgT = scratch("mrgT", [128, 8, T], BF16)
    xres = scratch("xres", [T, D], F32)
    xcur = scratch("xcur", [T, D], F32)

    with ExitStack() as es:
        S = Sched(nc, es)
        ARENA_W = 50000
        arena = es.enter_context(nc.sbuf_tensor("arena", [128, ARENA_W], F32))
        psum = [es.enter_context(nc.psum_tensor("ps%d" % i, [128, 512], F32)) for i in range(8)]
        bps = [S.buf("ps%d" % i) for i in range(8)]
        st = {"off": 0}

        def carve(shape, dt, nparts=128):
            n = int(np.prod(shape))
            words = (n * (4 if dt == F32 else 2) + 3) // 4
            words = (words + 7) // 8 * 8
            o = st["off"]
            assert o + words <= ARENA_W, ("arena overflow", o, words)
            st["off"] = o + words
            a = arena[0:nparts, o:o + words]
            if dt != F32:
                a = a.bitcast(dt)
            a = a[:, 0:n]
            if len(shape) == 2:
                a = a.rearrange("p (a b) -> p a b", a=shape[0])
            elif len(shape) == 3:
                a = a.rearrange("p (a b c) -> p a b c", a=shape[0], b=shape[1])
            return a

        def psb(i, n=1024):
            return psum[i].bitcast(BF16)[:, 0:n]

        def mm(o, lhsT, rhs, start, stop, reads, writes):
            S.op("pe", lambda e: e.matmul(o, lhsT, rhs, start=start, stop=stop), reads, writes)

        def tr(o, i, idn, reads, writes):
            S.op("pe", lambda e: e.transpose(o, i, idn), reads, writes)

        def act(o, i, func, reads, writes, bias=None, scale=None):
            kw = {}
            if bias is not None:
                kw["bias"] = bias
            if scale is not None:
                kw["scale"] = scale
            S.op("act", lambda e: e.activation(o, i, func, **kw), reads, writes)

        def tt(eng, o, a, b, op, reads, writes):
            S.op(eng, lambda e: e.tensor_tensor(o, a, b, op), reads, writes)

        def ts(eng, o, a, s1, s2, op0, op1, reads, writes):
            if s2 is None:
                S.op(eng, lambda e: e.tensor_scalar(o, a, s1, None, op0), reads, writes)
            else:
                S.op(eng, lambda e: e.tensor_scalar(o, a, s1, s2, op0, op1), reads, writes)

        def stt(eng, o, a, s, b, op0, op1, reads, writes):
            S.op(eng, lambda e: e.scalar_tensor_tensor(o, a, s, b, op0, op1), reads, writes)

        def cp(eng, o, i, reads, writes):
            if eng == "act":
                S.op("act", lambda e: e.copy(o, i), reads, writes)
            else:
                S.op(eng, lambda e: e.tensor_copy(o, i), reads, writes)

        def rsum(eng, o, i, reads, writes):
            S.op(eng, lambda e: e.reduce_sum(o, i, AX.X), reads, writes)

        def mset(eng, o, v, writes):
            S.op(eng, lambda e: e.memset(o, v), [], writes)

        def rstd_from(eng, o, i, scale, reads, writes):
            ts(eng, o, i, scale, EPS, ALU.mult, ALU.add, reads, writes)
            act(o, o, AF.Sqrt, writes, writes)
            S.op("dve", lambda e: e.reciprocal(o, o), writes, writes)

        def dump(name, ap, rbufs):
            if not (dbg and dbg.get("dump")):
                return
            shp = list(ap.shape)
            dtn = nc.dram_tensor("dmp_" + name, shp, ap.dtype, kind="ExternalOutput").ap()
            bb = S.buf()
            S.dma("sp", dtn, ap, list(rbufs), [bb], bb)

        identf = carve([128], F32)
        identb = carve([128], BF16)
        b_id = S.buf("ident")
        S.dma("sp", identf, ident_in, [], [b_id], b_id)
        cp("dve", identb, identf, [b_id], [b_id])
        onesf = carve([128], F32)
        mset("dve", onesf, 1.0, [b_id])
        onesm = carve([128], F32)
        mset("dve", onesm, 1.0 / 1024.0, [b_id])
        PERSIST = st["off"]

        def dram_bufs(n):
            return [S.buf() for _ in range(n)]
        b_modv = S.buf("modv")
        bz = {}

        def zb(i, name):
            k = (i, name)
            if k not in bz:
                bz[k] = S.buf()
            return bz[k]
        b_xcur = dram_bufs(NT)
        b_xres = dram_bufs(NT)
        b_out = S.buf("out")

        st["off"] = PERSIST
        cT = carve([16], F32)
        sT = carve([8, 2], F32)
        b_c = S.buf()
        S.dma("sp", cT, cvec_in, [], [b_c], b_c)
        act(sT.rearrange("p a b -> p (a b)"), cT, AF.Silu, [b_c], [b_c])
        wa = [carve([8, 512], F32) for _ in range(2)]
        b_wa = [S.buf() for _ in range(2)]
        bt = carve([6 * D], F32, nparts=2)
        modrow = carve([6 * D], F32, nparts=2)
        b_bt = S.buf()
        b_mr = S.buf()
        it = 0
        for l in range(L):
            S.dma("sp", bt, pbc(b_ada[l:l + 1, :], 2), [], [b_bt], b_bt)
            for n in range(12):
                s = it % 2
                it += 1
                S.dma("sp", wa[s], w_ada[l, :, n * 512:(n + 1) * 512].rearrange("(kc p) n -> p kc n", p=128),
                      [], [b_wa[s]], b_wa[s])
                for kc in range(8):
                    mm(psum[s][0:2, :], sT[:, kc, :], wa[s][:, kc, :], kc == 0, kc == 7, [b_c, b_wa[s]], [bps[s]])
                tt("dve", modrow[:, n * 512:(n + 1) * 512], psum[s][0:2, :], bt[:, n * 512:(n + 1) * 512], ALU.add,
                   [bps[s], b_bt], [b_mr])
            S.dma("sp", modv[l], modrow, [b_mr], [b_modv], b_mr)
        S.barrier()

        def mod_bc(dst, l, which, part, b_dst, plus1=False):
            src = modv[l, which:which + 1, part * D:(part + 1) * D]
            S.dma("sp", dst, pbc(src, 128), [b_modv], [b_dst], b_dst)
            if plus1:
                ts("pool", dst, dst, 1.0, None, ALU.add, None, [b_dst], [b_dst])

        def vec_bc(dst, src_row, n, b_dst):
            S.dma("sp", dst, pbc(src_row, 128), [], [b_dst], b_dst)

        def rope(eng, dst1, dst2, src, H, rp, tmp, reads, writes):
            x1 = src[:, :, 0:32]
            x2 = src[:, :, 32:64]
            cs = fv(rp[:, 0:32], [[0, H], [1, 32]])
            sn = fv(rp[:, 32:64], [[0, H], [1, 32]])
            ta, tb_ = tmp
            tt(eng, ta, x1, cs, ALU.mult, reads, writes)
            tt(eng, tb_, x2, sn, ALU.mult, reads, writes)
            tt(eng, dst1, ta, tb_, ALU.subtract, reads, writes)
            tt(eng, ta, x1, sn, ALU.mult, reads, writes)
            tt(eng, tb_, x2, cs, ALU.mult, reads, writes)
            tt(eng, dst2, ta, tb_, ALU.add, reads, writes)

        for l in range(L):
            last = (l == L - 1)

            def xin(i):
                if l == 0:
                    return ctxb_in[i * 128:(i + 1) * 128, :] if i < 2 else xb_in[(i - 2) * 128:(i - 1) * 128, :]
                return xcur[i * 128:(i + 1) * 128, :]

            st["off"] = PERSIST
            hT = carve([8, T], BF16)
            b_hT = [S.buf() for _ in range(NT)]
            sc1 = [carve([D], F32) for _ in range(2)]
            sh1 = [carve([D], F32) for _ in range(2)]
            b_m1 = S.buf()
            for w_ in range(2):
                mod_bc(sc1[w_], l, w_, 1, b_m1, plus1=True)
                mod_bc(sh1[w_], l, w_, 0, b_m1)
            xt = [carve([D], F32) for _ in range(2)]
            hb = [carve([D], BF16) for _ in range(2)]
            b_xt = [S.buf() for _ in range(2)]
            b_hb = [S.buf() for _ in range(2)]
            for i in range(NT):
                s = i % 2
                w_ = 1 if i < 2 else 0
                S.dma("sp", xt[s], xin(i), [b_xcur[i]], [b_xt[s]], b_xt[s])
                tt("dve", xt[s], xt[s], sc1[w_], ALU.mult, [b_xt[s], b_m1], [b_xt[s]])
                tt("pool", hb[s], xt[s], sh1[w_], ALU.add, [b_xt[s], b_m1], [b_hb[s]])
                for kc in range(8):
                    tr(psb(s)[:, kc * 128:(kc + 1) * 128], hb[s][:, kc * 128:(kc + 1) * 128], identb, [b_hb[s], b_id], [bps[s]])
                cp("act", hT[:, :, i * 128:(i + 1) * 128], psb(s).rearrange("p (a b) -> p a b", a=8), [bps[s]], [b_hT[i]])
            groups = []
            for k in range(2):
                groups.append((k * 512, k * 512, AF.Copy, ("rq", "rk")[k]))
            for k in range(2):
                groups.append((1024 + k * 512, 1024 + k * 512, AF.Copy, "rv%d" % k))
            for k in range(2):
                groups.append((2048 + k * 512, 2048 + k * 512, AF.Silu, "rg%d" % k))
            for k in range(2):
                groups.append((3072 + k * 512, 3072 + k * 512, AF.Copy, "aq%d" % k))
            groups.append((4096, 4096, AF.Copy, "kv"))
            for k in range(6):
                groups.append((6656 + k * 512, 4608 + k * 512, AF.Sigmoid, "sg%d" % k))
            wsl = [carve([8, 512], BF16) for _ in range(2)]
            b_wsl = [S.buf() for _ in range(2)]
            zt = [carve([512], BF16) for _ in range(3)]
            b_zt = [S.buf() for _ in range(3)]
            it = 0
            for gi, (wc0, zc0, func, nm) in enumerate(groups):
                s = gi % 2
                S.dma("pool", wsl[s], w_in[l, :, wc0:wc0 + 512].rearrange("(kc p) n -> p kc n", p=128),
                      [], [b_wsl[s]], b_wsl[s])
                for i in range(NT):
                    p = 2 + (it % 2)
                    q = it % 3
                    it += 1
                    for kc in range(8):
                        mm(psum[p][:, :], hT[:, kc, i * 128:(i + 1) * 128], wsl[s][:, kc, :], kc == 0, kc == 7,
                           [b_hT[i], b_wsl[s]], [bps[p]])
                    if func == AF.Copy:
                        cp("dve", zt[q], psum[p][:, :], [bps[p]], [b_zt[q]])
                    else:
                        act(zt[q], psum[p][:, :], func, [bps[p]], [b_zt[q]])
                    S.dma("sp", z_tok[i * 128:(i + 1) * 128, zc0:zc0 + 512], zt[q], [b_zt[q]], [zb(i, nm)], b_zt[q])
            wcs = [carve([8, 256], BF16) for _ in range(2)]
            b_wcs = [S.buf() for _ in range(2)]
            sgm = [carve([512], F32) for _ in range(2)]
            b_sgm = [S.buf() for _ in range(2)]
            glt = [carve([512], BF16) for _ in range(2)]
            b_glt = [S.buf() for _ in range(2)]
            b_glu = [S.buf() for _ in range(8)]
            it = 0
            for cc in range(8):
                s = cc % 2
                S.dma("pool", wcs[s][:, :, 0:128],
                      w_in[l, :, 4608 + cc * 128:4608 + (cc + 1) * 128].rearrange("(kc p) n -> p kc n", p=128),
                      [], [b_wcs[s]], b_wcs[s])
                S.dma("pool", wcs[s][:, :, 128:256],
                      w_in[l, :, 5632 + cc * 128:5632 + (cc + 1) * 128].rearrange("(kc p) n -> p kc n", p=128),
                      [], [b_wcs[s]], b_wcs[s])
                for tb in range(9):
                    t0 = tb * 512
                    n = min(512, T - t0)
                    tiles = range(t0 // 128, (t0 + n) // 128)
                    q = it % 2
                    it += 1
                    pa, pg = 4 + 2 * q, 5 + 2 * q
                    for kc in range(8):
                        mm(psum[pa][:, 0:n], wcs[s][:, kc, 0:128], hT[:, kc, t0:t0 + n], kc == 0, kc == 7,
                           [b_hT[i] for i in tiles] + [b_wcs[s]], [bps[pa]])
                    for kc in range(8):
                        mm(psum[pg][:, 0:n], wcs[s][:, kc, 128:256], hT[:, kc, t0:t0 + n], kc == 0, kc == 7,
                           [b_hT[i] for i in tiles] + [b_wcs[s]], [bps[pg]])
                    act(sgm[q][:, 0:n], psum[pg][:, 0:n], AF.Sigmoid, [bps[pg]], [b_sgm[q]])
                    tt("dve", glt[q][:, 0:n], psum[pa][:, 0:n], sgm[q][:, 0:n], ALU.mult, [bps[pa], b_sgm[q]], [b_glt[q]])
                    S.dma("sp", gluT[cc, :, t0:t0 + n], glt[q][:, 0:n], [b_glt[q]], [b_glu[cc]], b_glt[q])
            S.barrier()
            if dbg and dbg.get("stop") == "P1":
                break

            st["off"] = PERSIST
            kT2 = carve([4, T], BF16)
            b_kT = [S.buf() for _ in range(NT)]
            vext = carve([NT, 4, 192], BF16)
            b_v = [S.buf() for _ in range(NT)]
            b_vinit = S.buf()
            mset("pool", vext.rearrange("p a b c -> p (a b c)"), 0.0, [b_vinit])
            mset("pool", vext[:, :, :, 64:65], 1.0, [b_vinit])
            qg = carve([64], F32)
            kg = carve([64], F32)
            b_ng = S.buf()
            vec_bc(qg, qng[l:l + 1, :], 64, b_ng)
            vec_bc(kg, kng[l:l + 1, :], 64, b_ng)
            kvt = [carve([512], BF16) for _ in range(2)]
            b_kvt = [S.buf() for _ in range(2)]
            rp = [carve([64], F32) for _ in range(2)]
            b_rp = [S.buf() for _ in range(2)]
            sq = carve([D], F32)
            ss = carve([16], F32)
            qn = carve([16, 64], F32)
            tA = carve([16, 32], F32)
            tB = carve([16, 32], F32)
            kdup = carve([4, 2, 64], BF16)
            qb_ = carve([16, 64], BF16)
            b_tmp = S.buf()
            b_kd = S.buf()
            b_qb = S.buf()

            def normrope(src, H, gain, rpt, d1, d2, reads, wbuf):
                sv = sq[:, 0:H * 64].rearrange("p (h d) -> p h d", h=H)
                tt("dve", sv, src, src, ALU.mult, reads, [b_tmp])
                rsum("dve", ss[:, 0:H], sv, [b_tmp], [b_tmp])
                rstd_from("dve", ss[:, 0:H], ss[:, 0:H], 1.0 / 64.0, [b_tmp], [b_tmp])
                qv = qn[:, 0:H, :]
                tt("dve", qv, src, fv(ss[:, 0:H], [[1, H], [0, 64]]), ALU.mult, reads + [b_tmp], [b_tmp])
                tt("dve", qv, qv, fv(gain, [[0, H], [1, 64]]), ALU.mult, [b_tmp, b_ng], [b_tmp])
                rope("dve", d1, d2, qv, H, rpt, (tA[:, 0:H, :], tB[:, 0:H, :]), reads + [b_tmp], [b_tmp, wbuf])

            for i in range(NT):
                s = i % 2
                S.dma("sp", kvt[s], z_tok[i * 128:(i + 1) * 128, 4096:4608], [zb(i, "kv")], [b_kvt[s]], b_kvt[s])
                S.dma("sp", rp[s], rope_in[i * 128:(i + 1) * 128, :], [], [b_rp[s]], b_rp[s])
                ksrc = kvt[s][:, 0:256].rearrange("p (h d) -> p h d", h=4)
                normrope(ksrc, 4, kg, rp[s], kdup[:, :, 0, 0:32], kdup[:, :, 0, 32:64], [b_kvt[s], b_rp[s]], b_kd)
                cp("dve", kdup[:, :, 1, :], kdup[:, :, 0, :], [b_kd], [b_kd])
                for g in range(4):
                    tr(psb(7)[:, g * 128:(g + 1) * 128], kdup[:, g, :, :].rearrange("p a b -> p (a b)"), identb,
                       [b_kd, b_id], [bps[7]])
                cp("act", kT2[:, :, i * 128:(i + 1) * 128], psb(7, 512).rearrange("p (a b) -> p a b", a=4), [bps[7]], [b_kT[i]])
                vsrc = kvt[s][:, 256:512].rearrange("p (h d) -> p h d", h=4)
                cp("pool", vext[:, i, :, 0:64], vsrc, [b_kvt[s], b_vinit], [b_v[i]])
                cp("pool", vext[:, i, :, 128:192], vsrc, [b_kvt[s], b_vinit], [b_v[i]])
            aqt = [carve([D], BF16) for _ in range(2)]
            b_aqt = [S.buf() for _ in range(2)]
            qT = carve([8, 512], BF16)
            b_qT = S.buf()
            pT = [carve([512], BF16) for _ in range(3)]
            b_pT = [S.buf() for _ in range(3)]
            rden = carve([512], F32)
            b_rden = S.buf()
            osb = [carve([512], F32) for _ in range(2)]
            b_osb = [S.buf() for _ in range(2)]
            ablk = [carve([8, 512], BF16) for _ in range(2)]
            b_ablk = [S.buf() for _ in range(2)]
            b_att = [S.buf() for _ in range(9)]
            qblocks = [(0, 256, range(0, 2))] + [(256 + 512 * j, 512, range(NT)) for j in range(8)]
            its = 0
            ith = 0
            for bi, (t0, nq, kchunks) in enumerate(qblocks):
                ab = ablk[bi % 2]
                b_ab = b_ablk[bi % 2]
                for tq in range(nq // 128):
                    i = t0 // 128 + tq
                    s = tq % 2
                    S.dma("sp", aqt[s], z_tok[i * 128:(i + 1) * 128, 3072:4096],
                          [zb(i, "aq0"), zb(i, "aq1")], [b_aqt[s]], b_aqt[s])
                    S.dma("sp", rp[s], rope_in[i * 128:(i + 1) * 128, :], [], [b_rp[s]], b_rp[s])
                    qsrc = aqt[s].rearrange("p (h d) -> p h d", h=16)
                    normrope(qsrc, 16, qg, rp[s], qb_[:, :, 0:32], qb_[:, :, 32:64], [b_aqt[s], b_rp[s]], b_qb)
                    for hp in range(8):
                        tr(psb(7)[:, hp * 128:(hp + 1) * 128], qb_[:, 2 * hp:2 * hp + 2, :].rearrange("p a b -> p (a b)"),
                           identb, [b_qb, b_id], [bps[7]])
                    cp("dve", qT[:, :, tq * 128:(tq + 1) * 128], psb(7).rearrange("p (a b) -> p a b", a=8), [bps[7]], [b_qT])
                kl = list(kchunks)
                for h in range(16):
                    g, hp, r = h // 4, h // 2, h % 2
                    pr = slice(r * 64, (r + 1) * 64)
                    po = 3 + (ith % 2)
                    ob = ith % 2
                    ith += 1
                    for ci, c in enumerate(kl):
                        p = its % 3
                        its += 1
                        mm(psum[p][:, 0:nq], kT2[pr, g, c * 128:(c + 1) * 128], qT[pr, hp, 0:nq], True, True,
                           [b_kT[c], b_qT], [bps[p]])
                        act(pT[p][:, 0:nq], psum[p][:, 0:nq], AF.Exp, [bps[p]], [b_pT[p]], scale=0.125)
                        if r == 0:
                            mm(psum[po][0:65, 0:nq], vext[:, c, g, 0:65], pT[p][:, 0:nq], ci == 0, ci == len(kl) - 1,
                               [b_v[c], b_pT[p]], [bps[po]])
                        else:
                            mm(psum[po][:, 0:nq], vext[:, c, g, 64:192], pT[p][:, 0:nq], ci == 0, ci == len(kl) - 1,
                               [b_v[c], b_pT[p]], [bps[po]])
                    dr = 64 if r == 0 else 0
                    S.op("dve", lambda e, po=po, dr=dr, nq=nq: e.reciprocal(rden[dr:dr + 1, 0:nq], psum[po][dr:dr + 1, 0:nq]),
                         [bps[po]], [b_rden])
                    mm(psum[5][:, 0:nq], onesf[dr:dr + 1, :], rden[dr:dr + 1, 0:nq], True, True, [b_rden, b_id], [bps[5]])
                    cp("dve", osb[ob][pr, 0:nq], psum[po][pr, 0:nq], [bps[po]], [b_osb[ob]])
                    tt("dve", ab[pr, hp, 0:nq], osb[ob][pr, 0:nq], psum[5][pr, 0:nq], ALU.mult, [b_osb[ob], bps[5]], [b_ab])
                S.dma("sp", attT[:, :, t0:t0 + nq], ab[:, :, 0:nq], [b_ab], [b_att[bi]], b_ab)
            dump("kT2", kT2, b_kT)
            dump("vext", vext.rearrange("p a b c -> p (a b c)"), b_v)
            dump("qT", qT, [b_qT])
            dump("osb0", osb[0], [b_osb[0]])
            dump("osb1", osb[1], [b_osb[1]])
            dump("rden", rden, [b_rden])
            dump("pT0", pT[0], [b_pT[0]])
            dump("qn", qn, [b_tmp])
            dump("ss", ss, [b_tmp])
            S.barrier()
            if dbg and dbg.get("stop") == "P2":
                break

            st["off"] = PERSIST
            lgb = carve([16], F32)
            b_lg = S.buf()
            S.dma("sp", lgb, pbc(dlog[l:l + 1, :], 128), [], [b_lg], b_lg)
            act(lgb, lgb, AF.Exp, [b_lg], [b_lg], scale=-1.0)
            ts("dve", lgb, lgb, 1.0, None, ALU.add, None, [b_lg], [b_lg])
            act(lgb, lgb, AF.Ln, [b_lg], [b_lg])
            ts("dve", lgb, lgb, -1.0, None, ALU.mult, None, [b_lg], [b_lg])
            relt = carve([384], F32)
            posc = carve([258], F32)
            b_cst = S.buf()
            S.dma("sp", relt, relt_in, [], [b_cst], b_cst)
            S.dma("sp", posc, posc_in, [], [b_cst], b_cst)
            Dtot = carve([8, 128], F32)
            Dtmp = carve([8, 128], F32)
            b_tab = S.buf()
            relv = fv(relt[:, 0:128], [[0, 8], [1, 128]])
            tt("dve", Dtot, relv, fv(lgb[:, 0:8], [[1, 8], [0, 128]]), ALU.mult, [b_cst, b_lg], [b_tab])
            act(Dtot, Dtot, AF.Exp, [b_tab], [b_tab])
            tt("dve", Dtot, Dtot, fv(relt[:, 128:256], [[0, 8], [1, 128]]), ALU.mult, [b_tab, b_cst], [b_tab])
            tt("dve", Dtmp, relv, fv(lgb[:, 8:16], [[1, 8], [0, 128]]), ALU.mult, [b_cst, b_lg], [b_tab])
            act(Dtmp, Dtmp, AF.Exp, [b_tab], [b_tab], scale=-1.0)
            tt("dve", Dtmp, Dtmp, fv(relt[:, 256:384], [[0, 8], [1, 128]]), ALU.mult, [b_tab, b_cst], [b_tab])
            tt("dve", Dtot, Dtot, Dtmp, ALU.add, [b_tab], [b_tab])
            ts("dve", Dtot, Dtot, 0.125, None, ALU.mult, None, [b_tab], [b_tab])
            qwf = carve([8, 128], F32)
            qwb = carve([8, 128], F32)
            tt("dve", qwf, fv(posc[:, 0:128], [[0, 8], [1, 128]]), fv(lgb[:, 0:8], [[1, 8], [0, 128]]), ALU.mult, [b_cst, b_lg], [b_tab])
            act(qwf, qwf, AF.Exp, [b_tab], [b_tab])
            tt("dve", qwb, fv(posc[:, 128:256], [[0, 8], [1, 128]]), fv(lgb[:, 8:16], [[1, 8], [0, 128]]), ALU.mult, [b_cst, b_lg], [b_tab])
            act(qwb, qwb, AF.Exp, [b_tab], [b_tab])
            kwf = carve([8], F32)
            kwb = carve([8], F32)
            ts("dve", kwf, lgb[:, 0:8], posc[:, 256:257], None, ALU.mult, None, [b_cst, b_lg], [b_tab])
            act(kwf, kwf, AF.Exp, [b_tab], [b_tab])
            ts("dve", kwf, kwf, 0.125, None, ALU.mult, None, [b_tab], [b_tab])
            ts("dve", kwb, lgb[:, 8:16], posc[:, 257:258], None, ALU.mult, None, [b_cst, b_lg], [b_tab])
            act(kwb, kwb, AF.Exp, [b_tab], [b_tab])
            ts("dve", kwb, kwb, 0.125, None, ALU.mult, None, [b_tab], [b_tab])
            Gf = carve([8, 128], F32)
            Gb = carve([8, 128], F32)
            ts("dve", Gf.rearrange("p a b -> p (a b)")[:, 0:8], lgb[:, 0:8], 128.0, None, ALU.mult, None, [b_lg], [b_tab])
            act(Gf.rearrange("p a b -> p (a b)")[:, 8:16], Gf.rearrange("p a b -> p (a b)")[:, 0:8], AF.Exp, [b_tab], [b_tab])
            ts("dve", Gb.rearrange("p a b -> p (a b)")[:, 0:8], lgb[:, 8:16], 128.0, None, ALU.mult, None, [b_lg], [b_tab])
            act(Gb.rearrange("p a b -> p (a b)")[:, 8:16], Gb.rearrange("p a b -> p (a b)")[:, 0:8], AF.Exp, [b_tab], [b_tab])
            gtmp = carve([16], F32)
            cp("dve", gtmp[:, 0:8], Gf.rearrange("p a b -> p (a b)")[:, 8:16], [b_tab], [b_tab])
            cp("dve", gtmp[:, 8:16], Gb.rearrange("p a b -> p (a b)")[:, 8:16], [b_tab], [b_tab])
            cp("dve", Gf, fv(gtmp[:, 0:8], [[1, 8], [0, 128]]), [b_tab], [b_tab])
            cp("dve", Gb, fv(gtmp[:, 8:16], [[1, 8], [0, 128]]), [b_tab], [b_tab])

            rp = [carve([64], F32) for _ in range(2)]
            b_rp = [S.buf() for _ in range(2)]
            Sprev = carve([NT, 8, 128], BF16)
            b_sp = [S.buf() for _ in range(NT)]
            Sst = carve([8, 128], F32)
            b_S = S.buf()
            rkt = [carve([512], BF16) for _ in range(2)]
            rvt = [carve([D], BF16) for _ in range(2)]
            rqt = [carve([512], BF16) for _ in range(2)]
            sgt = [carve([D], BF16) for _ in range(2)]
            b_ld = [S.buf() for _ in range(2)]
            kr = carve([8, 64], F32)
            kw = carve([8, 64], BF16)
            krb = carve([8, 64], BF16)
            qrb = carve([8, 64], BF16)
            tA = carve([8, 32], F32)
            tB = carve([8, 32], F32)
            b_k = S.buf()
            b_kw = S.buf()
            b_q = S.buf()
            mset("dve", Sst[0:64], 0.0, [b_S])
            order = [1, 0] + list(range(NT - 1, 1, -1))
            for oi, n in enumerate(order):
                s = oi % 2
                cp("act", Sprev[0:64, n], Sst[0:64], [b_S], [b_sp[n]])
                S.dma("sp", rkt[s], z_tok[n * 128:(n + 1) * 128, 512:1024], [zb(n, "rk")], [b_ld[s]], b_ld[s])
                S.dma("sp", rvt[s], z_tok[n * 128:(n + 1) * 128, 1024:2048], [zb(n, "rv0"), zb(n, "rv1")], [b_ld[s]], b_ld[s])
                S.dma("sp", rp[s], rope_in[n * 128:(n + 1) * 128, :], [], [b_rp[s]], b_rp[s])
                ksrc = rkt[s].rearrange("p (h d) -> p h d", h=8)
                rope("pool", kr[:, :, 0:32], kr[:, :, 32:64], ksrc, 8, rp[s], (tA, tB), [b_ld[s], b_rp[s]], [b_k])
                tt("pool", kw, kr, fv(kwb, [[1, 8], [0, 64]]), ALU.mult, [b_k, b_tab], [b_kw])
                for h in range(8):
                    pk = 0 + h // 4
                    mm(psum[pk][0:64, (h % 4) * 128:(h % 4 + 1) * 128], kw[:, h, :], rvt[s][:, h * 128:(h + 1) * 128], True, True,
                       [b_kw, b_ld[s]], [bps[pk]])
                tt("dve", Sst[0:64], Sst[0:64], Gb[0:64], ALU.mult, [b_S, b_tab], [b_S])
                for hh in range(2):
                    tt("dve", Sst[0:64, hh * 4:(hh + 1) * 4, :], Sst[0:64, hh * 4:(hh + 1) * 4, :],
                       psum[hh][0:64, :].rearrange("p (a b) -> p a b", a=4), ALU.add, [b_S, bps[hh]], [b_S])
            qTs = carve([8, 128], BF16)
            kTs = carve([8, 128], BF16)
            b_qk = S.buf()
            scT = carve([8, 128], BF16)
            b_sc = S.buf()
            qwfq = carve([8, 128], BF16)
            qwbq = carve([8, 128], BF16)
            b_qw = S.buf()
            Sfb = carve([8, 128], BF16)
            b_sfb = S.buf()
            ysb = carve([8, 128], F32)
            ysq = carve([8, 128], F32)
            mst = carve([32], F32)
            b_y = S.buf()
            rtn = carve([D], BF16)
            b_rtn = S.buf()
            rTt = [carve([8, 128], BF16) for _ in range(2)]
            b_rTt = [S.buf() for _ in range(2)]
            b_retT = [S.buf() for _ in range(NT)]
            mset("dve", Sst[0:64], 0.0, [b_S])
            for n in range(NT):
                s = n % 2
                S.dma("sp", rkt[s], z_tok[n * 128:(n + 1) * 128, 512:1024], [zb(n, "rk")], [b_ld[s]], b_ld[s])
                S.dma("sp", rvt[s], z_tok[n * 128:(n + 1) * 128, 1024:2048], [zb(n, "rv0"), zb(n, "rv1")], [b_ld[s]], b_ld[s])
                S.dma("sp", rqt[s], z_tok[n * 128:(n + 1) * 128, 0:512], [zb(n, "rq")], [b_ld[s]], b_ld[s])
                S.dma("sp", sgt[s], z_tok[n * 128:(n + 1) * 128, 2048:3072], [zb(n, "rg0"), zb(n, "rg1")], [b_ld[s]], b_ld[s])
                S.dma("sp", rp[s], rope_in[n * 128:(n + 1) * 128, :], [], [b_rp[s]], b_rp[s])
                ksrc = rkt[s].rearrange("p (h d) -> p h d", h=8)
                qsrc = rqt[s].rearrange("p (h d) -> p h d", h=8)
                rope("pool", kr[:, :, 0:32], kr[:, :, 32:64], ksrc, 8, rp[s], (tA, tB), [b_ld[s], b_rp[s]], [b_k])
                tt("pool", kw, kr, fv(kwf, [[1, 8], [0, 64]]), ALU.mult, [b_k, b_tab], [b_kw])
                cp("pool", krb, kr, [b_k], [b_kw])
                rope("pool", qrb[:, :, 0:32], qrb[:, :, 32:64], qsrc, 8, rp[s], (tA, tB), [b_ld[s], b_rp[s]], [b_q])
                for h in range(8):
                    tr(psb(6)[0:64, h * 128:(h + 1) * 128], qrb[:, h, :], identb, [b_q, b_id], [bps[6]])
                for h in range(8):
                    tr(psb(7)[0:64, h * 128:(h + 1) * 128], krb[:, h, :], identb, [b_kw, b_id], [bps[7]])
                cp("act", qTs[0:64], psb(6)[0:64].rearrange("p (a b) -> p a b", a=8), [bps[6]], [b_qk])
                cp("act", kTs[0:64], psb(7)[0:64].rearrange("p (a b) -> p a b", a=8), [bps[7]], [b_qk])
                for h in range(8):
                    pk = 0 + h // 4
                    mm(psum[pk][:, (h % 4) * 128:(h % 4 + 1) * 128], kTs[0:64, h, :], qTs[0:64, h, :], True, True,
                       [b_qk], [bps[pk]])
                for hh in range(2):
                    tt("dve", scT[:, hh * 4:(hh + 1) * 4, :], psum[hh][:, :].rearrange("p (a b) -> p a b", a=4),
                       Dtot[:, hh * 4:(hh + 1) * 4, :], ALU.mult, [bps[hh], b_tab], [b_sc])
                tt("pool", qwfq[0:64], qTs[0:64], qwf[0:64], ALU.mult, [b_qk, b_tab], [b_qw])
                tt("pool", qwbq[0:64], qTs[0:64], qwb[0:64], ALU.mult, [b_qk, b_tab], [b_qw])
                cp("act", Sfb[0:64], Sst[0:64], [b_S], [b_sfb])
                for h in range(8):
                    pk = 2 + h // 4
                    o_ = psum[pk][:, (h % 4) * 128:(h % 4 + 1) * 128]
                    mm(o_, scT[:, h, :], rvt[s][:, h * 128:(h + 1) * 128], True, False, [b_sc, b_ld[s]], [bps[pk]])
                    mm(o_, qwfq[0:64, h, :], Sfb[0:64, h, :], False, False, [b_qw, b_sfb], [bps[pk]])
                    mm(o_, qwbq[0:64, h, :], Sprev[0:64, n, h, :], False, True, [b_qw, b_sp[n]], [bps[pk]])
                for h in range(8):
                    pk = 4 + h // 4
                    mm(psum[pk][0:64, (h % 4) * 128:(h % 4 + 1) * 128], kw[:, h, :], rvt[s][:, h * 128:(h + 1) * 128], True, True,
                       [b_kw, b_ld[s]], [bps[pk]])
                tt("dve", Sst[0:64], Sst[0:64], Gf[0:64], ALU.mult, [b_S, b_tab, b_sfb], [b_S])
                for hh in range(2):
                    tt("dve", Sst[0:64, hh * 4:(hh + 1) * 4, :], Sst[0:64, hh * 4:(hh + 1) * 4, :],
                       psum[4 + hh][0:64, :].rearrange("p (a b) -> p a b", a=4), ALU.add, [b_S, bps[4 + hh]], [b_S])
                for hh in range(2):
                    cp("act", ysb[:, hh * 4:(hh + 1) * 4, :], psum[2 + hh][:, :].rearrange("p (a b) -> p a b", a=4), [bps[2 + hh]], [b_y])
                rsum("dve", mst[:, 0:8], ysb, [b_y], [b_y])
                tt("pool", ysq, ysb, ysb, ALU.mult, [b_y], [b_y])
                rsum("dve", mst[:, 8:16], ysq, [b_y], [b_y])
                ts("dve", mst[:, 0:8], mst[:, 0:8], 1.0 / 128.0, None, ALU.mult, None, [b_y], [b_y])
                tt("dve", mst[:, 16:24], mst[:, 0:8], mst[:, 0:8], ALU.mult, [b_y], [b_y])
                stt("dve", mst[:, 8:16], mst[:, 8:16], 1.0 / 128.0, mst[:, 16:24], ALU.mult, ALU.subtract, [b_y], [b_y])
                rstd_from("dve", mst[:, 8:16], mst[:, 8:16], 1.0, [b_y], [b_y])
                tt("dve", ysb, ysb, fv(mst[:, 0:8], [[1, 8], [0, 128]]), ALU.subtract, [b_y], [b_y])
                tt("pool", ysb, ysb, fv(mst[:, 8:16], [[1, 8], [0, 128]]), ALU.mult, [b_y], [b_y])
                tt("pool", rtn, ysb.rearrange("p a b -> p (a b)"), sgt[s], ALU.mult, [b_y, b_ld[s]], [b_rtn])
                for kc in range(8):
                    tr(psb(6)[:, kc * 128:(kc + 1) * 128], rtn[:, kc * 128:(kc + 1) * 128], identb, [b_rtn, b_id], [bps[6]])
                cp("act", rTt[s], psb(6).rearrange("p (a b) -> p a b", a=8), [bps[6]], [b_rTt[s]])
                S.dma("sp", retT[:, :, n * 128:(n + 1) * 128], rTt[s], [b_rTt[s]], [b_retT[n]], b_rTt[s])
            S.barrier()
            if dbg and dbg.get("stop") == "P3":
                break

            st["off"] = PERSIST
            GP = 286 + 30 + 4096
            gp = carve([8, GP], BF16)
            b_gp = S.buf()
            mset("pool", gp.rearrange("p a b -> p (a b)"), 0.0, [b_gp])
            for cc in range(8):
                S.dma("sp", gp[:, cc, 15:271], gluT[cc, :, 0:256], [b_glu[cc]], [b_gp], b_gp)
                S.dma("sp", gp[:, cc, 301:301 + 4096], gluT[cc, :, 256:T], [b_glu[cc]], [b_gp], b_gp)
            cw = carve([D], F32, nparts=31)
            b_cw = S.buf()
            S.dma("sp", cw, conv_dw[l], [], [b_cw], b_cw)
            wdT = carve([8, 32], F32)
            for cc in range(8):
                tr(psum[0][:, cc * 32:cc * 32 + 31], cw[0:31, cc * 128:(cc + 1) * 128], identf[0:31, 0:31], [b_cw, b_id], [bps[0]])
            cp("dve", wdT[:, :, 0:31], psum[0][:, 0:256].rearrange("p (a b) -> p a b", a=8)[:, :, 0:31], [bps[0]], [b_cw])
            cv = carve([128], F32, nparts=24)
            S.dma("sp", cv, cvp[l], [], [b_cw], b_cw)
            cvT = carve([24], F32)
            tr(psum[1][:, 0:24], cv[0:24, :], identf[0:24, 0:24], [b_cw, b_id], [bps[1]])
            cp("dve", cvT, psum[1][:, 0:24], [bps[1]], [b_cw])
            dgw = carve([8, 31, 128], BF16)
            b_dg = S.buf()
            for cc in range(8):
                tt("pool", dgw[:, cc], fv(identf, [[0, 31], [1, 128]]), fv(wdT[:, cc, 0:31], [[1, 31], [0, 128]]), ALU.mult,
                   [b_cw, b_id], [b_dg])
            yf = carve([8, 512], F32)
            ysq2 = carve([8, 512], F32)
            b_yf = S.buf()
            mean = carve([512], F32)
            rstd = carve([512], F32)
            b_st = S.buf()
            cblk = [carve([8, 512], BF16) for _ in range(2)]
            b_cblk = [S.buf() for _ in range(2)]
            b_convT = [S.buf() for _ in range(9)]
            tmpc = carve([512], F32)
            b_tc = S.buf()
            cblocks = [(0, 256, 0)] + [(256 + 512 * j, 512, 286 + 512 * j) for j in range(8)]
            it = 0
            for bi, (t0, n, g0) in enumerate(cblocks):
                cb = cblk[bi % 2]
                b_cb = b_cblk[bi % 2]
                for cc in range(8):
                    p = it % 2
                    it += 1
                    for k in range(31):
                        mm(psum[p][:, 0:n], dgw[:, cc, k, :], gp[:, cc, g0 + k:g0 + k + n], k == 0, k == 30,
                           [b_dg, b_gp], [bps[p]])
                    act(yf[:, cc, 0:n], psum[p][:, 0:n], AF.Identity, [bps[p], b_cw], [b_yf], bias=cvT[:, cc:cc + 1])
                    tt("pool", ysq2[:, cc, 0:n], yf[:, cc, 0:n], yf[:, cc, 0:n], ALU.mult, [b_yf], [b_yf])
                for cc in range(8):
                    mm(psum[2][:, 0:n], onesm, yf[:, cc, 0:n], cc == 0, cc == 7, [b_yf, b_id], [bps[2]])
                for cc in range(8):
                    mm(psum[3][:, 0:n], onesm, ysq2[:, cc, 0:n], cc == 0, cc == 7, [b_yf, b_id], [bps[3]])
                cp("act", mean[:, 0:n], psum[2][:, 0:n], [bps[2]], [b_st])
                tt("dve", rstd[:, 0:n], mean[:, 0:n], mean[:, 0:n], ALU.mult, [b_st], [b_st])
                tt("dve", rstd[:, 0:n], psum[3][:, 0:n], rstd[:, 0:n], ALU.subtract, [b_st, bps[3]], [b_st])
                rstd_from("dve", rstd[:, 0:n], rstd[:, 0:n], 1.0, [b_st], [b_st])
                for cc in range(8):
                    tt("dve", tmpc[:, 0:n], yf[:, cc, 0:n], mean[:, 0:n], ALU.subtract, [b_yf, b_st], [b_tc])
                    tt("pool", tmpc[:, 0:n], tmpc[:, 0:n], rstd[:, 0:n], ALU.mult, [b_tc, b_st], [b_tc])
                    act(cb[:, cc, 0:n], tmpc[:, 0:n], AF.Silu, [b_tc, b_cw], [b_cb],
                        bias=cvT[:, 16 + cc:17 + cc], scale=cvT[:, 8 + cc:9 + cc])
                S.dma("sp", convT[:, :, t0:t0 + n], cb[:, :, 0:n], [b_cb], [b_convT[bi]], b_cb)
            S.barrier()
            if dbg and dbg.get("stop") == "P4":
                break

            st["off"] = PERSIST
            wbr = [carve([8, D], BF16) for _ in range(3)]
            b_wbr = S.buf()
            for k, wsrc in enumerate((w_ret_o, w_att_o, w_conv_o)):
                S.dma("pool", wbr[k], wsrc[l].rearrange("(kc p) n -> p kc n", p=128), [], [b_wbr], b_wbr)
            brt = [[carve([8, 128], BF16) for _ in range(3)] for _ in range(2)]
            sgl = [carve([3 * D], BF16) for _ in range(2)]
            b_in5 = [S.buf() for _ in range(2)]
            m0 = carve([D], F32)
            m1 = carve([D], F32)
            mgb = carve([D], BF16)
            b_m = S.buf()
            b_mg = S.buf()
            mTt = [carve([8, 128], BF16) for _ in range(2)]
            b_mTt = [S.buf() for _ in range(2)]
            b_mrg = [S.buf() for _ in range(NT)]
            for i in range(NT):
                s = i % 2
                bi_att = 0 if i < 2 else 1 + (i - 2) // 4
                for k, (src, bsrc) in enumerate(((retT, b_retT[i]), (attT, b_att[bi_att]), (convT, b_convT[bi_att]))):
                    S.dma("sp", brt[s][k], src[:, :, i * 128:(i + 1) * 128], [bsrc], [b_in5[s]], b_in5[s])
                S.dma("sp", sgl[s], z_tok[i * 128:(i + 1) * 128, 4608:7680], [zb(i, "sg%d" % k) for k in range(6)],
                      [b_in5[s]], b_in5[s])
                for k in range(3):
                    for hf in range(2):
                        p = 2 * k + hf
                        for kc in range(8):
                            mm(psum[p][:, :], brt[s][k][:, kc, :], wbr[k][:, kc, hf * 512:(hf + 1) * 512], kc == 0, kc == 7,
                               [b_in5[s], b_wbr], [bps[p]])
                for hf in range(2):
                    c0 = hf * 512
                    tt("dve", m0[:, c0:c0 + 512], psum[hf][:, :], sgl[s][:, c0:c0 + 512], ALU.mult, [bps[hf], b_in5[s]], [b_m])
                    tt("dve", m1[:, c0:c0 + 512], psum[2 + hf][:, :], sgl[s][:, D + c0:D + c0 + 512], ALU.mult, [bps[2 + hf], b_in5[s]], [b_m])
                tt("pool", m0, m0, m1, ALU.add, [b_m], [b_m])
                for hf in range(2):
                    c0 = hf * 512
                    tt("dve", m1[:, c0:c0 + 512], psum[4 + hf][:, :], sgl[s][:, 2 * D + c0:2 * D + c0 + 512], ALU.mult, [bps[4 + hf], b_in5[s]], [b_m])
                tt("pool", mgb, m0, m1, ALU.add, [b_m], [b_mg])
                for kc in range(8):
                    tr(psb(6 + s)[:, kc * 128:(kc + 1) * 128], mgb[:, kc * 128:(kc + 1) * 128], identb, [b_mg, b_id], [bps[6 + s]])
                cp("act", mTt[s], psb(6 + s).rearrange("p (a b) -> p a b", a=8), [bps[6 + s]], [b_mTt[s]])
                S.dma("sp", mrgT[:, :, i * 128:(i + 1) * 128], mTt[s], [b_mTt[s]], [b_mrg[i]], b_mTt[s])
            S.barrier()
            if dbg and dbg.get("stop") == "P5a":
                break

            st["off"] = PERSIST
            h2T = carve([8, T], BF16)
            b_h2T = [S.buf() for _ in range(NT)]
            Gall = carve([NT, 65], F32)
            b_G = [S.buf() for _ in range(NT)]
            b_Gi = S.buf()
            mset("pool", Gall.rearrange("p a b -> p (a b)"), 1.0, [b_Gi])
            MOE_BASE = st["off"]
            wo = carve([8, D], BF16)
            b_wo = S.buf()
            S.dma("pool", wo, w_out[l].rearrange("(kc p) n -> p kc n", p=128), [], [b_wo], b_wo)
            wr = carve([8, NE], F32)
            S.dma("sp", wr, w_router[l].rearrange("(kc p) n -> p kc n", p=128), [], [b_wo], b_wo)
            rb = carve([NE], F32)
            S.dma("sp", rb, pbc(rbias[l:l + 1, :], 128), [], [b_wo], b_wo)
            g1 = [carve([D], F32) for _ in range(2)]
            sc2 = [carve([D], F32) for _ in range(2)]
            sh2 = [carve([D], F32) for _ in range(2)]
            lg_ = carve([D], F32)
            lb_ = carve([D], F32)
            b_m5 = S.buf()
            for w_ in range(2):
                mod_bc(g1[w_], l, w_, 2, b_m5)
                mod_bc(sc2[w_], l, w_, 4, b_m5, plus1=True)
                mod_bc(sh2[w_], l, w_, 3, b_m5)
            vec_bc(lg_, ln1_g[l:l + 1, :], D, b_m5)
            vec_bc(lb_, ln1_b[l:l + 1, :], D, b_m5)
            mT5 = [carve([8, 128], BF16) for _ in range(2)]
            x5 = [carve([D], F32) for _ in range(2)]
            b_l5 = [S.buf() for _ in range(2)]
            u5 = carve([D], F32)
            v5 = carve([D], F32)
            st5 = carve([8], F32)
            b_u = S.buf()
            x1t = [carve([D], F32) for _ in range(2)]
            b_x1 = [S.buf() for _ in range(2)]
            h2f = carve([D], F32)
            b_h2 = S.buf()
            h2loT = carve([8, 128], BF16)
            b_h2Tf = S.buf()
            h2hi = carve([D], BF16)
            h2lo = carve([D], BF16)
            b_h2s = S.buf()
            wrh = carve([8, NE], BF16)
            wrl = carve([8, NE], BF16)
            wrt = carve([8, NE], F32)
            cp("dve", wrh, wr, [b_wo], [b_wo])
            cp("dve", wrt, wrh, [b_wo], [b_wo])
            tt("dve", wrl, wr, wrt, ALU.subtract, [b_wo], [b_wo])
            rs = carve([NE], F32)
            rsel = carve([NE], F32)
            rt1 = carve([NE], F32)
            rg8 = carve([32], F32)
            b_r = S.buf()

            def layer_norm(eng2, dst, u, gam, bet, reads, wb):
                rsum("dve", st5[:, 0:1], u, reads, [b_u])
                tt(eng2, v5, u, u, ALU.mult, reads, [b_u])
                rsum("dve", st5[:, 1:2], v5, [b_u], [b_u])
                ts("dve", st5[:, 0:1], st5[:, 0:1], 1.0 / D, None, ALU.mult, None, [b_u], [b_u])
                tt("dve", st5[:, 2:3], st5[:, 0:1], st5[:, 0:1], ALU.mult, [b_u], [b_u])
                stt("dve", st5[:, 1:2], st5[:, 1:2], 1.0 / D, st5[:, 2:3], ALU.mult, ALU.subtract, [b_u], [b_u])
                rstd_from("dve", st5[:, 1:2], st5[:, 1:2], 1.0, [b_u], [b_u])
                ts("dve", v5, u, st5[:, 0:1], st5[:, 1:2], ALU.subtract, ALU.mult, reads + [b_u], [b_u])
                tt(eng2, v5, v5, gam, ALU.mult, [b_u, b_m5], [b_u])
                tt(eng2, dst, v5, bet, ALU.add, [b_u, b_m5], wb)

            for i in range(NT):
                s = i % 2
                w_ = 1 if i < 2 else 0
                S.dma("sp", mT5[s], mrgT[:, :, i * 128:(i + 1) * 128], [b_mrg[i]], [b_l5[s]], b_l5[s])
                S.dma("sp", x5[s], xin(i), [b_xcur[i]], [b_l5[s]], b_l5[s])
                for hf in range(2):
                    for kc in range(8):
                        mm(psum[hf][:, :], mT5[s][:, kc, :], wo[:, kc, hf * 512:(hf + 1) * 512], kc == 0, kc == 7,
                           [b_l5[s], b_wo], [bps[hf]])
                for hf in range(2):
                    c0 = hf * 512
                    tt("dve", u5[:, c0:c0 + 512], psum[hf][:, :], g1[w_][:, c0:c0 + 512], ALU.mult, [bps[hf], b_m5], [b_u])
                stt("dve", u5, x5[s], ALPHA, u5, ALU.mult, ALU.add, [b_l5[s], b_u], [b_u])
                layer_norm("pool", x1t[s], u5, lg_, lb_, [b_u], [b_x1[s]])
                S.dma("sp", xres[i * 128:(i + 1) * 128, :], x1t[s], [b_x1[s]], [b_xres[i]], b_x1[s])
                tt("pool", h2f, x1t[s], sc2[w_], ALU.mult, [b_x1[s], b_m5], [b_h2])
                tt("pool", h2f, h2f, sh2[w_], ALU.add, [b_h2, b_m5], [b_h2])
                cp("pool", h2hi, h2f, [b_h2], [b_h2s])
                tt("pool", h2lo, h2f, h2hi, ALU.subtract, [b_h2, b_h2s], [b_h2s])
                for kc in range(8):
                    tr(psb(2)[:, kc * 128:(kc + 1) * 128], h2hi[:, kc * 128:(kc + 1) * 128], identb, [b_h2s, b_id], [bps[2]])
                for kc in range(8):
                    tr(psb(3)[:, kc * 128:(kc + 1) * 128], h2lo[:, kc * 128:(kc + 1) * 128], identb, [b_h2s, b_id], [bps[3]])
                cp("act", h2T[:, :, i * 128:(i + 1) * 128], psb(2).rearrange("p (a b) -> p a b", a=8), [bps[2]], [b_h2T[i]])
                cp("dve", h2loT, psb(3).rearrange("p (a b) -> p a b", a=8), [bps[3]], [b_h2Tf])
                for kc in range(8):
                    hT_i = h2T[:, kc, i * 128:(i + 1) * 128]
                    mm(psum[4][:, 0:NE], hT_i, wrh[:, kc, :], kc == 0, False, [b_h2T[i], b_wo], [bps[4]])
                    mm(psum[4][:, 0:NE], h2loT[:, kc, :], wrh[:, kc, :], False, False, [b_h2Tf, b_wo], [bps[4]])
                    mm(psum[4][:, 0:NE], hT_i, wrl[:, kc, :], False, kc == 7, [b_h2T[i], b_wo], [bps[4]])
                act(rs, psum[4][:, 0:NE], AF.Sigmoid, [bps[4]], [b_r])
                R = [b_r]
                if dbg and dbg.get("noroute"):
                    continue
                tt("dve", rsel, rs, rb, ALU.add, R + [b_wo], R)
                sel3 = rsel.rearrange("p (g e) -> p g e", g=8)
                S.op("dve", lambda e, sel3=sel3: e.tensor_reduce(rg8[:, 0:8], sel3, AX.X, ALU.max), R, R)
                t13 = rt1.rearrange("p (g e) -> p g e", g=8)
                tt("dve", t13, sel3, fv(rg8[:, 0:8], [[1, 8], [0, 8]]), ALU.is_ge, R, R)
                stt("dve", rt1, rt1, -1.0e4, rsel, ALU.mult, ALU.add, R, R)
                S.op("dve", lambda e, t13=t13: e.tensor_reduce(rg8[:, 8:16], t13, AX.X, ALU.max), R, R)
                tt("dve", rg8[:, 0:8], rg8[:, 0:8], rg8[:, 8:16], ALU.add, R, R)
                S.op("dve", lambda e: e.max(rg8[:, 16:24], rg8[:, 0:8]), R, R)
                ts("dve", rg8[:, 24:32], rg8[:, 0:8], rg8[:, 19:20], None, ALU.is_ge, None, R, R)
                ts("dve", rg8[:, 8:16], rg8[:, 24:32], 1.0e4, -1.0e4, ALU.mult, ALU.add, R, R)
                tt("dve", t13, sel3, fv(rg8[:, 24:32], [[1, 8], [0, 8]]), ALU.mult, R, R)
                tt("dve", t13, t13, fv(rg8[:, 8:16], [[1, 8], [0, 8]]), ALU.add, R, R)
                S.op("dve", lambda e: e.max(rg8[:, 16:24], rt1), R, R)
                ts("dve", rt1, rt1, rg8[:, 23:24], None, ALU.is_ge, None, R, R)
                tt("dve", rt1, rt1, rs, ALU.mult, R, R)
                rsum("dve", rg8[:, 0:1], rt1, R, R)
                S.op("dve", lambda e: e.reciprocal(rg8[:, 1:2], rg8[:, 0:1]), R, R)
                ts("dve", Gall[:, i, 0:NE], rt1, rg8[:, 1:2], 2.5, ALU.mult, ALU.mult, R + [b_Gi], [b_G[i]])
            if dbg and dbg.get("stop") == "P5b":
                dump("Gall", Gall.rearrange("p a b -> p (a b)"), b_G)
                S.barrier()
                break
            S.barrier()

            st["off"] = MOE_BASE
            SBT = [(0, 12), (12, 12), (24, 10)]
            acc = carve([12, D], F32)
            b_acc = [S.buf() for _ in range(12)]
            wg = [carve([8, 256], BF16) for _ in range(2)]
            wu = [carve([8, 256], BF16) for _ in range(2)]
            wd = [carve([2, D], BF16) for _ in range(2)]
            b_we = [S.buf() for _ in range(2)]
            sil = [carve([512], BF16) for _ in range(2)]
            b_sil = [S.buf() for _ in range(2)]
            hid = [carve([2, 512], BF16) for _ in range(2)]
            b_hid = [S.buf() for _ in range(2)]
            g2 = [carve([D], F32) for _ in range(2)]
            l2g = carve([D], F32)
            l2b = carve([D], F32)
            b_m6 = S.buf()
            for w_ in range(2):
                mod_bc(g2[w_], l, w_, 5, b_m6)
            vec_bc(l2g, ln2_g[l:l + 1, :], D, b_m6)
            vec_bc(l2b, ln2_b[l:l + 1, :], D, b_m6)
            x6 = [carve([D], F32) for _ in range(2)]
            b_x6 = [S.buf() for _ in range(2)]
            v5 = carve([D], F32)
            st5 = carve([8], F32)
            b_u = S.buf()
            b_m5 = b_m6
            ite = 0
            itb = 0
            itd = 0
            for (tile0, ntile) in SBT:
                blocks = []
                k = 0
                while k < ntile:
                    nb = min(4, ntile - k)
                    blocks.append((tile0 + k, nb))
                    k += nb
                for e in range(NE + 1):
                    s = ite % 2
                    ite += 1
                    if e < NE:
                        srcs = (w_eg[l, e], w_eu[l, e], w_ed[l, e])
                    else:
                        srcs = (w_sg[l], w_su[l], w_sd[l])
                    S.dma("pool", wg[s], srcs[0].rearrange("(kc p) f -> p kc f", p=128), [], [b_we[s]], b_we[s])
                    S.dma("pool", wu[s], srcs[1].rearrange("(kc p) f -> p kc f", p=128), [], [b_we[s]], b_we[s])
                    S.dma("pool", wd[s], srcs[2].rearrange("(fc p) n -> p fc n", p=128), [], [b_we[s]], b_we[s])
                    for (ti0, ntl) in blocks:
                        n = ntl * 128
                        t0 = ti0 * 128
                        hs = itb % 2
                        itb += 1
                        hreads = [b_h2T[ti0 + k] for k in range(ntl)] + [b_we[s]]
                        for fc in range(2):
                            pg, pu = fc, 2 + fc
                            for kc in range(8):
                                mm(psum[pg][:, 0:n], wg[s][:, kc, fc * 128:(fc + 1) * 128], h2T[:, kc, t0:t0 + n], kc == 0, kc == 7,
                                   hreads, [bps[pg]])
                            for kc in range(8):
                                mm(psum[pu][:, 0:n], wu[s][:, kc, fc * 128:(fc + 1) * 128], h2T[:, kc, t0:t0 + n], kc == 0, kc == 7,
                                   hreads, [bps[pu]])
                            act(sil[fc][:, 0:n], psum[pg][:, 0:n], AF.Silu, [bps[pg]], [b_sil[fc]])
                            tt("dve", hid[hs][:, fc, 0:n], psum[pu][:, 0:n], sil[fc][:, 0:n], ALU.mult, [bps[pu], b_sil[fc]], [b_hid[hs]])
                        for k in range(ntl):
                            ti = ti0 + k
                            la = ti - tile0
                            pd = 4 + 2 * (itd % 2)
                            itd += 1
                            for hf in range(2):
                                for fc in range(2):
                                    mm(psum[pd + hf][:, :], hid[hs][:, fc, k * 128:(k + 1) * 128], wd[s][:, fc, hf * 512:(hf + 1) * 512],
                                       fc == 0, fc == 1, [b_hid[hs], b_we[s]], [bps[pd + hf]])
                            for hf in range(2):
                                c0 = hf * 512
                                if e == 0:
                                    ts("dve", acc[:, la, c0:c0 + 512], psum[pd + hf][:, :], Gall[:, ti, e:e + 1], None, ALU.mult, None,
                                       [bps[pd + hf], b_G[ti]], [b_acc[la]])
                                else:
                                    stt("dve", acc[:, la, c0:c0 + 512], psum[pd + hf][:, :], Gall[:, ti, e:e + 1], acc[:, la, c0:c0 + 512],
                                        ALU.mult, ALU.add, [bps[pd + hf], b_G[ti], b_acc[la]], [b_acc[la]])
                for la in range(ntile):
                    ti = tile0 + la
                    s = la % 2
                    w_ = 1 if ti < 2 else 0
                    if last and ti < 2:
                        continue
                    S.dma("sp", x6[s], xres[ti * 128:(ti + 1) * 128, :], [b_xres[ti]], [b_x6[s]], b_x6[s])
                    tt("pool", acc[:, la, :], acc[:, la, :], g2[w_], ALU.mult, [b_acc[la], b_m6], [b_acc[la]])
                    stt("dve", acc[:, la, :], x6[s], ALPHA, acc[:, la, :], ALU.mult, ALU.add, [b_x6[s], b_acc[la]], [b_acc[la]])
                    layer_norm("pool", x6[s], acc[:, la, :], l2g, l2b, [b_acc[la]], [b_x6[s]])
                    if last:
                        S.dma("sp", out[(ti - 2) * 128:(ti - 1) * 128, :], x6[s], [b_x6[s]], [b_out], b_x6[s])
                    else:
                        S.dma("sp", xcur[ti * 128:(ti + 1) * 128, :], x6[s], [b_x6[s]], [b_xcur[ti]], b_x6[s])
            S.barrier()
        S.barrier()
        S.emit()
        print("ops", {e: len(S.ops[e]) for e in ENGS}, "dsems", len(S.dsems))
    return nc


def _consts():
    ident = np.eye(128, dtype=np.float32)
    n_freq = 16
    inv = (10000.0 ** (-np.arange(n_freq, dtype=np.float32) / n_freq)).astype(np.float32)
    t = np.arange(4096)
    row = (t // 64).astype(np.float32)
    col = (t % 64).astype(np.float32)
    ang = np.concatenate([row[:, None] * inv, col[:, None] * inv], axis=-1).astype(np.float32)
    rope = np.zeros((T, 64), np.float32)
    rope[:256, 0:32] = 1.0
    rope[256:, 0:32] = np.cos(ang)
    rope[256:, 32:64] = np.sin(ang)
    j = np.arange(128, dtype=np.float32)[:, None]
    i = np.arange(128, dtype=np.float32)[None, :]
    relt = np.concatenate([(i - j) + 0 * j, (i >= j).astype(np.float32), (i <= j).astype(np.float32)], axis=1).astype(np.float32)
    posc = np.zeros((128, 258), np.float32)
    posc[:, 0:128] = i + 1.0
    posc[:, 128:256] = 128.0 - i
    posc[:, 256] = 127.0 - j[:, 0]
    posc[:, 257] = j[:, 0]
    return ident, rope, relt, posc


_NC_CACHE = {}


def make_in_maps(inputs, NLW=4):
    ident, rope, relt, posc = _consts()
    f0 = lambda a: np.ascontiguousarray(np.asarray(a, dtype=np.float32))
    f = lambda a: f0(np.asarray(a)[:NLW])
    shared = {
        "w_ada": f(inputs["w_ada"]), "b_ada": f(inputs["b_ada"]), "w_in": f(inputs["w_in"]),
        "ret_decay_logit": f(inputs["ret_decay_logit"]).reshape(NLW, 16),
        "att_q_norm": f(inputs["att_q_norm"]), "att_k_norm": f(inputs["att_k_norm"]),
        "conv_dw": f(inputs["conv_dw"]),
        "conv_vecs": np.ascontiguousarray(np.concatenate(
            [f(inputs["conv_db"]).reshape(NLW, 8, 128), f(inputs["conv_ln_g"]).reshape(NLW, 8, 128),
             f(inputs["conv_ln_b"]).reshape(NLW, 8, 128)], axis=1)),
        "w_ret_o": f(inputs["w_ret_o"]), "w_att_o": f(inputs["w_att_o"]), "w_conv_o": f(inputs["w_conv_o"]),
        "w_out": f(inputs["w_out"]), "ln1_g": f(inputs["ln1_g"]), "ln1_b": f(inputs["ln1_b"]),
        "w_router": f(inputs["w_router"]), "router_bias": f(inputs["router_bias"]),
        "w_exp_gate": f(inputs["w_exp_gate"]), "w_exp_up": f(inputs["w_exp_up"]), "w_exp_down": f(inputs["w_exp_down"]),
        "w_sh_gate": f(inputs["w_sh_gate"]), "w_sh_up": f(inputs["w_sh_up"]), "w_sh_down": f(inputs["w_sh_down"]),
        "ln2_g": f(inputs["ln2_g"]), "ln2_b": f(inputs["ln2_b"]),
        "c_ident": ident, "c_rope": rope, "c_relt": relt, "c_posc": posc,
    }
    x = f0(inputs["x"])
    ctx = f0(inputs["ctx"])
    c = f0(inputs["c"])
    cc = f0(inputs["c_ctx"])
    maps = []
    for b in range(8):
        cv = np.zeros((128, 8, 2), np.float32)
        cv[:, :, 0] = c[b].reshape(8, 128).T
        cv[:, :, 1] = cc.reshape(8, 128).T
        m = dict(shared)
        m["xb"] = x[b]
        m["ctxb"] = ctx[b]
        m["cvec"] = np.ascontiguousarray(cv.reshape(128, 16))
        maps.append(m)
    return maps


def kernel(**inputs):
    if "nc" not in _NC_CACHE:
        _NC_CACHE["nc"] = build(4)
    nc = _NC_CACHE["nc"]
    maps = make_in_maps(inputs)
    res = run_bass_kernel_spmd(nc, maps, core_ids=list(range(8)))
    return np.stack([np.asarray(r["out"], dtype=np.float32) for r in res.results], axis=0)
```

```python
import numpy as np
from contextlib import ExitStack
import concourse.bass as bass
import concourse.mybir as mybir
from concourse.bass_utils import run_bass_kernel_spmd

F32 = mybir.dt.float32
BF16 = mybir.dt.bfloat16
AF = mybir.ActivationFunctionType
ALU = mybir.AluOpType
AX = mybir.AxisListType

D = 1024
NT = 34
T = NT * 128
DIN = 9728
ZC = 7680
EPS = 1e-6
ALPHA = 8.0 ** 0.25
NE = 64
ENGS = ("pe", "act", "dve", "pool", "sp")


class Buf:
    __slots__ = ("name", "w", "r", "sem", "semcnt")

    def __init__(self, name=""):
        self.name = name
        self.w = {}
        self.r = {}
        self.sem = None
        self.semcnt = 0


class Op:
    __slots__ = ("eng", "fn", "deps", "idx", "sig", "dma", "sigval")

    def __init__(self, eng, fn, deps, idx, dma=None):
        self.eng = eng
        self.fn = fn
        self.deps = deps
        self.idx = idx
        self.sig = False
        self.dma = dma
        self.sigval = 0


class Sched:
    def __init__(self, nc, es):
        self.nc = nc
        self.es = es
        self.ops = {e: [] for e in ENGS}
        self.esem = {e: es.enter_context(nc.semaphore("es_" + e)) for e in ENGS}
        self.dsems = []
        self.dcnt = []
        self.free = []
        self.allbufs = []

    def buf(self, name=""):
        b = Buf(name)
        self.allbufs.append(b)
        return b

    def _collect(self, eng, reads, writes):
        deps = {}
        for b in reads:
            for k, v in b.w.items():
                if deps.get(k, -1) < v:
                    deps[k] = v
        for b in writes:
            for k, v in b.w.items():
                if deps.get(k, -1) < v:
                    deps[k] = v
            for k, v in b.r.items():
                if deps.get(k, -1) < v:
                    deps[k] = v
        out = []
        for k, v in deps.items():
            if k[0] == "e":
                if k[1] == eng and eng in ("pe", "sp"):
                    continue
                out.append(("e", k[1], v))
            else:
                out.append(("d", k[1], v))
        return out

    def _commit(self, key, val, reads, writes):
        for b in reads:
            if b.r.get(key, -1) < val:
                b.r[key] = val
        for b in writes:
            b.w = {key: val}
            b.r = {}

    def op(self, eng, fn, reads=(), writes=()):
        deps = self._collect(eng, reads, writes)
        idx = len(self.ops[eng])
        o = Op(eng, fn, deps, idx)
        self.ops[eng].append(o)
        self._commit(("e", eng), idx, reads, writes)
        return o

    def _getsem(self, buf):
        if buf.sem is None:
            if self.free:
                buf.sem = self.free.pop()
            else:
                buf.sem = len(self.dsems)
                self.dsems.append(self.es.enter_context(self.nc.semaphore("ds%d" % buf.sem)))
                self.dcnt.append(0)
        return buf.sem

    def dma(self, eng, out_ap, in_ap, reads, writes, sembuf):
        deps = self._collect(eng, reads, writes)
        si = self._getsem(sembuf)
        self.dcnt[si] += 16
        val = self.dcnt[si]
        idx = len(self.ops[eng])
        o = Op(eng, (out_ap, in_ap), deps, idx, dma=si)
        o.sigval = val
        self.ops[eng].append(o)
        self._commit(("d", si), val, reads, writes)
        return o

    def barrier(self):
        bb = Buf("bar")
        deps = {}
        for b in self.allbufs:
            for dct in (b.w, b.r):
                for k, v in dct.items():
                    if deps.get(k, -1) < v:
                        deps[k] = v
        dl = []
        for k, v in deps.items():
            if k[0] == "e":
                if k[1] != "sp":
                    dl.append(("e", k[1], v))
            else:
                dl.append(("d", k[1], v))
        idx = len(self.ops["sp"])
        o = Op("sp", lambda e: e.nop(), dl, idx)
        self.ops["sp"].append(o)
        bb.w = {("e", "sp"): idx}
        for e in ENGS:
            if e != "sp":
                self.op(e, None, [bb], [])
        for b in self.allbufs:
            b.w = {}
            b.r = {}
            b.sem = None
        self.free = list(range(len(self.dsems)))

    def emit(self):
        for e in ENGS:
            for o in self.ops[e]:
                for d in o.deps:
                    if d[0] == "e":
                        self.ops[d[1]][d[2]].sig = True
        for e in ENGS:
            c = 0
            for o in self.ops[e]:
                if o.dma is None and o.sig:
                    c += 1
                    o.sigval = c
        with self.nc.Block() as block:
            deco = {"pe": block.tensor, "act": block.scalar, "dve": block.vector,
                    "pool": block.gpsimd, "sp": block.sync}
            for e in ENGS:
                ops = self.ops[e]
                if not ops:
                    continue

                def body(engine, ops=ops, e=e):
                    waited = {}
                    for o in ops:
                        for d in o.deps:
                            if d[0] == "e":
                                key = ("e", d[1])
                                val = self.ops[d[1]][d[2]].sigval
                                sem = self.esem[d[1]]
                            else:
                                key = ("d", d[1])
                                val = d[2]
                                sem = self.dsems[d[1]]
                            if waited.get(key, 0) < val:
                                engine.wait_ge(sem, val)
                                waited[key] = val
                        if o.fn is None:
                            continue
                        if o.dma is not None:
                            if callable(o.fn):
                                o.fn(engine).then_inc(self.dsems[o.dma], 1)
                            else:
                                engine.dma_start(out=o.fn[0], in_=o.fn[1]).then_inc(self.dsems[o.dma], 16)
                        else:
                            ins = o.fn(engine)
                            if o.sig:
                                ins.then_inc(self.esem[e], 1)
                deco[e](body)


def fv(ap, pairs):
    return bass.AP(ap.tensor, ap.offset, [list(ap.ap[0])] + [list(p) for p in pairs])


def pbc(ap, nparts):
    return bass.AP(ap.tensor, ap.offset, [[0, nparts]] + [list(p) for p in ap.ap[1:]])


def build(L=4, dbg=None, NLW=4):
    nc = bass.Bass("TRN2", target_bir_lowering=False)
    dt_in = lambda name, shape: nc.dram_tensor(name, shape, F32, kind="ExternalInput").ap()
    xb_in = dt_in("xb", [4096, D])
    ctxb_in = dt_in("ctxb", [256, D])
    cvec_in = dt_in("cvec", [128, 16])
    w_ada = dt_in("w_ada", [NLW, D, 6 * D])
    b_ada = dt_in("b_ada", [NLW, 6 * D])
    w_in = dt_in("w_in", [NLW, D, DIN])
    dlog = dt_in("ret_decay_logit", [NLW, 16])
    qng = dt_in("att_q_norm", [NLW, 64])
    kng = dt_in("att_k_norm", [NLW, 64])
    conv_dw = dt_in("conv_dw", [NLW, 31, D])
    cvp = dt_in("conv_vecs", [NLW, 24, 128])
    w_ret_o = dt_in("w_ret_o", [NLW, D, D])
    w_att_o = dt_in("w_att_o", [NLW, D, D])
    w_conv_o = dt_in("w_conv_o", [NLW, D, D])
    w_out = dt_in("w_out", [NLW, D, D])
    ln1_g = dt_in("ln1_g", [NLW, D])
    ln1_b = dt_in("ln1_b", [NLW, D])
    w_router = dt_in("w_router", [NLW, D, NE])
    rbias = dt_in("router_bias", [NLW, NE])
    w_eg = dt_in("w_exp_gate", [NLW, NE, D, 256])
    w_eu = dt_in("w_exp_up", [NLW, NE, D, 256])
    w_ed = dt_in("w_exp_down", [NLW, NE, 256, D])
    w_sg = dt_in("w_sh_gate", [NLW, D, 256])
    w_su = dt_in("w_sh_up", [NLW, D, 256])
    w_sd = dt_in("w_sh_down", [NLW, 256, D])
    ln2_g = dt_in("ln2_g", [NLW, D])
    ln2_b = dt_in("ln2_b", [NLW, D])
    ident_in = dt_in("c_ident", [128, 128])
    rope_in = dt_in("c_rope", [T, 64])
    relt_in = dt_in("c_relt", [128, 384])
    posc_in = dt_in("c_posc", [128, 258])
    out = nc.dram_tensor("out", [4096, D], F32, kind="ExternalOutput").ap()

    def scratch(name, shape, dt):
        if dbg and name in dbg:
            return nc.dram_tensor(name, shape, dt, kind="ExternalOutput").ap()
        return nc.dram_tensor(name, shape, dt, kind="Internal").ap()
    modv = scratch("modv", [4, 2, 6 * D], F32)
    z_tok = scratch("z_tok", [T, ZC], BF16)
    gluT = scratch("gluT", [8, 128, T], BF16)
    retT = scratch("retT", [128, 8, T], BF16)
    attT = scratch("attT", [128, 8, T], BF16)
    convT = scratch("convT", [128, 8, T], BF16)
    mrgT = scratch("mrgT", [128, 8, T], BF16)
    xres = scratch("xres", [T, D], F32)
    xcur = scratch("xcur", [T, D], F32)

    with ExitStack() as es:
        S = Sched(nc, es)
        ARENA_W = 50000
        arena = es.enter_context(nc.sbuf_tensor("arena", [128, ARENA_W], F32))
        psum = [es.enter_context(nc.psum_tensor("ps%d" % i, [128, 512], F32)) for i in range(8)]
        bps = [S.buf("ps%d" % i) for i in range(8)]
        st = {"off": 0}

        def carve(shape, dt, nparts=128):
            n = int(np.prod(shape))
            words = (n * (4 if dt == F32 else 2) + 3) // 4
            words = (words + 7) // 8 * 8
            o = st["off"]
            assert o + words <= ARENA_W, ("arena overflow", o, words)
            st["off"] = o + words
            a = arena[0:nparts, o:o + words]
            if dt != F32:
                a = a.bitcast(dt)
            a = a[:, 0:n]
            if len(shape) == 2:
                a = a.rearrange("p (a b) -> p a b", a=shape[0])
            elif len(shape) == 3:
                a = a.rearrange("p (a b c) -> p a b c", a=shape[0], b=shape[1])
            return a

        def psb(i, n=1024):
            return psum[i].bitcast(BF16)[:, 0:n]

        def mm(o, lhsT, rhs, start, stop, reads, writes):
            S.op("pe", lambda e: e.matmul(o, lhsT, rhs, start=start, stop=stop), reads, writes)

        def tr(o, i, idn, reads, writes):
            S.op("pe", lambda e: e.transpose(o, i, idn), reads, writes)

        def act(o, i, func, reads, writes, bias=None, scale=None):
            kw = {}
            if bias is not None:
                kw["bias"] = bias
            if scale is not None:
                kw["scale"] = scale
            S.op("act", lambda e: e.activation(o, i, func, **kw), reads, writes)

        def tt(eng, o, a, b, op, reads, writes):
            S.op(eng, lambda e: e.tensor_tensor(o, a, b, op), reads, writes)

        def ts(eng, o, a, s1, s2, op0, op1, reads, writes):
            if s2 is None:
                S.op(eng, lambda e: e.tensor_scalar(o, a, s1, None, op0), reads, writes)
            else:
                S.op(eng, lambda e: e.tensor_scalar(o, a, s1, s2, op0, op1), reads, writes)

        def stt(eng, o, a, s, b, op0, op1, reads, writes):
            S.op(eng, lambda e: e.scalar_tensor_tensor(o, a, s, b, op0, op1), reads, writes)

        def cp(eng, o, i, reads, writes):
            if eng == "act":
                S.op("act", lambda e: e.copy(o, i), reads, writes)
            else:
                S.op(eng, lambda e: e.tensor_copy(o, i), reads, writes)

        def rsum(eng, o, i, reads, writes):
            S.op(eng, lambda e: e.reduce_sum(o, i, AX.X), reads, writes)

        def mset(eng, o, v, writes):
            S.op(eng, lambda e: e.memset(o, v), [], writes)

        def rstd_from(eng, o, i, scale, reads, writes):
            ts(eng, o, i, scale, EPS, ALU.mult, ALU.add, reads, writes)
            act(o, o, AF.Sqrt, writes, writes)
            S.op("dve", lambda e: e.reciprocal(o, o), writes, writes)

        def dump(name, ap, rbufs):
            if not (dbg and dbg.get("dump")):
                return
            shp = list(ap.shape)
            dtn = nc.dram_tensor("dmp_" + name, shp, ap.dtype, kind="ExternalOutput").ap()
            bb = S.buf()
            S.dma("sp", dtn, ap, list(rbufs), [bb], bb)

        identf = carve([128], F32)
        identb = carve([128], BF16)
        b_id = S.buf("ident")
        S.dma("sp", identf, ident_in, [], [b_id], b_id)
        cp("dve", identb, identf, [b_id], [b_id])
        onesf = carve([128], F32)
        mset("dve", onesf, 1.0, [b_id])
        onesm = carve([128], F32)
        mset("dve", onesm, 1.0 / 1024.0, [b_id])
        PERSIST = st["off"]

        def dram_bufs(n):
            return [S.buf() for _ in range(n)]
        b_modv = S.buf("modv")
        bz = {}

        def zb(i, name):
            k = (i, name)
            if k not in bz:
                bz[k] = S.buf()
            return bz[k]
        b_xcur = dram_bufs(NT)
        b_xres = dram_bufs(NT)
        b_out = S.buf("out")

        st["off"] = PERSIST
        cT = carve([16], F32)
        sT = carve([8, 2], F32)
        b_c = S.buf()
        S.dma("sp", cT, cvec_in, [], [b_c], b_c)
        act(sT.rearrange("p a b -> p (a b)"), cT, AF.Silu, [b_c], [b_c])
        wa = [carve([8, 512], F32) for _ in range(2)]
        b_wa = [S.buf() for _ in range(2)]
        bt = carve([6 * D], F32, nparts=2)
        modrow = carve([6 * D], F32, nparts=2)
        b_bt = S.buf()
        b_mr = S.buf()
        it = 0
        for l in range(L):
            S.dma("sp", bt, pbc(b_ada[l:l + 1, :], 2), [], [b_bt], b_bt)
            for n in range(12):
                s = it % 2
                it += 1
                S.dma("sp", wa[s], w_ada[l, :, n * 512:(n + 1) * 512].rearrange("(kc p) n -> p kc n", p=128),
                      [], [b_wa[s]], b_wa[s])
                for kc in range(8):
                    mm(psum[s][0:2, :], sT[:, kc, :], wa[s][:, kc, :], kc == 0, kc == 7, [b_c, b_wa[s]], [bps[s]])
                tt("dve", modrow[:, n * 512:(n + 1) * 512], psum[s][0:2, :], bt[:, n * 512:(n + 1) * 512], ALU.add,
                   [bps[s], b_bt], [b_mr])
            S.dma("sp", modv[l], modrow, [b_mr], [b_modv], b_mr)
        S.barrier()

        def mod_bc(dst, l, which, part, b_dst, plus1=False):
            src = modv[l, which:which + 1, part * D:(part + 1) * D]
            S.dma("sp", dst, pbc(src, 128), [b_modv], [b_dst], b_dst)
            if plus1:
                ts("pool", dst, dst, 1.0, None, ALU.add, None, [b_dst], [b_dst])

        def vec_bc(dst, src_row, n, b_dst):
            S.dma("sp", dst, pbc(src_row, 128), [], [b_dst], b_dst)

        def rope(eng, dst1, dst2, src, H, rp, tmp, reads, writes):
            x1 = src[:, :, 0:32]
            x2 = src[:, :, 32:64]
            cs = fv(rp[:, 0:32], [[0, H], [1, 32]])
            sn = fv(rp[:, 32:64], [[0, H], [1, 32]])
            ta, tb_ = tmp
            tt(eng, ta, x1, cs, ALU.mult, reads, writes)
            tt(eng, tb_, x2, sn, ALU.mult, reads, writes)
            tt(eng, dst1, ta, tb_, ALU.subtract, reads, writes)
            tt(eng, ta, x1, sn, ALU.mult, reads, writes)
            tt(eng, tb_, x2, cs, ALU.mult, reads, writes)
            tt(eng, dst2, ta, tb_, ALU.add, reads, writes)

        for l in range(L):
            last = (l == L - 1)

            def xin(i):
                if l == 0:
                    return ctxb_in[i * 128:(i + 1) * 128, :] if i < 2 else xb_in[(i - 2) * 128:(i - 1) * 128, :]
                return xcur[i * 128:(i + 1) * 128, :]

            st["off"] = PERSIST
            hT = carve([8, T], BF16)
            b_hT = [S.buf() for _ in range(NT)]
            sc1 = [carve([D], F32) for _ in range(2)]
            sh1 = [carve([D], F32) for _ in range(2)]
            b_m1 = S.buf()
            for w_ in range(2):
                mod_bc(sc1[w_], l, w_, 1, b_m1, plus1=True)
                mod_bc(sh1[w_], l, w_, 0, b_m1)
            xt = [carve([D], F32) for _ in range(2)]
            hb = [carve([D], BF16) for _ in range(2)]
            b_xt = [S.buf() for _ in range(2)]
            b_hb = [S.buf() for _ in range(2)]
            for i in range(NT):
                s = i % 2
                w_ = 1 if i < 2 else 0
                S.dma("sp", xt[s], xin(i), [b_xcur[i]], [b_xt[s]], b_xt[s])
                tt("dve", xt[s], xt[s], sc1[w_], ALU.mult, [b_xt[s], b_m1], [b_xt[s]])
                tt("pool", hb[s], xt[s], sh1[w_], ALU.add, [b_xt[s], b_m1], [b_hb[s]])
                for kc in range(8):
                    tr(psb(s)[:, kc * 128:(kc + 1) * 128], hb[s][:, kc * 128:(kc + 1) * 128], identb, [b_hb[s], b_id], [bps[s]])
                cp("act", hT[:, :, i * 128:(i + 1) * 128], psb(s).rearrange("p (a b) -> p a b", a=8), [bps[s]], [b_hT[i]])
            groups = []
            for k in range(2):
                groups.append((k * 512, k * 512, AF.Copy, ("rq", "rk")[k]))
            for k in range(2):
                groups.append((1024 + k * 512, 1024 + k * 512, AF.Copy, "rv%d" % k))
            for k in range(2):
                groups.append((2048 + k * 512, 2048 + k * 512, AF.Silu, "rg%d" % k))
            for k in range(2):
                groups.append((3072 + k * 512, 3072 + k * 512, AF.Copy, "aq%d" % k))
            groups.append((4096, 4096, AF.Copy, "kv"))
            for k in range(6):
                groups.append((6656 + k * 512, 4608 + k * 512, AF.Sigmoid, "sg%d" % k))
            wsl = [carve([8, 512], BF16) for _ in range(2)]
            b_wsl = [S.buf() for _ in range(2)]
            zt = [carve([512], BF16) for _ in range(3)]
            b_zt = [S.buf() for _ in range(3)]
            it = 0
            for gi, (wc0, zc0, func, nm) in enumerate(groups):
                s = gi % 2
                S.dma("pool", wsl[s], w_in[l, :, wc0:wc0 + 512].rearrange("(kc p) n -> p kc n", p=128),
                      [], [b_wsl[s]], b_wsl[s])
                for i in range(NT):
                    p = 2 + (it % 2)
                    q = it % 3
                    it += 1
                    for kc in range(8):
                        mm(psum[p][:, :], hT[:, kc, i * 128:(i + 1) * 128], wsl[s][:, kc, :], kc == 0, kc == 7,
                           [b_hT[i], b_wsl[s]], [bps[p]])
                    if func == AF.Copy:
                        cp("dve", zt[q], psum[p][:, :], [bps[p]], [b_zt[q]])
                    else:
                        act(zt[q], psum[p][:, :], func, [bps[p]], [b_zt[q]])
                    S.dma("sp", z_tok[i * 128:(i + 1) * 128, zc0:zc0 + 512], zt[q], [b_zt[q]], [zb(i, nm)], b_zt[q])
            wcs = [carve([8, 256], BF16) for _ in range(2)]
            b_wcs = [S.buf() for _ in range(2)]
            sgm = [carve([512], F32) for _ in range(2)]
            b_sgm = [S.buf() for _ in range(2)]
            glt = [carve([512], BF16) for _ in range(2)]
            b_glt = [S.buf() for _ in range(2)]
            b_glu = [S.buf() for _ in range(8)]
            it = 0
            for cc in range(8):
                s = cc % 2
                S.dma("pool", wcs[s][:, :, 0:128],
                      w_in[l, :, 4608 + cc * 128:4608 + (cc + 1) * 128].rearrange("(kc p) n -> p kc n", p=128),
                      [], [b_wcs[s]], b_wcs[s])
                S.dma("pool", wcs[s][:, :, 128:256],
                      w_in[l, :, 5632 + cc * 128:5632 + (cc + 1) * 128].rearrange("(kc p) n -> p kc n", p=128),
                      [], [b_wcs[s]], b_wcs[s])
                for tb in range(9):
                    t0 = tb * 512
                    n = min(512, T - t0)
                    tiles = range(t0 // 128, (t0 + n) // 128)
                    q = it % 2
                    it += 1
                    pa, pg = 4 + 2 * q, 5 + 2 * q
                    for kc in range(8):
                        mm(psum[pa][:, 0:n], wcs[s][:, kc, 0:128], hT[:, kc, t0:t0 + n], kc == 0, kc == 7,
                           [b_hT[i] for i in tiles] + [b_wcs[s]], [bps[pa]])
                    for kc in range(8):
                        mm(psum[pg][:, 0:n], wcs[s][:, kc, 128:256], hT[:, kc, t0:t0 + n], kc == 0, kc == 7,
                           [b_hT[i] for i in tiles] + [b_wcs[s]], [bps[pg]])
                    act(sgm[q][:, 0:n], psum[pg][:, 0:n], AF.Sigmoid, [bps[pg]], [b_sgm[q]])
                    tt("dve", glt[q][:, 0:n], psum[pa][:, 0:n], sgm[q][:, 0:n], ALU.mult, [bps[pa], b_sgm[q]], [b_glt[q]])
                    S.dma("sp", gluT[cc, :, t0:t0 + n], glt[q][:, 0:n], [b_glt[q]], [b_glu[cc]], b_glt[q])
            S.barrier()
            if dbg and dbg.get("stop") == "P1":
                break

            st["off"] = PERSIST
            kT2 = carve([4, T], BF16)
            b_kT = [S.buf() for _ in range(NT)]
            vext = carve([NT, 4, 192], BF16)
            b_v = [S.buf() for _ in range(NT)]
            b_vinit = S.buf()
            mset("pool", vext.rearrange("p a b c -> p (a b c)"), 0.0, [b_vinit])
            mset("pool", vext[:, :, :, 64:65], 1.0, [b_vinit])
            qg = carve([64], F32)
            kg = carve([64], F32)
            b_ng = S.buf()
            vec_bc(qg, qng[l:l + 1, :], 64, b_ng)
            vec_bc(kg, kng[l:l + 1, :], 64, b_ng)
            kvt = [carve([512], BF16) for _ in range(2)]
            b_kvt = [S.buf() for _ in range(2)]
            rp = [carve([64], F32) for _ in range(2)]
            b_rp = [S.buf() for _ in range(2)]
            sq = carve([D], F32)
            ss = carve([16], F32)
            qn = carve([16, 64], F32)
            tA = carve([16, 32], F32)
            tB = carve([16, 32], F32)
            kdup = carve([4, 2, 64], BF16)
            qb_ = carve([16, 64], BF16)
            b_tmp = S.buf()
            b_kd = S.buf()
            b_qb = S.buf()

            def normrope(src, H, gain, rpt, d1, d2, reads, wbuf):
                sv = sq[:, 0:H * 64].rearrange("p (h d) -> p h d", h=H)
                tt("dve", sv, src, src, ALU.mult, reads, [b_tmp])
                rsum("dve", ss[:, 0:H], sv, [b_tmp], [b_tmp])
                rstd_from("dve", ss[:, 0:H], ss[:, 0:H], 1.0 / 64.0, [b_tmp], [b_tmp])
                qv = qn[:, 0:H, :]
                tt("dve", qv, src, fv(ss[:, 0:H], [[1, H], [0, 64]]), ALU.mult, reads + [b_tmp], [b_tmp])
                tt("dve", qv, qv, fv(gain, [[0, H], [1, 64]]), ALU.mult, [b_tmp, b_ng], [b_tmp])
                rope("dve", d1, d2, qv, H, rpt, (tA[:, 0:H, :], tB[:, 0:H, :]), reads + [b_tmp], [b_tmp, wbuf])

            for i in range(NT):
                s = i % 2
                S.dma("sp", kvt[s], z_tok[i * 128:(i + 1) * 128, 4096:4608], [zb(i, "kv")], [b_kvt[s]], b_kvt[s])
                S.dma("sp", rp[s], rope_in[i * 128:(i + 1) * 128, :], [], [b_rp[s]], b_rp[s])
                ksrc = kvt[s][:, 0:256].rearrange("p (h d) -> p h d", h=4)
                normrope(ksrc, 4, kg, rp[s], kdup[:, :, 0, 0:32], kdup[:, :, 0, 32:64], [b_kvt[s], b_rp[s]], b_kd)
                cp("dve", kdup[:, :, 1, :], kdup[:, :, 0, :], [b_kd], [b_kd])
                for g in range(4):
                    tr(psb(7)[:, g * 128:(g + 1) * 128], kdup[:, g, :, :].rearrange("p a b -> p (a b)"), identb,
                       [b_kd, b_id], [bps[7]])
                cp("act", kT2[:, :, i * 128:(i + 1) * 128], psb(7, 512).rearrange("p (a b) -> p a b", a=4), [bps[7]], [b_kT[i]])
                vsrc = kvt[s][:, 256:512].rearrange("p (h d) -> p h d", h=4)
                cp("pool", vext[:, i, :, 0:64], vsrc, [b_kvt[s], b_vinit], [b_v[i]])
                cp("pool", vext[:, i, :, 128:192], vsrc, [b_kvt[s], b_vinit], [b_v[i]])
            aqt = [carve([D], BF16) for _ in range(2)]
            b_aqt = [S.buf() for _ in range(2)]
            qT = carve([8, 512], BF16)
            b_qT = S.buf()
            pT = [carve([512], BF16) for _ in range(3)]
            b_pT = [S.buf() for _ in range(3)]
            rden = carve([512], F32)
            b_rden = S.buf()
            osb = [carve([512], F32) for _ in range(2)]
            b_osb = [S.buf() for _ in range(2)]
            ablk = [carve([8, 512], BF16) for _ in range(2)]
            b_ablk = [S.buf() for _ in range(2)]
            b_att = [S.buf() for _ in range(9)]
            qblocks = [(0, 256, range(0, 2))] + [(256 + 512 * j, 512, range(NT)) for j in range(8)]
            its = 0
            ith = 0
            for bi, (t0, nq, kchunks) in enumerate(qblocks):
                ab = ablk[bi % 2]
                b_ab = b_ablk[bi % 2]
                for tq in range(nq // 128):
                    i = t0 // 128 + tq
                    s = tq % 2
                    S.dma("sp", aqt[s], z_tok[i * 128:(i + 1) * 128, 3072:4096],
                          [zb(i, "aq0"), zb(i, "aq1")], [b_aqt[s]], b_aqt[s])
                    S.dma("sp", rp[s], rope_in[i * 128:(i + 1) * 128, :], [], [b_rp[s]], b_rp[s])
                    qsrc = aqt[s].rearrange("p (h d) -> p h d", h=16)
                    normrope(qsrc, 16, qg, rp[s], qb_[:, :, 0:32], qb_[:, :, 32:64], [b_aqt[s], b_rp[s]], b_qb)
                    for hp in range(8):
                        tr(psb(7)[:, hp * 128:(hp + 1) * 128], qb_[:, 2 * hp:2 * hp + 2, :].rearrange("p a b -> p (a b)"),
                           identb, [b_qb, b_id], [bps[7]])
                    cp("dve", qT[:, :, tq * 128:(tq + 1) * 128], psb(7).rearrange("p (a b) -> p a b", a=8), [bps[7]], [b_qT])
                kl = list(kchunks)
                items = [(h, ci, c) for h in range(16) for ci, c in enumerate(kl)]
                LOOK = 2

                def issue_S(idx):
                    h, ci, c = items[idx]
                    g, hp, r = h // 4, h // 2, h % 2
                    pr = slice(r * 64, (r + 1) * 64)
                    p = (its0 + idx) % 3
                    mm(psum[p][:, 0:nq], kT2[pr, g, c * 128:(c + 1) * 128], qT[pr, hp, 0:nq], True, True,
                       [b_kT[c], b_qT], [bps[p]])

                def epilogue(h):
                    hp, r = h // 2, h % 2
                    pr = slice(r * 64, (r + 1) * 64)
                    po = 3 + (h % 2)
                    ob = h % 2
                    dr = 64 if r == 0 else 0
                    S.op("dve", lambda e, po=po, dr=dr, nq=nq: e.reciprocal(rden[dr:dr + 1, 0:nq], psum[po][dr:dr + 1, 0:nq]),
                         [bps[po]], [b_rden])
                    mm(psum[5][:, 0:nq], onesf[dr:dr + 1, :], rden[dr:dr + 1, 0:nq], True, True, [b_rden, b_id], [bps[5]])
                    cp("dve", osb[ob][pr, 0:nq], psum[po][pr, 0:nq], [bps[po]], [b_osb[ob]])
                    tt("dve", ab[pr, hp, 0:nq], osb[ob][pr, 0:nq], psum[5][pr, 0:nq], ALU.mult, [b_osb[ob], bps[5]], [b_ab])

                its0 = its
                for idx in range(min(LOOK, len(items))):
                    issue_S(idx)
                pending = None
                for idx, (h, ci, c) in enumerate(items):
                    g, r = h // 4, h % 2
                    if idx + LOOK < len(items):
                        issue_S(idx + LOOK)
                    p = (its0 + idx) % 3
                    po = 3 + (h % 2)
                    act(pT[p][:, 0:nq], psum[p][:, 0:nq], AF.Exp, [bps[p]], [b_pT[p]], scale=0.125)
                    if r == 0:
                        mm(psum[po][0:65, 0:nq], vext[:, c, g, 0:65], pT[p][:, 0:nq], ci == 0, ci == len(kl) - 1,
                           [b_v[c], b_pT[p]], [bps[po]])
                    else:
                        mm(psum[po][:, 0:nq], vext[:, c, g, 64:192], pT[p][:, 0:nq], ci == 0, ci == len(kl) - 1,
                           [b_v[c], b_pT[p]], [bps[po]])
                    if ci == 1 and pending is not None:
                        epilogue(pending)
                        pending = None
                    if ci == len(kl) - 1:
                        if pending is not None:
                            epilogue(pending)
                        pending = h
                if pending is not None:
                    epilogue(pending)
                its += len(items)
                S.dma("sp", attT[:, :, t0:t0 + nq], ab[:, :, 0:nq], [b_ab], [b_att[bi]], b_ab)
            dump("kT2", kT2, b_kT)
            dump("vext", vext.rearrange("p a b c -> p (a b c)"), b_v)
            dump("qT", qT, [b_qT])
            dump("osb0", osb[0], [b_osb[0]])
            dump("osb1", osb[1], [b_osb[1]])
            dump("rden", rden, [b_rden])
            dump("pT0", pT[0], [b_pT[0]])
            dump("qn", qn, [b_tmp])
            dump("ss", ss, [b_tmp])
            S.barrier()
            if dbg and dbg.get("stop") == "P2":
                break

            st["off"] = PERSIST
            lgb = carve([16], F32)
            b_lg = S.buf()
            S.dma("sp", lgb, pbc(dlog[l:l + 1, :], 128), [], [b_lg], b_lg)
            act(lgb, lgb, AF.Exp, [b_lg], [b_lg], scale=-1.0)
            ts("dve", lgb, lgb, 1.0, None, ALU.add, None, [b_lg], [b_lg])
            act(lgb, lgb, AF.Ln, [b_lg], [b_lg])
            ts("dve", lgb, lgb, -1.0, None, ALU.mult, None, [b_lg], [b_lg])
            relt = carve([384], F32)
            posc = carve([258], F32)
            b_cst = S.buf()
            S.dma("sp", relt, relt_in, [], [b_cst], b_cst)
            S.dma("sp", posc, posc_in, [], [b_cst], b_cst)
            Dtot = carve([8, 128], F32)
            Dtmp = carve([8, 128], F32)
            b_tab = S.buf()
            relv = fv(relt[:, 0:128], [[0, 8], [1, 128]])
            tt("dve", Dtot, relv, fv(lgb[:, 0:8], [[1, 8], [0, 128]]), ALU.mult, [b_cst, b_lg], [b_tab])
            act(Dtot, Dtot, AF.Exp, [b_tab], [b_tab])
            tt("dve", Dtot, Dtot, fv(relt[:, 128:256], [[0, 8], [1, 128]]), ALU.mult, [b_tab, b_cst], [b_tab])
            tt("dve", Dtmp, relv, fv(lgb[:, 8:16], [[1, 8], [0, 128]]), ALU.mult, [b_cst, b_lg], [b_tab])
            act(Dtmp, Dtmp, AF.Exp, [b_tab], [b_tab], scale=-1.0)
            tt("dve", Dtmp, Dtmp, fv(relt[:, 256:384], [[0, 8], [1, 128]]), ALU.mult, [b_tab, b_cst], [b_tab])
            tt("dve", Dtot, Dtot, Dtmp, ALU.add, [b_tab], [b_tab])
            ts("dve", Dtot, Dtot, 0.125, None, ALU.mult, None, [b_tab], [b_tab])
            qwf = carve([8, 128], F32)
            qwb = carve([8, 128], F32)
            tt("dve", qwf, fv(posc[:, 0:128], [[0, 8], [1, 128]]), fv(lgb[:, 0:8], [[1, 8], [0, 128]]), ALU.mult, [b_cst, b_lg], [b_tab])
            act(qwf, qwf, AF.Exp, [b_tab], [b_tab])
            tt("dve", qwb, fv(posc[:, 128:256], [[0, 8], [1, 128]]), fv(lgb[:, 8:16], [[1, 8], [0, 128]]), ALU.mult, [b_cst, b_lg], [b_tab])
            act(qwb, qwb, AF.Exp, [b_tab], [b_tab])
            kwf = carve([8], F32)
            kwb = carve([8], F32)
            ts("dve", kwf, lgb[:, 0:8], posc[:, 256:257], None, ALU.mult, None, [b_cst, b_lg], [b_tab])
            act(kwf, kwf, AF.Exp, [b_tab], [b_tab])
            ts("dve", kwf, kwf, 0.125, None, ALU.mult, None, [b_tab], [b_tab])
            ts("dve", kwb, lgb[:, 8:16], posc[:, 257:258], None, ALU.mult, None, [b_cst, b_lg], [b_tab])
            act(kwb, kwb, AF.Exp, [b_tab], [b_tab])
            ts("dve", kwb, kwb, 0.125, None, ALU.mult, None, [b_tab], [b_tab])
            Gf = carve([8, 128], F32)
            Gb = carve([8, 128], F32)
            ts("dve", Gf.rearrange("p a b -> p (a b)")[:, 0:8], lgb[:, 0:8], 128.0, None, ALU.mult, None, [b_lg], [b_tab])
            act(Gf.rearrange("p a b -> p (a b)")[:, 8:16], Gf.rearrange("p a b -> p (a b)")[:, 0:8], AF.Exp, [b_tab], [b_tab])
            ts("dve", Gb.rearrange("p a b -> p (a b)")[:, 0:8], lgb[:, 8:16], 128.0, None, ALU.mult, None, [b_lg], [b_tab])
            act(Gb.rearrange("p a b -> p (a b)")[:, 8:16], Gb.rearrange("p a b -> p (a b)")[:, 0:8], AF.Exp, [b_tab], [b_tab])
            gtmp = carve([16], F32)
            cp("dve", gtmp[:, 0:8], Gf.rearrange("p a b -> p (a b)")[:, 8:16], [b_tab], [b_tab])
            cp("dve", gtmp[:, 8:16], Gb.rearrange("p a b -> p (a b)")[:, 8:16], [b_tab], [b_tab])
            cp("dve", Gf, fv(gtmp[:, 0:8], [[1, 8], [0, 128]]), [b_tab], [b_tab])
            cp("dve", Gb, fv(gtmp[:, 8:16], [[1, 8], [0, 128]]), [b_tab], [b_tab])

            rp = [carve([64], F32) for _ in range(2)]
            b_rp = [S.buf() for _ in range(2)]
            Sprev = carve([NT, 8, 128], BF16)
            b_sp = [S.buf() for _ in range(NT)]
            Sst = carve([8, 128], F32)
            b_S = S.buf()
            rkt = [carve([512], BF16) for _ in range(2)]
            rvt = [carve([D], BF16) for _ in range(2)]
            rqt = [carve([512], BF16) for _ in range(2)]
            sgt = [carve([D], BF16) for _ in range(2)]
            b_ld = [S.buf() for _ in range(2)]
            kr = carve([8, 64], F32)
            kw = carve([8, 64], BF16)
            krb = carve([8, 64], BF16)
            qrb = carve([8, 64], BF16)
            tA = carve([8, 32], F32)
            tB = carve([8, 32], F32)
            b_k = S.buf()
            b_kw = S.buf()
            b_q = S.buf()
            mset("dve", Sst[0:64], 0.0, [b_S])
            order = [1, 0] + list(range(NT - 1, 1, -1))
            for oi, n in enumerate(order):
                s = oi % 2
                cp("act", Sprev[0:64, n], Sst[0:64], [b_S], [b_sp[n]])
                S.dma("sp", rkt[s], z_tok[n * 128:(n + 1) * 128, 512:1024], [zb(n, "rk")], [b_ld[s]], b_ld[s])
                S.dma("sp", rvt[s], z_tok[n * 128:(n + 1) * 128, 1024:2048], [zb(n, "rv0"), zb(n, "rv1")], [b_ld[s]], b_ld[s])
                S.dma("sp", rp[s], rope_in[n * 128:(n + 1) * 128, :], [], [b_rp[s]], b_rp[s])
                ksrc = rkt[s].rearrange("p (h d) -> p h d", h=8)
                rope("pool", kr[:, :, 0:32], kr[:, :, 32:64], ksrc, 8, rp[s], (tA, tB), [b_ld[s], b_rp[s]], [b_k])
                tt("pool", kw, kr, fv(kwb, [[1, 8], [0, 64]]), ALU.mult, [b_k, b_tab], [b_kw])
                for h in range(8):
                    pk = 0 + h // 4
                    mm(psum[pk][0:64, (h % 4) * 128:(h % 4 + 1) * 128], kw[:, h, :], rvt[s][:, h * 128:(h + 1) * 128], True, True,
                       [b_kw, b_ld[s]], [bps[pk]])
                tt("dve", Sst[0:64], Sst[0:64], Gb[0:64], ALU.mult, [b_S, b_tab], [b_S])
                for hh in range(2):
                    tt("dve", Sst[0:64, hh * 4:(hh + 1) * 4, :], Sst[0:64, hh * 4:(hh + 1) * 4, :],
                       psum[hh][0:64, :].rearrange("p (a b) -> p a b", a=4), ALU.add, [b_S, bps[hh]], [b_S])
            qTs = carve([8, 128], BF16)
            kTs = carve([8, 128], BF16)
            b_qk = S.buf()
            scT = carve([8, 128], BF16)
            b_sc = S.buf()
            qwfq = carve([8, 128], BF16)
            qwbq = carve([8, 128], BF16)
            b_qw = S.buf()
            Sfb = carve([8, 128], BF16)
            b_sfb = S.buf()
            ysb = carve([8, 128], F32)
            ysq = carve([8, 128], F32)
            mst = carve([32], F32)
            b_y = S.buf()
            rtn = carve([D], BF16)
            b_rtn = S.buf()
            rTt = [carve([8, 128], BF16) for _ in range(2)]
            b_rTt = [S.buf() for _ in range(2)]
            b_retT = [S.buf() for _ in range(NT)]
            mset("dve", Sst[0:64], 0.0, [b_S])
            for n in range(NT):
                s = n % 2
                S.dma("sp", rkt[s], z_tok[n * 128:(n + 1) * 128, 512:1024], [zb(n, "rk")], [b_ld[s]], b_ld[s])
                S.dma("sp", rvt[s], z_tok[n * 128:(n + 1) * 128, 1024:2048], [zb(n, "rv0"), zb(n, "rv1")], [b_ld[s]], b_ld[s])
                S.dma("sp", rqt[s], z_tok[n * 128:(n + 1) * 128, 0:512], [zb(n, "rq")], [b_ld[s]], b_ld[s])
                S.dma("sp", sgt[s], z_tok[n * 128:(n + 1) * 128, 2048:3072], [zb(n, "rg0"), zb(n, "rg1")], [b_ld[s]], b_ld[s])
                S.dma("sp", rp[s], rope_in[n * 128:(n + 1) * 128, :], [], [b_rp[s]], b_rp[s])
                ksrc = rkt[s].rearrange("p (h d) -> p h d", h=8)
                qsrc = rqt[s].rearrange("p (h d) -> p h d", h=8)
                rope("pool", kr[:, :, 0:32], kr[:, :, 32:64], ksrc, 8, rp[s], (tA, tB), [b_ld[s], b_rp[s]], [b_k])
                tt("pool", kw, kr, fv(kwf, [[1, 8], [0, 64]]), ALU.mult, [b_k, b_tab], [b_kw])
                cp("pool", krb, kr, [b_k], [b_kw])
                rope("pool", qrb[:, :, 0:32], qrb[:, :, 32:64], qsrc, 8, rp[s], (tA, tB), [b_ld[s], b_rp[s]], [b_q])
                for h in range(8):
                    tr(psb(6)[0:64, h * 128:(h + 1) * 128], qrb[:, h, :], identb, [b_q, b_id], [bps[6]])
                for h in range(8):
                    tr(psb(7)[0:64, h * 128:(h + 1) * 128], krb[:, h, :], identb, [b_kw, b_id], [bps[7]])
                cp("act", qTs[0:64], psb(6)[0:64].rearrange("p (a b) -> p a b", a=8), [bps[6]], [b_qk])
                cp("act", kTs[0:64], psb(7)[0:64].rearrange("p (a b) -> p a b", a=8), [bps[7]], [b_qk])
                for h in range(8):
                    pk = 0 + h // 4
                    mm(psum[pk][:, (h % 4) * 128:(h % 4 + 1) * 128], kTs[0:64, h, :], qTs[0:64, h, :], True, True,
                       [b_qk], [bps[pk]])
                for hh in range(2):
                    tt("dve", scT[:, hh * 4:(hh + 1) * 4, :], psum[hh][:, :].rearrange("p (a b) -> p a b", a=4),
                       Dtot[:, hh * 4:(hh + 1) * 4, :], ALU.mult, [bps[hh], b_tab], [b_sc])
                tt("pool", qwfq[0:64], qTs[0:64], qwf[0:64], ALU.mult, [b_qk, b_tab], [b_qw])
                tt("pool", qwbq[0:64], qTs[0:64], qwb[0:64], ALU.mult, [b_qk, b_tab], [b_qw])
                cp("act", Sfb[0:64], Sst[0:64], [b_S], [b_sfb])
                for h in range(8):
                    pk = 2 + h // 4
                    o_ = psum[pk][:, (h % 4) * 128:(h % 4 + 1) * 128]
                    mm(o_, scT[:, h, :], rvt[s][:, h * 128:(h + 1) * 128], True, False, [b_sc, b_ld[s]], [bps[pk]])
                    mm(o_, qwfq[0:64, h, :], Sfb[0:64, h, :], False, False, [b_qw, b_sfb], [bps[pk]])
                    mm(o_, qwbq[0:64, h, :], Sprev[0:64, n, h, :], False, True, [b_qw, b_sp[n]], [bps[pk]])
                for h in range(8):
                    pk = 4 + h // 4
                    mm(psum[pk][0:64, (h % 4) * 128:(h % 4 + 1) * 128], kw[:, h, :], rvt[s][:, h * 128:(h + 1) * 128], True, True,
                       [b_kw, b_ld[s]], [bps[pk]])
                tt("dve", Sst[0:64], Sst[0:64], Gf[0:64], ALU.mult, [b_S, b_tab, b_sfb], [b_S])
                for hh in range(2):
                    tt("dve", Sst[0:64, hh * 4:(hh + 1) * 4, :], Sst[0:64, hh * 4:(hh + 1) * 4, :],
                       psum[4 + hh][0:64, :].rearrange("p (a b) -> p a b", a=4), ALU.add, [b_S, bps[4 + hh]], [b_S])
                for hh in range(2):
                    cp("act", ysb[:, hh * 4:(hh + 1) * 4, :], psum[2 + hh][:, :].rearrange("p (a b) -> p a b", a=4), [bps[2 + hh]], [b_y])
                rsum("dve", mst[:, 0:8], ysb, [b_y], [b_y])
                tt("pool", ysq, ysb, ysb, ALU.mult, [b_y], [b_y])
                rsum("dve", mst[:, 8:16], ysq, [b_y], [b_y])
                ts("dve", mst[:, 0:8], mst[:, 0:8], 1.0 / 128.0, None, ALU.mult, None, [b_y], [b_y])
                tt("dve", mst[:, 16:24], mst[:, 0:8], mst[:, 0:8], ALU.mult, [b_y], [b_y])
                stt("dve", mst[:, 8:16], mst[:, 8:16], 1.0 / 128.0, mst[:, 16:24], ALU.mult, ALU.subtract, [b_y], [b_y])
                rstd_from("dve", mst[:, 8:16], mst[:, 8:16], 1.0, [b_y], [b_y])
                tt("dve", ysb, ysb, fv(mst[:, 0:8], [[1, 8], [0, 128]]), ALU.subtract, [b_y], [b_y])
                tt("pool", ysb, ysb, fv(mst[:, 8:16], [[1, 8], [0, 128]]), ALU.mult, [b_y], [b_y])
                tt("pool", rtn, ysb.rearrange("p a b -> p (a b)"), sgt[s], ALU.mult, [b_y, b_ld[s]], [b_rtn])
                for kc in range(8):
                    tr(psb(6)[:, kc * 128:(kc + 1) * 128], rtn[:, kc * 128:(kc + 1) * 128], identb, [b_rtn, b_id], [bps[6]])
                cp("act", rTt[s], psb(6).rearrange("p (a b) -> p a b", a=8), [bps[6]], [b_rTt[s]])
                S.dma("sp", retT[:, :, n * 128:(n + 1) * 128], rTt[s], [b_rTt[s]], [b_retT[n]], b_rTt[s])
            S.barrier()
            if dbg and dbg.get("stop") == "P3":
                break

            st["off"] = PERSIST
            GP = 286 + 30 + 4096
            gp = carve([8, GP], BF16)
            b_gp = S.buf()
            mset("pool", gp.rearrange("p a b -> p (a b)"), 0.0, [b_gp])
            for cc in range(8):
                S.dma("sp", gp[:, cc, 15:271], gluT[cc, :, 0:256], [b_glu[cc]], [b_gp], b_gp)
                S.dma("sp", gp[:, cc, 301:301 + 4096], gluT[cc, :, 256:T], [b_glu[cc]], [b_gp], b_gp)
            cw = carve([D], F32, nparts=31)
            b_cw = S.buf()
            S.dma("sp", cw, conv_dw[l], [], [b_cw], b_cw)
            wdT = carve([8, 32], F32)
            for cc in range(8):
                tr(psum[0][:, cc * 32:cc * 32 + 31], cw[0:31, cc * 128:(cc + 1) * 128], identf[0:31, 0:31], [b_cw, b_id], [bps[0]])
            cp("dve", wdT[:, :, 0:31], psum[0][:, 0:256].rearrange("p (a b) -> p a b", a=8)[:, :, 0:31], [bps[0]], [b_cw])
            cv = carve([128], F32, nparts=24)
            S.dma("sp", cv, cvp[l], [], [b_cw], b_cw)
            cvT = carve([24], F32)
            tr(psum[1][:, 0:24], cv[0:24, :], identf[0:24, 0:24], [b_cw, b_id], [bps[1]])
            cp("dve", cvT, psum[1][:, 0:24], [bps[1]], [b_cw])
            dgw = carve([8, 31, 128], BF16)
            b_dg = S.buf()
            for cc in range(8):
                tt("pool", dgw[:, cc], fv(identf, [[0, 31], [1, 128]]), fv(wdT[:, cc, 0:31], [[1, 31], [0, 128]]), ALU.mult,
                   [b_cw, b_id], [b_dg])
            yf = carve([8, 512], F32)
            ysq2 = carve([8, 512], F32)
            b_yf = S.buf()
            mean = carve([512], F32)
            rstd = carve([512], F32)
            b_st = S.buf()
            cblk = [carve([8, 512], BF16) for _ in range(2)]
            b_cblk = [S.buf() for _ in range(2)]
            b_convT = [S.buf() for _ in range(9)]
            tmpc = carve([512], F32)
            b_tc = S.buf()
            cblocks = [(0, 256, 0)] + [(256 + 512 * j, 512, 286 + 512 * j) for j in range(8)]
            it = 0
            for bi, (t0, n, g0) in enumerate(cblocks):
                cb = cblk[bi % 2]
                b_cb = b_cblk[bi % 2]
                for cc in range(8):
                    p = it % 2
                    it += 1
                    for k in range(31):
                        mm(psum[p][:, 0:n], dgw[:, cc, k, :], gp[:, cc, g0 + k:g0 + k + n], k == 0, k == 30,
                           [b_dg, b_gp], [bps[p]])
                    act(yf[:, cc, 0:n], psum[p][:, 0:n], AF.Identity, [bps[p], b_cw], [b_yf], bias=cvT[:, cc:cc + 1])
                    tt("pool", ysq2[:, cc, 0:n], yf[:, cc, 0:n], yf[:, cc, 0:n], ALU.mult, [b_yf], [b_yf])
                for cc in range(8):
                    mm(psum[2][:, 0:n], onesm, yf[:, cc, 0:n], cc == 0, cc == 7, [b_yf, b_id], [bps[2]])
                for cc in range(8):
                    mm(psum[3][:, 0:n], onesm, ysq2[:, cc, 0:n], cc == 0, cc == 7, [b_yf, b_id], [bps[3]])
                cp("act", mean[:, 0:n], psum[2][:, 0:n], [bps[2]], [b_st])
                tt("dve", rstd[:, 0:n], mean[:, 0:n], mean[:, 0:n], ALU.mult, [b_st], [b_st])
                tt("dve", rstd[:, 0:n], psum[3][:, 0:n], rstd[:, 0:n], ALU.subtract, [b_st, bps[3]], [b_st])
                rstd_from("dve", rstd[:, 0:n], rstd[:, 0:n], 1.0, [b_st], [b_st])
                for cc in range(8):
                    tt("dve", tmpc[:, 0:n], yf[:, cc, 0:n], mean[:, 0:n], ALU.subtract, [b_yf, b_st], [b_tc])
                    tt("pool", tmpc[:, 0:n], tmpc[:, 0:n], rstd[:, 0:n], ALU.mult, [b_tc, b_st], [b_tc])
                    act(cb[:, cc, 0:n], tmpc[:, 0:n], AF.Silu, [b_tc, b_cw], [b_cb],
                        bias=cvT[:, 16 + cc:17 + cc], scale=cvT[:, 8 + cc:9 + cc])
                S.dma("sp", convT[:, :, t0:t0 + n], cb[:, :, 0:n], [b_cb], [b_convT[bi]], b_cb)
            S.barrier()
            if dbg and dbg.get("stop") == "P4":
                break

            st["off"] = PERSIST
            wbr = [carve([8, D], BF16) for _ in range(3)]
            b_wbr = S.buf()
            for k, wsrc in enumerate((w_ret_o, w_att_o, w_conv_o)):
                S.dma("pool", wbr[k], wsrc[l].rearrange("(kc p) n -> p kc n", p=128), [], [b_wbr], b_wbr)
            brt = [[carve([8, 128], BF16) for _ in range(3)] for _ in range(2)]
            sgl = [carve([3 * D], BF16) for _ in range(2)]
            b_in5 = [S.buf() for _ in range(2)]
            m0 = carve([D], F32)
            m1 = carve([D], F32)
            mgb = carve([D], BF16)
            b_m = S.buf()
            b_mg = S.buf()
            mTt = [carve([8, 128], BF16) for _ in range(2)]
            b_mTt = [S.buf() for _ in range(2)]
            b_mrg = [S.buf() for _ in range(NT)]
            for i in range(NT):
                s = i % 2
                bi_att = 0 if i < 2 else 1 + (i - 2) // 4
                for k, (src, bsrc) in enumerate(((retT, b_retT[i]), (attT, b_att[bi_att]), (convT, b_convT[bi_att]))):
                    S.dma("sp", brt[s][k], src[:, :, i * 128:(i + 1) * 128], [bsrc], [b_in5[s]], b_in5[s])
                S.dma("sp", sgl[s], z_tok[i * 128:(i + 1) * 128, 4608:7680], [zb(i, "sg%d" % k) for k in range(6)],
                      [b_in5[s]], b_in5[s])
                for k in range(3):
                    for hf in range(2):
                        p = 2 * k + hf
                        for kc in range(8):
                            mm(psum[p][:, :], brt[s][k][:, kc, :], wbr[k][:, kc, hf * 512:(hf + 1) * 512], kc == 0, kc == 7,
                               [b_in5[s], b_wbr], [bps[p]])
                for hf in range(2):
                    c0 = hf * 512
                    tt("dve", m0[:, c0:c0 + 512], psum[hf][:, :], sgl[s][:, c0:c0 + 512], ALU.mult, [bps[hf], b_in5[s]], [b_m])
                    tt("dve", m1[:, c0:c0 + 512], psum[2 + hf][:, :], sgl[s][:, D + c0:D + c0 + 512], ALU.mult, [bps[2 + hf], b_in5[s]], [b_m])
                tt("pool", m0, m0, m1, ALU.add, [b_m], [b_m])
                for hf in range(2):
                    c0 = hf * 512
                    tt("dve", m1[:, c0:c0 + 512], psum[4 + hf][:, :], sgl[s][:, 2 * D + c0:2 * D + c0 + 512], ALU.mult, [bps[4 + hf], b_in5[s]], [b_m])
                tt("pool", mgb, m0, m1, ALU.add, [b_m], [b_mg])
                for kc in range(8):
                    tr(psb(6 + s)[:, kc * 128:(kc + 1) * 128], mgb[:, kc * 128:(kc + 1) * 128], identb, [b_mg, b_id], [bps[6 + s]])
                cp("act", mTt[s], psb(6 + s).rearrange("p (a b) -> p a b", a=8), [bps[6 + s]], [b_mTt[s]])
                S.dma("sp", mrgT[:, :, i * 128:(i + 1) * 128], mTt[s], [b_mTt[s]], [b_mrg[i]], b_mTt[s])
            S.barrier()
            if dbg and dbg.get("stop") == "P5a":
                break

            st["off"] = PERSIST
            h2T = carve([8, T], BF16)
            b_h2T = [S.buf() for _ in range(NT)]
            Gall = carve([NT, 65], F32)
            b_G = [S.buf() for _ in range(NT)]
            b_Gi = S.buf()
            mset("pool", Gall.rearrange("p a b -> p (a b)"), 1.0, [b_Gi])
            MOE_BASE = st["off"]
            wo = carve([8, D], BF16)
            b_wo = S.buf()
            S.dma("pool", wo, w_out[l].rearrange("(kc p) n -> p kc n", p=128), [], [b_wo], b_wo)
            wr = carve([8, NE], F32)
            S.dma("sp", wr, w_router[l].rearrange("(kc p) n -> p kc n", p=128), [], [b_wo], b_wo)
            rb = carve([NE], F32)
            S.dma("sp", rb, pbc(rbias[l:l + 1, :], 128), [], [b_wo], b_wo)
            g1 = [carve([D], F32) for _ in range(2)]
            sc2 = [carve([D], F32) for _ in range(2)]
            sh2 = [carve([D], F32) for _ in range(2)]
            lg_ = carve([D], F32)
            lb_ = carve([D], F32)
            b_m5 = S.buf()
            for w_ in range(2):
                mod_bc(g1[w_], l, w_, 2, b_m5)
                mod_bc(sc2[w_], l, w_, 4, b_m5, plus1=True)
                mod_bc(sh2[w_], l, w_, 3, b_m5)
            vec_bc(lg_, ln1_g[l:l + 1, :], D, b_m5)
            vec_bc(lb_, ln1_b[l:l + 1, :], D, b_m5)
            mT5 = [carve([8, 128], BF16) for _ in range(2)]
            x5 = [carve([D], F32) for _ in range(2)]
            b_l5 = [S.buf() for _ in range(2)]
            u5 = carve([D], F32)
            v5 = carve([D], F32)
            st5 = carve([8], F32)
            b_u = S.buf()
            x1t = [carve([D], F32) for _ in range(2)]
            b_x1 = [S.buf() for _ in range(2)]
            h2f = carve([D], F32)
            b_h2 = S.buf()
            h2loT = carve([8, 128], BF16)
            b_h2Tf = S.buf()
            h2hi = carve([D], BF16)
            h2lo = carve([D], BF16)
            b_h2s = S.buf()
            wrh = carve([8, NE], BF16)
            wrl = carve([8, NE], BF16)
            wrt = carve([8, NE], F32)
            cp("dve", wrh, wr, [b_wo], [b_wo])
            cp("dve", wrt, wrh, [b_wo], [b_wo])
            tt("dve", wrl, wr, wrt, ALU.subtract, [b_wo], [b_wo])
            rs = carve([NE], F32)
            rsel = carve([NE], F32)
            rt1 = carve([NE], F32)
            rg8 = carve([32], F32)
            b_r = S.buf()

            def layer_norm(eng2, dst, u, gam, bet, reads, wb):
                rsum("dve", st5[:, 0:1], u, reads, [b_u])
                tt(eng2, v5, u, u, ALU.mult, reads, [b_u])
                rsum("dve", st5[:, 1:2], v5, [b_u], [b_u])
                ts("dve", st5[:, 0:1], st5[:, 0:1], 1.0 / D, None, ALU.mult, None, [b_u], [b_u])
                tt("dve", st5[:, 2:3], st5[:, 0:1], st5[:, 0:1], ALU.mult, [b_u], [b_u])
                stt("dve", st5[:, 1:2], st5[:, 1:2], 1.0 / D, st5[:, 2:3], ALU.mult, ALU.subtract, [b_u], [b_u])
                rstd_from("dve", st5[:, 1:2], st5[:, 1:2], 1.0, [b_u], [b_u])
                ts("dve", v5, u, st5[:, 0:1], st5[:, 1:2], ALU.subtract, ALU.mult, reads + [b_u], [b_u])
                tt(eng2, v5, v5, gam, ALU.mult, [b_u, b_m5], [b_u])
                tt(eng2, dst, v5, bet, ALU.add, [b_u, b_m5], wb)

            for i in range(NT):
                s = i % 2
                w_ = 1 if i < 2 else 0
                S.dma("sp", mT5[s], mrgT[:, :, i * 128:(i + 1) * 128], [b_mrg[i]], [b_l5[s]], b_l5[s])
                S.dma("sp", x5[s], xin(i), [b_xcur[i]], [b_l5[s]], b_l5[s])
                for hf in range(2):
                    for kc in range(8):
                        mm(psum[hf][:, :], mT5[s][:, kc, :], wo[:, kc, hf * 512:(hf + 1) * 512], kc == 0, kc == 7,
                           [b_l5[s], b_wo], [bps[hf]])
                for hf in range(2):
                    c0 = hf * 512
                    tt("dve", u5[:, c0:c0 + 512], psum[hf][:, :], g1[w_][:, c0:c0 + 512], ALU.mult, [bps[hf], b_m5], [b_u])
                stt("dve", u5, x5[s], ALPHA, u5, ALU.mult, ALU.add, [b_l5[s], b_u], [b_u])
                layer_norm("pool", x1t[s], u5, lg_, lb_, [b_u], [b_x1[s]])
                S.dma("sp", xres[i * 128:(i + 1) * 128, :], x1t[s], [b_x1[s]], [b_xres[i]], b_x1[s])
                tt("pool", h2f, x1t[s], sc2[w_], ALU.mult, [b_x1[s], b_m5], [b_h2])
                tt("pool", h2f, h2f, sh2[w_], ALU.add, [b_h2, b_m5], [b_h2])
                cp("pool", h2hi, h2f, [b_h2], [b_h2s])
                tt("pool", h2lo, h2f, h2hi, ALU.subtract, [b_h2, b_h2s], [b_h2s])
                for kc in range(8):
                    tr(psb(2)[:, kc * 128:(kc + 1) * 128], h2hi[:, kc * 128:(kc + 1) * 128], identb, [b_h2s, b_id], [bps[2]])
                for kc in range(8):
                    tr(psb(3)[:, kc * 128:(kc + 1) * 128], h2lo[:, kc * 128:(kc + 1) * 128], identb, [b_h2s, b_id], [bps[3]])
                cp("act", h2T[:, :, i * 128:(i + 1) * 128], psb(2).rearrange("p (a b) -> p a b", a=8), [bps[2]], [b_h2T[i]])
                cp("dve", h2loT, psb(3).rearrange("p (a b) -> p a b", a=8), [bps[3]], [b_h2Tf])
                for kc in range(8):
                    hT_i = h2T[:, kc, i * 128:(i + 1) * 128]
                    mm(psum[4][:, 0:NE], hT_i, wrh[:, kc, :], kc == 0, False, [b_h2T[i], b_wo], [bps[4]])
                    mm(psum[4][:, 0:NE], h2loT[:, kc, :], wrh[:, kc, :], False, False, [b_h2Tf, b_wo], [bps[4]])
                    mm(psum[4][:, 0:NE], hT_i, wrl[:, kc, :], False, kc == 7, [b_h2T[i], b_wo], [bps[4]])
                act(rs, psum[4][:, 0:NE], AF.Sigmoid, [bps[4]], [b_r])
                R = [b_r]
                if dbg and dbg.get("noroute"):
                    continue
                tt("dve", rsel, rs, rb, ALU.add, R + [b_wo], R)
                sel3 = rsel.rearrange("p (g e) -> p g e", g=8)
                S.op("dve", lambda e, sel3=sel3: e.tensor_reduce(rg8[:, 0:8], sel3, AX.X, ALU.max), R, R)
                t13 = rt1.rearrange("p (g e) -> p g e", g=8)
                tt("dve", t13, sel3, fv(rg8[:, 0:8], [[1, 8], [0, 8]]), ALU.is_ge, R, R)
                stt("dve", rt1, rt1, -1.0e4, rsel, ALU.mult, ALU.add, R, R)
                S.op("dve", lambda e, t13=t13: e.tensor_reduce(rg8[:, 8:16], t13, AX.X, ALU.max), R, R)
                tt("dve", rg8[:, 0:8], rg8[:, 0:8], rg8[:, 8:16], ALU.add, R, R)
                S.op("dve", lambda e: e.max(rg8[:, 16:24], rg8[:, 0:8]), R, R)
                ts("dve", rg8[:, 24:32], rg8[:, 0:8], rg8[:, 19:20], None, ALU.is_ge, None, R, R)
                ts("dve", rg8[:, 8:16], rg8[:, 24:32], 1.0e4, -1.0e4, ALU.mult, ALU.add, R, R)
                tt("dve", t13, sel3, fv(rg8[:, 24:32], [[1, 8], [0, 8]]), ALU.mult, R, R)
                tt("dve", t13, t13, fv(rg8[:, 8:16], [[1, 8], [0, 8]]), ALU.add, R, R)
                S.op("dve", lambda e: e.max(rg8[:, 16:24], rt1), R, R)
                ts("dve", rt1, rt1, rg8[:, 23:24], None, ALU.is_ge, None, R, R)
                tt("dve", rt1, rt1, rs, ALU.mult, R, R)
                rsum("dve", rg8[:, 0:1], rt1, R, R)
                S.op("dve", lambda e: e.reciprocal(rg8[:, 1:2], rg8[:, 0:1]), R, R)
                ts("dve", Gall[:, i, 0:NE], rt1, rg8[:, 1:2], 2.5, ALU.mult, ALU.mult, R + [b_Gi], [b_G[i]])
            if dbg and dbg.get("stop") == "P5b":
                dump("Gall", Gall.rearrange("p a b -> p (a b)"), b_G)
                S.barrier()
                break
            S.barrier()

            st["off"] = MOE_BASE
            SBT = [(0, 12), (12, 12), (24, 10)]
            acc = carve([12, D], F32)
            b_acc = [S.buf() for _ in range(12)]
            wg = [carve([8, 256], BF16) for _ in range(2)]
            wu = [carve([8, 256], BF16) for _ in range(2)]
            wd = [carve([2, D], BF16) for _ in range(2)]
            b_we = [S.buf() for _ in range(2)]
            sil = [carve([512], BF16) for _ in range(2)]
            b_sil = [S.buf() for _ in range(2)]
            hid = [carve([2, 512], BF16) for _ in range(2)]
            b_hid = [S.buf() for _ in range(2)]
            g2 = [carve([D], F32) for _ in range(2)]
            l2g = carve([D], F32)
            l2b = carve([D], F32)
            b_m6 = S.buf()
            for w_ in range(2):
                mod_bc(g2[w_], l, w_, 5, b_m6)
            vec_bc(l2g, ln2_g[l:l + 1, :], D, b_m6)
            vec_bc(l2b, ln2_b[l:l + 1, :], D, b_m6)
            x6 = [carve([D], F32) for _ in range(2)]
            b_x6 = [S.buf() for _ in range(2)]
            v5 = carve([D], F32)
            st5 = carve([8], F32)
            b_u = S.buf()
            b_m5 = b_m6
            ite = 0
            itb = 0
            itd = 0
            for (tile0, ntile) in SBT:
                blocks = []
                k = 0
                while k < ntile:
                    nb = min(4, ntile - k)
                    blocks.append((tile0 + k, nb))
                    k += nb
                witems = [(e, bk) for e in range(NE + 1) for bk in range(len(blocks))]
                wslot = {}

                def load_w(e):
                    nonlocal ite
                    s_ = ite % 2
                    ite += 1
                    wslot[e] = s_
                    if e < NE:
                        srcs = (w_eg[l, e], w_eu[l, e], w_ed[l, e])
                    else:
                        srcs = (w_sg[l], w_su[l], w_sd[l])
                    S.dma("pool", wg[s_], srcs[0].rearrange("(kc p) f -> p kc f", p=128), [], [b_we[s_]], b_we[s_])
                    S.dma("pool", wu[s_], srcs[1].rearrange("(kc p) f -> p kc f", p=128), [], [b_we[s_]], b_we[s_])
                    S.dma("pool", wd[s_], srcs[2].rearrange("(fc p) n -> p fc n", p=128), [], [b_we[s_]], b_we[s_])

                hslot = {}

                def gate_up(wi):
                    nonlocal itb
                    e, bk = witems[wi]
                    if bk == 0:
                        load_w(e)
                    s_ = wslot[e]
                    ti0, ntl = blocks[bk]
                    n = ntl * 128
                    t0 = ti0 * 128
                    hs = itb % 2
                    itb += 1
                    hslot[wi] = hs
                    hreads = [b_h2T[ti0 + k] for k in range(ntl)] + [b_we[s_]]
                    for fc in range(2):
                        pg, pu = fc, 2 + fc
                        for kc in range(8):
                            mm(psum[pg][:, 0:n], wg[s_][:, kc, fc * 128:(fc + 1) * 128], h2T[:, kc, t0:t0 + n], kc == 0, kc == 7,
                               hreads, [bps[pg]])
                        for kc in range(8):
                            mm(psum[pu][:, 0:n], wu[s_][:, kc, fc * 128:(fc + 1) * 128], h2T[:, kc, t0:t0 + n], kc == 0, kc == 7,
                               hreads, [bps[pu]])
                        act(sil[fc][:, 0:n], psum[pg][:, 0:n], AF.Silu, [bps[pg]], [b_sil[fc]])
                        tt("dve", hid[hs][:, fc, 0:n], psum[pu][:, 0:n], sil[fc][:, 0:n], ALU.mult, [bps[pu], b_sil[fc]], [b_hid[hs]])

                def down(wi):
                    nonlocal itd
                    e, bk = witems[wi]
                    s_ = wslot[e]
                    hs = hslot[wi]
                    ti0, ntl = blocks[bk]
                    for k in range(ntl):
                        ti = ti0 + k
                        la = ti - tile0
                        pd = 4 + 2 * (itd % 2)
                        itd += 1
                        for hf in range(2):
                            for fc in range(2):
                                mm(psum[pd + hf][:, :], hid[hs][:, fc, k * 128:(k + 1) * 128], wd[s_][:, fc, hf * 512:(hf + 1) * 512],
                                   fc == 0, fc == 1, [b_hid[hs], b_we[s_]], [bps[pd + hf]])
                        for hf in range(2):
                            c0 = hf * 512
                            if e == 0:
                                ts("dve", acc[:, la, c0:c0 + 512], psum[pd + hf][:, :], Gall[:, ti, e:e + 1], None, ALU.mult, None,
                                   [bps[pd + hf], b_G[ti]], [b_acc[la]])
                            else:
                                stt("dve", acc[:, la, c0:c0 + 512], psum[pd + hf][:, :], Gall[:, ti, e:e + 1], acc[:, la, c0:c0 + 512],
                                    ALU.mult, ALU.add, [bps[pd + hf], b_G[ti], b_acc[la]], [b_acc[la]])

                gate_up(0)
                for wi in range(len(witems)):
                    if wi + 1 < len(witems):
                        gate_up(wi + 1)
                    down(wi)
                for la in range(ntile):
                    ti = tile0 + la
                    s = la % 2
                    w_ = 1 if ti < 2 else 0
                    if last and ti < 2:
                        continue
                    S.dma("sp", x6[s], xres[ti * 128:(ti + 1) * 128, :], [b_xres[ti]], [b_x6[s]], b_x6[s])
                    tt("pool", acc[:, la, :], acc[:, la, :], g2[w_], ALU.mult, [b_acc[la], b_m6], [b_acc[la]])
                    stt("dve", acc[:, la, :], x6[s], ALPHA, acc[:, la, :], ALU.mult, ALU.add, [b_x6[s], b_acc[la]], [b_acc[la]])
                    layer_norm("pool", x6[s], acc[:, la, :], l2g, l2b, [b_acc[la]], [b_x6[s]])
                    if last:
                        S.dma("sp", out[(ti - 2) * 128:(ti - 1) * 128, :], x6[s], [b_x6[s]], [b_out], b_x6[s])
                    else:
                        S.dma("sp", xcur[ti * 128:(ti + 1) * 128, :], x6[s], [b_x6[s]], [b_xcur[ti]], b_x6[s])
            S.barrier()
        S.barrier()
        S.emit()
        print("ops", {e: len(S.ops[e]) for e in ENGS}, "dsems", len(S.dsems))
    return nc


def _consts():
    ident = np.eye(128, dtype=np.float32)
    n_freq = 16
    inv = (10000.0 ** (-np.arange(n_freq, dtype=np.float32) / n_freq)).astype(np.float32)
    t = np.arange(4096)
    row = (t // 64).astype(np.float32)
    col = (t % 64).astype(np.float32)
    ang = np.concatenate([row[:, None] * inv, col[:, None] * inv], axis=-1).astype(np.float32)
    rope = np.zeros((T, 64), np.float32)
    rope[:256, 0:32] = 1.0
    rope[256:, 0:32] = np.cos(ang)
    rope[256:, 32:64] = np.sin(ang)
    j = np.arange(128, dtype=np.float32)[:, None]
    i = np.arange(128, dtype=np.float32)[None, :]
    relt = np.concatenate([(i - j) + 0 * j, (i >= j).astype(np.float32), (i <= j).astype(np.float32)], axis=1).astype(np.float32)
    posc = np.zeros((128, 258), np.float32)
    posc[:, 0:128] = i + 1.0
    posc[:, 128:256] = 128.0 - i
    posc[:, 256] = 127.0 - j[:, 0]
    posc[:, 257] = j[:, 0]
    return ident, rope, relt, posc


_NC_CACHE = {}


def make_in_maps(inputs, NLW=4):
    ident, rope, relt, posc = _consts()
    f0 = lambda a: np.ascontiguousarray(np.asarray(a, dtype=np.float32))
    f = lambda a: f0(np.asarray(a)[:NLW])
    shared = {
        "w_ada": f(inputs["w_ada"]), "b_ada": f(inputs["b_ada"]), "w_in": f(inputs["w_in"]),
        "ret_decay_logit": f(inputs["ret_decay_logit"]).reshape(NLW, 16),
        "att_q_norm": f(inputs["att_q_norm"]), "att_k_norm": f(inputs["att_k_norm"]),
        "conv_dw": f(inputs["conv_dw"]),
        "conv_vecs": np.ascontiguousarray(np.concatenate(
            [f(inputs["conv_db"]).reshape(NLW, 8, 128), f(inputs["conv_ln_g"]).reshape(NLW, 8, 128),
             f(inputs["conv_ln_b"]).reshape(NLW, 8, 128)], axis=1)),
        "w_ret_o": f(inputs["w_ret_o"]), "w_att_o": f(inputs["w_att_o"]), "w_conv_o": f(inputs["w_conv_o"]),
        "w_out": f(inputs["w_out"]), "ln1_g": f(inputs["ln1_g"]), "ln1_b": f(inputs["ln1_b"]),
        "w_router": f(inputs["w_router"]), "router_bias": f(inputs["router_bias"]),
        "w_exp_gate": f(inputs["w_exp_gate"]), "w_exp_up": f(inputs["w_exp_up"]), "w_exp_down": f(inputs["w_exp_down"]),
        "w_sh_gate": f(inputs["w_sh_gate"]), "w_sh_up": f(inputs["w_sh_up"]), "w_sh_down": f(inputs["w_sh_down"]),
        "ln2_g": f(inputs["ln2_g"]), "ln2_b": f(inputs["ln2_b"]),
        "c_ident": ident, "c_rope": rope, "c_relt": relt, "c_posc": posc,
    }
    x = f0(inputs["x"])
    ctx = f0(inputs["ctx"])
    c = f0(inputs["c"])
    cc = f0(inputs["c_ctx"])
    maps = []
    for b in range(8):
        cv = np.zeros((128, 8, 2), np.float32)
        cv[:, :, 0] = c[b].reshape(8, 128).T
        cv[:, :, 1] = cc.reshape(8, 128).T
        m = dict(shared)
        m["xb"] = x[b]
        m["ctxb"] = ctx[b]
        m["cvec"] = np.ascontiguousarray(cv.reshape(128, 16))
        maps.append(m)
    return maps


def kernel(**inputs):
    if "nc" not in _NC_CACHE:
        _NC_CACHE["nc"] = build(4)
    nc = _NC_CACHE["nc"]
    maps = make_in_maps(inputs)
    res = run_bass_kernel_spmd(nc, maps, core_ids=list(range(8)))
    return np.stack([np.asarray(r["out"], dtype=np.float32) for r in res.results], axis=0)
```

```python
import numpy as np
from contextlib import ExitStack
import concourse.bass as bass
import concourse.mybir as mybir
from concourse.bass_utils import run_bass_kernel_spmd

F32 = mybir.dt.float32
BF16 = mybir.dt.bfloat16
AF = mybir.ActivationFunctionType
ALU = mybir.AluOpType
AX = mybir.AxisListType

D = 1024
NT = 34
T = NT * 128
DIN = 9728
ZC = 7680
EPS = 1e-6
ALPHA = 8.0 ** 0.25
NE = 64
ENGS = ("pe", "act", "dve", "pool", "sp")


class Buf:
    __slots__ = ("name", "w", "r", "sem", "semcnt")

    def __init__(self, name=""):
        self.name = name
        self.w = {}
        self.r = {}
        self.sem = None
        self.semcnt = 0


class Op:
    __slots__ = ("eng", "fn", "deps", "idx", "sig", "dma", "sigval")

    def __init__(self, eng, fn, deps, idx, dma=None):
        self.eng = eng
        self.fn = fn
        self.deps = deps
        self.idx = idx
        self.sig = False
        self.dma = dma
        self.sigval = 0


class Sched:
    def __init__(self, nc, es):
        self.nc = nc
        self.es = es
        self.ops = {e: [] for e in ENGS}
        self.esem = {e: es.enter_context(nc.semaphore("es_" + e)) for e in ENGS}
        self.dsems = []
        self.dcnt = []
        self.free = []
        self.allbufs = []

    def buf(self, name=""):
        b = Buf(name)
        self.allbufs.append(b)
        return b

    def _collect(self, eng, reads, writes):
        deps = {}
        for b in reads:
            for k, v in b.w.items():
                if deps.get(k, -1) < v:
                    deps[k] = v
        for b in writes:
            for k, v in b.w.items():
                if deps.get(k, -1) < v:
                    deps[k] = v
            for k, v in b.r.items():
                if deps.get(k, -1) < v:
                    deps[k] = v
        out = []
        for k, v in deps.items():
            if k[0] == "e":
                if k[1] == eng and eng in ("pe", "sp"):
                    continue
                out.append(("e", k[1], v))
            else:
                out.append(("d", k[1], v))
        return out

    def _commit(self, key, val, reads, writes):
        for b in reads:
            if b.r.get(key, -1) < val:
                b.r[key] = val
        for b in writes:
            b.w = {key: val}
            b.r = {}

    def op(self, eng, fn, reads=(), writes=()):
        deps = self._collect(eng, reads, writes)
        idx = len(self.ops[eng])
        o = Op(eng, fn, deps, idx)
        self.ops[eng].append(o)
        self._commit(("e", eng), idx, reads, writes)
        return o

    def _getsem(self, buf):
        if buf.sem is None:
            if self.free:
                buf.sem = self.free.pop()
            else:
                buf.sem = len(self.dsems)
                self.dsems.append(self.es.enter_context(self.nc.semaphore("ds%d" % buf.sem)))
                self.dcnt.append(0)
        return buf.sem

    def dma(self, eng, out_ap, in_ap, reads, writes, sembuf):
        deps = self._collect(eng, reads, writes)
        si = self._getsem(sembuf)
        self.dcnt[si] += 16
        val = self.dcnt[si]
        idx = len(self.ops[eng])
        o = Op(eng, (out_ap, in_ap), deps, idx, dma=si)
        o.sigval = val
        self.ops[eng].append(o)
        self._commit(("d", si), val, reads, writes)
        return o

    def barrier(self):
        bb = Buf("bar")
        deps = {}
        for b in self.allbufs:
            for dct in (b.w, b.r):
                for k, v in dct.items():
                    if deps.get(k, -1) < v:
                        deps[k] = v
        dl = []
        for k, v in deps.items():
            if k[0] == "e":
                if k[1] != "sp":
                    dl.append(("e", k[1], v))
            else:
                dl.append(("d", k[1], v))
        idx = len(self.ops["sp"])
        o = Op("sp", lambda e: e.nop(), dl, idx)
        self.ops["sp"].append(o)
        bb.w = {("e", "sp"): idx}
        for e in ENGS:
            if e != "sp":
                self.op(e, None, [bb], [])
        for b in self.allbufs:
            b.w = {}
            b.r = {}
            b.sem = None
        self.free = list(range(len(self.dsems)))

    def emit(self):
        for e in ENGS:
            for o in self.ops[e]:
                for d in o.deps:
                    if d[0] == "e":
                        self.ops[d[1]][d[2]].sig = True
        for e in ENGS:
            c = 0
            for o in self.ops[e]:
                if o.dma is None and o.sig:
                    c += 1
                    o.sigval = c
        with self.nc.Block() as block:
            deco = {"pe": block.tensor, "act": block.scalar, "dve": block.vector,
                    "pool": block.gpsimd, "sp": block.sync}
            for e in ENGS:
                ops = self.ops[e]
                if not ops:
                    continue

                def body(engine, ops=ops, e=e):
                    waited = {}
                    for o in ops:
                        for d in o.deps:
                            if d[0] == "e":
                                key = ("e", d[1])
                                val = self.ops[d[1]][d[2]].sigval
                                sem = self.esem[d[1]]
                            else:
                                key = ("d", d[1])
                                val = d[2]
                                sem = self.dsems[d[1]]
                            if waited.get(key, 0) < val:
                                engine.wait_ge(sem, val)
                                waited[key] = val
                        if o.fn is None:
                            continue
                        if o.dma is not None:
                            if callable(o.fn):
                                o.fn(engine).then_inc(self.dsems[o.dma], 1)
                            else:
                                engine.dma_start(out=o.fn[0], in_=o.fn[1]).then_inc(self.dsems[o.dma], 16)
                        else:
                            ins = o.fn(engine)
                            if o.sig:
                                ins.then_inc(self.esem[e], 1)
                deco[e](body)


def fv(ap, pairs):
    return bass.AP(ap.tensor, ap.offset, [list(ap.ap[0])] + [list(p) for p in pairs])


def pbc(ap, nparts):
    return bass.AP(ap.tensor, ap.offset, [[0, nparts]] + [list(p) for p in ap.ap[1:]])


def build(L=4, dbg=None, NLW=4):
    nc = bass.Bass("TRN2", target_bir_lowering=False)
    dt_in = lambda name, shape: nc.dram_tensor(name, shape, F32, kind="ExternalInput").ap()
    xb_in = dt_in("xb", [4096, D])
    ctxb_in = dt_in("ctxb", [256, D])
    cvec_in = dt_in("cvec", [128, 16])
    w_ada = dt_in("w_ada", [NLW, D, 6 * D])
    b_ada = dt_in("b_ada", [NLW, 6 * D])
    w_in = dt_in("w_in", [NLW, D, DIN])
    dlog = dt_in("ret_decay_logit", [NLW, 16])
    qng = dt_in("att_q_norm", [NLW, 64])
    kng = dt_in("att_k_norm", [NLW, 64])
    conv_dw = dt_in("conv_dw", [NLW, 31, D])
    cvp = dt_in("conv_vecs", [NLW, 24, 128])
    w_ret_o = dt_in("w_ret_o", [NLW, D, D])
    w_att_o = dt_in("w_att_o", [NLW, D, D])
    w_conv_o = dt_in("w_conv_o", [NLW, D, D])
    w_out = dt_in("w_out", [NLW, D, D])
    ln1_g = dt_in("ln1_g", [NLW, D])
    ln1_b = dt_in("ln1_b", [NLW, D])
    w_router = dt_in("w_router", [NLW, D, NE])
    rbias = dt_in("router_bias", [NLW, NE])
    w_eg = dt_in("w_exp_gate", [NLW, NE, D, 256])
    w_eu = dt_in("w_exp_up", [NLW, NE, D, 256])
    w_ed = dt_in("w_exp_down", [NLW, NE, 256, D])
    w_sg = dt_in("w_sh_gate", [NLW, D, 256])
    w_su = dt_in("w_sh_up", [NLW, D, 256])
    w_sd = dt_in("w_sh_down", [NLW, 256, D])
    ln2_g = dt_in("ln2_g", [NLW, D])
    ln2_b = dt_in("ln2_b", [NLW, D])
    ident_in = dt_in("c_ident", [128, 128])
    rope_in = dt_in("c_rope", [T, 64])
    relt_in = dt_in("c_relt", [128, 384])
    posc_in = dt_in("c_posc", [128, 258])
    out = nc.dram_tensor("out", [4096, D], F32, kind="ExternalOutput").ap()

    def scratch(name, shape, dt):
        if dbg and name in dbg:
            return nc.dram_tensor(name, shape, dt, kind="ExternalOutput").ap()
        return nc.dram_tensor(name, shape, dt, kind="Internal").ap()
    modv = scratch("modv", [4, 2, 6 * D], F32)
    z_tok = scratch("z_tok", [T, ZC], BF16)
    gluT = scratch("gluT", [8, 128, T], BF16)
    retT = scratch("retT", [128, 8, T], BF16)
    attT = scratch("attT", [128, 8, T], BF16)
    convT = scratch("convT", [128, 8, T], BF16)
    mrgT = scratch("mrgT", [128, 8, T], BF16)
    xres = scratch("xres", [T, D], F32)
    xcur = scratch("xcur", [T, D], F32)

    with ExitStack() as es:
        S = Sched(nc, es)
        ARENA_W = 50000
        arena = es.enter_context(nc.sbuf_tensor("arena", [128, ARENA_W], F32))
        psum = [es.enter_context(nc.psum_tensor("ps%d" % i, [128, 512], F32)) for i in range(8)]
        bps = [S.buf("ps%d" % i) for i in range(8)]
        st = {"off": 0}

        def carve(shape, dt, nparts=128):
            n = int(np.prod(shape))
            words = (n * (4 if dt == F32 else 2) + 3) // 4
            words = (words + 7) // 8 * 8
            o = st["off"]
            assert o + words <= ARENA_W, ("arena overflow", o, words)
            st["off"] = o + words
            a = arena[0:nparts, o:o + words]
            if dt != F32:
                a = a.bitcast(dt)
            a = a[:, 0:n]
            if len(shape) == 2:
                a = a.rearrange("p (a b) -> p a b", a=shape[0])
            elif len(shape) == 3:
                a = a.rearrange("p (a b c) -> p a b c", a=shape[0], b=shape[1])
            return a

        def psb(i, n=1024):
            return psum[i].bitcast(BF16)[:, 0:n]

        def mm(o, lhsT, rhs, start, stop, reads, writes):
            S.op("pe", lambda e: e.matmul(o, lhsT, rhs, start=start, stop=stop), reads, writes)

        def tr(o, i, idn, reads, writes):
            S.op("pe", lambda e: e.transpose(o, i, idn), reads, writes)

        def act(o, i, func, reads, writes, bias=None, scale=None):
            kw = {}
            if bias is not None:
                kw["bias"] = bias
            if scale is not None:
                kw["scale"] = scale
            S.op("act", lambda e: e.activation(o, i, func, **kw), reads, writes)

        def tt(eng, o, a, b, op, reads, writes):
            S.op(eng, lambda e: e.tensor_tensor(o, a, b, op), reads, writes)

        def ts(eng, o, a, s1, s2, op0, op1, reads, writes):
            if s2 is None:
                S.op(eng, lambda e: e.tensor_scalar(o, a, s1, None, op0), reads, writes)
            else:
                S.op(eng, lambda e: e.tensor_scalar(o, a, s1, s2, op0, op1), reads, writes)

        def stt(eng, o, a, s, b, op0, op1, reads, writes):
            S.op(eng, lambda e: e.scalar_tensor_tensor(o, a, s, b, op0, op1), reads, writes)

        def cp(eng, o, i, reads, writes):
            if eng == "act":
                S.op("act", lambda e: e.copy(o, i), reads, writes)
            else:
                S.op(eng, lambda e: e.tensor_copy(o, i), reads, writes)

        def rsum(eng, o, i, reads, writes):
            S.op(eng, lambda e: e.reduce_sum(o, i, AX.X), reads, writes)

        def mset(eng, o, v, writes):
            S.op(eng, lambda e: e.memset(o, v), [], writes)

        def rstd_from(eng, o, i, scale, reads, writes):
            ts(eng, o, i, scale, EPS, ALU.mult, ALU.add, reads, writes)
            act(o, o, AF.Sqrt, writes, writes)
            S.op("dve", lambda e: e.reciprocal(o, o), writes, writes)

        def dump(name, ap, rbufs):
            if not (dbg and dbg.get("dump")):
                return
            shp = list(ap.shape)
            dtn = nc.dram_tensor("dmp_" + name, shp, ap.dtype, kind="ExternalOutput").ap()
            bb = S.buf()
            S.dma("sp", dtn, ap, list(rbufs), [bb], bb)

        identf = carve([128], F32)
        identb = carve([128], BF16)
        b_id = S.buf("ident")
        S.dma("sp", identf, ident_in, [], [b_id], b_id)
        cp("dve", identb, identf, [b_id], [b_id])
        onesf = carve([128], F32)
        mset("dve", onesf, 1.0, [b_id])
        onesm = carve([128], F32)
        mset("dve", onesm, 1.0 / 1024.0, [b_id])
        PERSIST = st["off"]

        def dram_bufs(n):
            return [S.buf() for _ in range(n)]
        b_modv = S.buf("modv")
        bz = {}

        def zb(i, name):
            k = (i, name)
            if k not in bz:
                bz[k] = S.buf()
            return bz[k]
        b_xcur = dram_bufs(NT)
        b_xres = dram_bufs(NT)
        b_out = S.buf("out")

        st["off"] = PERSIST
        cT = carve([16], F32)
        sT = carve([8, 2], F32)
        b_c = S.buf()
        S.dma("sp", cT, cvec_in, [], [b_c], b_c)
        act(sT.rearrange("p a b -> p (a b)"), cT, AF.Silu, [b_c], [b_c])
        wa = [carve([8, 512], F32) for _ in range(2)]
        b_wa = [S.buf() for _ in range(2)]
        bt = carve([6 * D], F32, nparts=2)
        modrow = carve([6 * D], F32, nparts=2)
        b_bt = S.buf()
        b_mr = S.buf()
        it = 0
        for l in range(L):
            S.dma("sp", bt, pbc(b_ada[l:l + 1, :], 2), [], [b_bt], b_bt)
            for n in range(12):
                s = it % 2
                it += 1
                S.dma("sp", wa[s], w_ada[l, :, n * 512:(n + 1) * 512].rearrange("(kc p) n -> p kc n", p=128),
                      [], [b_wa[s]], b_wa[s])
                for kc in range(8):
                    mm(psum[s][0:2, :], sT[:, kc, :], wa[s][:, kc, :], kc == 0, kc == 7, [b_c, b_wa[s]], [bps[s]])
                tt("dve", modrow[:, n * 512:(n + 1) * 512], psum[s][0:2, :], bt[:, n * 512:(n + 1) * 512], ALU.add,
                   [bps[s], b_bt], [b_mr])
            S.dma("sp", modv[l], modrow, [b_mr], [b_modv], b_mr)
        S.barrier()

        def mod_bc(dst, l, which, part, b_dst, plus1=False):
            src = modv[l, which:which + 1, part * D:(part + 1) * D]
            S.dma("sp", dst, pbc(src, 128), [b_modv], [b_dst], b_dst)
            if plus1:
                ts("pool", dst, dst, 1.0, None, ALU.add, None, [b_dst], [b_dst])

        def vec_bc(dst, src_row, n, b_dst):
            S.dma("sp", dst, pbc(src_row, 128), [], [b_dst], b_dst)

        def rope(eng, dst1, dst2, src, H, rp, tmp, reads, writes):
            x1 = src[:, :, 0:32]
            x2 = src[:, :, 32:64]
            cs = fv(rp[:, 0:32], [[0, H], [1, 32]])
            sn = fv(rp[:, 32:64], [[0, H], [1, 32]])
            ta, tb_ = tmp
            tt(eng, ta, x1, cs, ALU.mult, reads, writes)
            tt(eng, tb_, x2, sn, ALU.mult, reads, writes)
            tt(eng, dst1, ta, tb_, ALU.subtract, reads, writes)
            tt(eng, ta, x1, sn, ALU.mult, reads, writes)
            tt(eng, tb_, x2, cs, ALU.mult, reads, writes)
            tt(eng, dst2, ta, tb_, ALU.add, reads, writes)

        for l in range(L):
            last = (l == L - 1)

            def xin(i):
                if l == 0:
                    return ctxb_in[i * 128:(i + 1) * 128, :] if i < 2 else xb_in[(i - 2) * 128:(i - 1) * 128, :]
                return xcur[i * 128:(i + 1) * 128, :]

            st["off"] = PERSIST
            hT = carve([8, T], BF16)
            b_hT = [S.buf() for _ in range(NT)]
            sc1 = [carve([D], F32) for _ in range(2)]
            sh1 = [carve([D], F32) for _ in range(2)]
            b_m1 = S.buf()
            for w_ in range(2):
                mod_bc(sc1[w_], l, w_, 1, b_m1, plus1=True)
                mod_bc(sh1[w_], l, w_, 0, b_m1)
            xt = [carve([D], F32) for _ in range(2)]
            hb = [carve([D], BF16) for _ in range(2)]
            b_xt = [S.buf() for _ in range(2)]
            b_hb = [S.buf() for _ in range(2)]
            for i in range(NT):
                s = i % 2
                w_ = 1 if i < 2 else 0
                S.dma("sp", xt[s], xin(i), [b_xcur[i]], [b_xt[s]], b_xt[s])
                tt("dve", xt[s], xt[s], sc1[w_], ALU.mult, [b_xt[s], b_m1], [b_xt[s]])
                tt("pool", hb[s], xt[s], sh1[w_], ALU.add, [b_xt[s], b_m1], [b_hb[s]])
                for kc in range(8):
                    tr(psb(s)[:, kc * 128:(kc + 1) * 128], hb[s][:, kc * 128:(kc + 1) * 128], identb, [b_hb[s], b_id], [bps[s]])
                cp("act", hT[:, :, i * 128:(i + 1) * 128], psb(s).rearrange("p (a b) -> p a b", a=8), [bps[s]], [b_hT[i]])
            groups = []
            for k in range(2):
                groups.append((k * 512, k * 512, AF.Copy, ("rq", "rk")[k]))
            for k in range(2):
                groups.append((1024 + k * 512, 1024 + k * 512, AF.Copy, "rv%d" % k))
            for k in range(2):
                groups.append((2048 + k * 512, 2048 + k * 512, AF.Silu, "rg%d" % k))
            for k in range(2):
                groups.append((3072 + k * 512, 3072 + k * 512, AF.Copy, "aq%d" % k))
            groups.append((4096, 4096, AF.Copy, "kv"))
            for k in range(6):
                groups.append((6656 + k * 512, 4608 + k * 512, AF.Sigmoid, "sg%d" % k))
            wsl = [carve([8, 512], BF16) for _ in range(2)]
            b_wsl = [S.buf() for _ in range(2)]
            zt = [carve([512], BF16) for _ in range(3)]
            b_zt = [S.buf() for _ in range(3)]
            it = 0
            for gi, (wc0, zc0, func, nm) in enumerate(groups):
                s = gi % 2
                S.dma("pool", wsl[s], w_in[l, :, wc0:wc0 + 512].rearrange("(kc p) n -> p kc n", p=128),
                      [], [b_wsl[s]], b_wsl[s])
                for i in range(NT):
                    p = 2 + (it % 2)
                    q = it % 3
                    it += 1
                    for kc in range(8):
                        mm(psum[p][:, :], hT[:, kc, i * 128:(i + 1) * 128], wsl[s][:, kc, :], kc == 0, kc == 7,
                           [b_hT[i], b_wsl[s]], [bps[p]])
                    if func == AF.Copy:
                        cp("dve", zt[q], psum[p][:, :], [bps[p]], [b_zt[q]])
                    else:
                        act(zt[q], psum[p][:, :], func, [bps[p]], [b_zt[q]])
                    S.dma("sp", z_tok[i * 128:(i + 1) * 128, zc0:zc0 + 512], zt[q], [b_zt[q]], [zb(i, nm)], b_zt[q])
            wcs = [carve([8, 256], BF16) for _ in range(2)]
            b_wcs = [S.buf() for _ in range(2)]
            sgm = [carve([512], F32) for _ in range(2)]
            b_sgm = [S.buf() for _ in range(2)]
            glt = [carve([512], BF16) for _ in range(2)]
            b_glt = [S.buf() for _ in range(2)]
            b_glu = [S.buf() for _ in range(8)]
            it = 0
            for cc in range(8):
                s = cc % 2
                S.dma("pool", wcs[s][:, :, 0:128],
                      w_in[l, :, 4608 + cc * 128:4608 + (cc + 1) * 128].rearrange("(kc p) n -> p kc n", p=128),
                      [], [b_wcs[s]], b_wcs[s])
                S.dma("pool", wcs[s][:, :, 128:256],
                      w_in[l, :, 5632 + cc * 128:5632 + (cc + 1) * 128].rearrange("(kc p) n -> p kc n", p=128),
                      [], [b_wcs[s]], b_wcs[s])
                for tb in range(9):
                    t0 = tb * 512
                    n = min(512, T - t0)
                    tiles = range(t0 // 128, (t0 + n) // 128)
                    q = it % 2
                    it += 1
                    pa, pg = 4 + 2 * q, 5 + 2 * q
                    for kc in range(8):
                        mm(psum[pa][:, 0:n], wcs[s][:, kc, 0:128], hT[:, kc, t0:t0 + n], kc == 0, kc == 7,
                           [b_hT[i] for i in tiles] + [b_wcs[s]], [bps[pa]])
                    for kc in range(8):
                        mm(psum[pg][:, 0:n], wcs[s][:, kc, 128:256], hT[:, kc, t0:t0 + n], kc == 0, kc == 7,
                           [b_hT[i] for i in tiles] + [b_wcs[s]], [bps[pg]])
                    act(sgm[q][:, 0:n], psum[pg][:, 0:n], AF.Sigmoid, [bps[pg]], [b_sgm[q]])
                    tt("dve", glt[q][:, 0:n], psum[pa][:, 0:n], sgm[q][:, 0:n], ALU.mult, [bps[pa], b_sgm[q]], [b_glt[q]])
                    S.dma("sp", gluT[cc, :, t0:t0 + n], glt[q][:, 0:n], [b_glt[q]], [b_glu[cc]], b_glt[q])
            S.barrier()
            if dbg and dbg.get("stop") == "P1":
                break

            st["off"] = PERSIST
            kT2 = carve([4, T], BF16)
            b_kT = [S.buf() for _ in range(NT)]
            vext = carve([NT, 4, 192], BF16)
            b_v = [S.buf() for _ in range(NT)]
            b_vinit = S.buf()
            mset("pool", vext.rearrange("p a b c -> p (a b c)"), 0.0, [b_vinit])
            mset("pool", vext[:, :, :, 64:65], 1.0, [b_vinit])
            qg = carve([64], F32)
            kg = carve([64], F32)
            b_ng = S.buf()
            vec_bc(qg, qng[l:l + 1, :], 64, b_ng)
            vec_bc(kg, kng[l:l + 1, :], 64, b_ng)
            kvt = [carve([512], BF16) for _ in range(2)]
            b_kvt = [S.buf() for _ in range(2)]
            rp = [carve([64], F32) for _ in range(2)]
            b_rp = [S.buf() for _ in range(2)]
            sq = carve([D], F32)
            ss = carve([16], F32)
            qn = carve([16, 64], F32)
            tA = carve([16, 32], F32)
            tB = carve([16, 32], F32)
            kdup = carve([4, 2, 64], BF16)
            qb_ = carve([16, 64], BF16)
            b_tmp = S.buf()
            b_kd = S.buf()
            b_qb = S.buf()

            def normrope(src, H, gain, rpt, d1, d2, reads, wbuf):
                sv = sq[:, 0:H * 64].rearrange("p (h d) -> p h d", h=H)
                tt("dve", sv, src, src, ALU.mult, reads, [b_tmp])
                rsum("dve", ss[:, 0:H], sv, [b_tmp], [b_tmp])
                rstd_from("dve", ss[:, 0:H], ss[:, 0:H], 1.0 / 64.0, [b_tmp], [b_tmp])
                qv = qn[:, 0:H, :]
                tt("dve", qv, src, fv(ss[:, 0:H], [[1, H], [0, 64]]), ALU.mult, reads + [b_tmp], [b_tmp])
                tt("dve", qv, qv, fv(gain, [[0, H], [1, 64]]), ALU.mult, [b_tmp, b_ng], [b_tmp])
                rope("dve", d1, d2, qv, H, rpt, (tA[:, 0:H, :], tB[:, 0:H, :]), reads + [b_tmp], [b_tmp, wbuf])

            for i in range(NT):
                s = i % 2
                S.dma("sp", kvt[s], z_tok[i * 128:(i + 1) * 128, 4096:4608], [zb(i, "kv")], [b_kvt[s]], b_kvt[s])
                S.dma("sp", rp[s], rope_in[i * 128:(i + 1) * 128, :], [], [b_rp[s]], b_rp[s])
                ksrc = kvt[s][:, 0:256].rearrange("p (h d) -> p h d", h=4)
                normrope(ksrc, 4, kg, rp[s], kdup[:, :, 0, 0:32], kdup[:, :, 0, 32:64], [b_kvt[s], b_rp[s]], b_kd)
                cp("dve", kdup[:, :, 1, :], kdup[:, :, 0, :], [b_kd], [b_kd])
                for g in range(4):
                    tr(psb(7)[:, g * 128:(g + 1) * 128], kdup[:, g, :, :].rearrange("p a b -> p (a b)"), identb,
                       [b_kd, b_id], [bps[7]])
                cp("act", kT2[:, :, i * 128:(i + 1) * 128], psb(7, 512).rearrange("p (a b) -> p a b", a=4), [bps[7]], [b_kT[i]])
                vsrc = kvt[s][:, 256:512].rearrange("p (h d) -> p h d", h=4)
                cp("pool", vext[:, i, :, 0:64], vsrc, [b_kvt[s], b_vinit], [b_v[i]])
                cp("pool", vext[:, i, :, 128:192], vsrc, [b_kvt[s], b_vinit], [b_v[i]])
            aqt = [carve([D], BF16) for _ in range(2)]
            b_aqt = [S.buf() for _ in range(2)]
            qT = carve([8, 512], BF16)
            b_qT = S.buf()
            pT = [carve([512], BF16) for _ in range(3)]
            b_pT = [S.buf() for _ in range(3)]
            rden = carve([512], F32)
            b_rden = S.buf()
            osb = [carve([512], F32) for _ in range(2)]
            b_osb = [S.buf() for _ in range(2)]
            ablk = [carve([8, 512], BF16) for _ in range(2)]
            b_ablk = [S.buf() for _ in range(2)]
            b_att = [S.buf() for _ in range(9)]
            qblocks = [(0, 256, range(0, 2))] + [(256 + 512 * j, 512, range(NT)) for j in range(8)]
            its = 0
            ith = 0
            for bi, (t0, nq, kchunks) in enumerate(qblocks):
                ab = ablk[bi % 2]
                b_ab = b_ablk[bi % 2]
                for tq in range(nq // 128):
                    i = t0 // 128 + tq
                    s = tq % 2
                    S.dma("sp", aqt[s], z_tok[i * 128:(i + 1) * 128, 3072:4096],
                          [zb(i, "aq0"), zb(i, "aq1")], [b_aqt[s]], b_aqt[s])
                    S.dma("sp", rp[s], rope_in[i * 128:(i + 1) * 128, :], [], [b_rp[s]], b_rp[s])
                    qsrc = aqt[s].rearrange("p (h d) -> p h d", h=16)
                    normrope(qsrc, 16, qg, rp[s], qb_[:, :, 0:32], qb_[:, :, 32:64], [b_aqt[s], b_rp[s]], b_qb)
                    for hp in range(8):
                        tr(psb(7)[:, hp * 128:(hp + 1) * 128], qb_[:, 2 * hp:2 * hp + 2, :].rearrange("p a b -> p (a b)"),
                           identb, [b_qb, b_id], [bps[7]])
                    cp("dve", qT[:, :, tq * 128:(tq + 1) * 128], psb(7).rearrange("p (a b) -> p a b", a=8), [bps[7]], [b_qT])
                kl = list(kchunks)
                items = [(h, ci, c) for h in range(16) for ci, c in enumerate(kl)]
                LOOK = 2

                def issue_S(idx):
                    h, ci, c = items[idx]
                    g, hp, r = h // 4, h // 2, h % 2
                    pr = slice(r * 64, (r + 1) * 64)
                    p = (its0 + idx) % 3
                    mm(psum[p][:, 0:nq], kT2[pr, g, c * 128:(c + 1) * 128], qT[pr, hp, 0:nq], True, True,
                       [b_kT[c], b_qT], [bps[p]])

                def epilogue(h):
                    hp, r = h // 2, h % 2
                    pr = slice(r * 64, (r + 1) * 64)
                    po = 3 + (h % 2)
                    ob = h % 2
                    dr = 64 if r == 0 else 0
                    S.op("dve", lambda e, po=po, dr=dr, nq=nq: e.reciprocal(rden[dr:dr + 1, 0:nq], psum[po][dr:dr + 1, 0:nq]),
                         [bps[po]], [b_rden])
                    mm(psum[5][:, 0:nq], onesf[dr:dr + 1, :], rden[dr:dr + 1, 0:nq], True, True, [b_rden, b_id], [bps[5]])
                    cp("dve", osb[ob][pr, 0:nq], psum[po][pr, 0:nq], [bps[po]], [b_osb[ob]])
                    tt("dve", ab[pr, hp, 0:nq], osb[ob][pr, 0:nq], psum[5][pr, 0:nq], ALU.mult, [b_osb[ob], bps[5]], [b_ab])

                its0 = its
                for idx in range(min(LOOK, len(items))):
                    issue_S(idx)
                pending = None
                for idx, (h, ci, c) in enumerate(items):
                    g, r = h // 4, h % 2
                    if idx + LOOK < len(items):
                        issue_S(idx + LOOK)
                    p = (its0 + idx) % 3
                    po = 3 + (h % 2)
                    act(pT[p][:, 0:nq], psum[p][:, 0:nq], AF.Exp, [bps[p]], [b_pT[p]], scale=0.125)
                    if r == 0:
                        mm(psum[po][0:65, 0:nq], vext[:, c, g, 0:65], pT[p][:, 0:nq], ci == 0, ci == len(kl) - 1,
                           [b_v[c], b_pT[p]], [bps[po]])
                    else:
                        mm(psum[po][:, 0:nq], vext[:, c, g, 64:192], pT[p][:, 0:nq], ci == 0, ci == len(kl) - 1,
                           [b_v[c], b_pT[p]], [bps[po]])
                    if ci == 1 and pending is not None:
                        epilogue(pending)
                        pending = None
                    if ci == len(kl) - 1:
                        if pending is not None:
                            epilogue(pending)
                        pending = h
                if pending is not None:
                    epilogue(pending)
                its += len(items)
                S.dma("sp", attT[:, :, t0:t0 + nq], ab[:, :, 0:nq], [b_ab], [b_att[bi]], b_ab)
            dump("kT2", kT2, b_kT)
            dump("vext", vext.rearrange("p a b c -> p (a b c)"), b_v)
            dump("qT", qT, [b_qT])
            dump("osb0", osb[0], [b_osb[0]])
            dump("osb1", osb[1], [b_osb[1]])
            dump("rden", rden, [b_rden])
            dump("pT0", pT[0], [b_pT[0]])
            dump("qn", qn, [b_tmp])
            dump("ss", ss, [b_tmp])
            S.barrier()
            if dbg and dbg.get("stop") == "P2":
                break

            st["off"] = PERSIST
            lgb = carve([16], F32)
            b_lg = S.buf()
            S.dma("sp", lgb, pbc(dlog[l:l + 1, :], 128), [], [b_lg], b_lg)
            act(lgb, lgb, AF.Exp, [b_lg], [b_lg], scale=-1.0)
            ts("dve", lgb, lgb, 1.0, None, ALU.add, None, [b_lg], [b_lg])
            act(lgb, lgb, AF.Ln, [b_lg], [b_lg])
            ts("dve", lgb, lgb, -1.0, None, ALU.mult, None, [b_lg], [b_lg])
            relt = carve([384], F32)
            posc = carve([258], F32)
            b_cst = S.buf()
            S.dma("sp", relt, relt_in, [], [b_cst], b_cst)
            S.dma("sp", posc, posc_in, [], [b_cst], b_cst)
            Dtot = carve([8, 128], F32)
            Dtmp = carve([8, 128], F32)
            b_tab = S.buf()
            relv = fv(relt[:, 0:128], [[0, 8], [1, 128]])
            tt("dve", Dtot, relv, fv(lgb[:, 0:8], [[1, 8], [0, 128]]), ALU.mult, [b_cst, b_lg], [b_tab])
            act(Dtot, Dtot, AF.Exp, [b_tab], [b_tab])
            tt("dve", Dtot, Dtot, fv(relt[:, 128:256], [[0, 8], [1, 128]]), ALU.mult, [b_tab, b_cst], [b_tab])
            tt("dve", Dtmp, relv, fv(lgb[:, 8:16], [[1, 8], [0, 128]]), ALU.mult, [b_cst, b_lg], [b_tab])
            act(Dtmp, Dtmp, AF.Exp, [b_tab], [b_tab], scale=-1.0)
            tt("dve", Dtmp, Dtmp, fv(relt[:, 256:384], [[0, 8], [1, 128]]), ALU.mult, [b_tab, b_cst], [b_tab])
            tt("dve", Dtot, Dtot, Dtmp, ALU.add, [b_tab], [b_tab])
            ts("dve", Dtot, Dtot, 0.125, None, ALU.mult, None, [b_tab], [b_tab])
            qwf = carve([8, 128], F32)
            qwb = carve([8, 128], F32)
            tt("dve", qwf, fv(posc[:, 0:128], [[0, 8], [1, 128]]), fv(lgb[:, 0:8], [[1, 8], [0, 128]]), ALU.mult, [b_cst, b_lg], [b_tab])
            act(qwf, qwf, AF.Exp, [b_tab], [b_tab])
            tt("dve", qwb, fv(posc[:, 128:256], [[0, 8], [1, 128]]), fv(lgb[:, 8:16], [[1, 8], [0, 128]]), ALU.mult, [b_cst, b_lg], [b_tab])
            act(qwb, qwb, AF.Exp, [b_tab], [b_tab])
            kwf = carve([8], F32)
            kwb = carve([8], F32)
            ts("dve", kwf, lgb[:, 0:8], posc[:, 256:257], None, ALU.mult, None, [b_cst, b_lg], [b_tab])
            act(kwf, kwf, AF.Exp, [b_tab], [b_tab])
            ts("dve", kwf, kwf, 0.125, None, ALU.mult, None, [b_tab], [b_tab])
            ts("dve", kwb, lgb[:, 8:16], posc[:, 257:258], None, ALU.mult, None, [b_cst, b_lg], [b_tab])
            act(kwb, kwb, AF.Exp, [b_tab], [b_tab])
            ts("dve", kwb, kwb, 0.125, None, ALU.mult, None, [b_tab], [b_tab])
            Gf = carve([8, 128], F32)
            Gb = carve([8, 128], F32)
            ts("dve", Gf.rearrange("p a b -> p (a b)")[:, 0:8], lgb[:, 0:8], 128.0, None, ALU.mult, None, [b_lg], [b_tab])
            act(Gf.rearrange("p a b -> p (a b)")[:, 8:16], Gf.rearrange("p a b -> p (a b)")[:, 0:8], AF.Exp, [b_tab], [b_tab])
            ts("dve", Gb.rearrange("p a b -> p (a b)")[:, 0:8], lgb[:, 8:16], 128.0, None, ALU.mult, None, [b_lg], [b_tab])
            act(Gb.rearrange("p a b -> p (a b)")[:, 8:16], Gb.rearrange("p a b -> p (a b)")[:, 0:8], AF.Exp, [b_tab], [b_tab])
            gtmp = carve([16], F32)
            cp("dve", gtmp[:, 0:8], Gf.rearrange("p a b -> p (a b)")[:, 8:16], [b_tab], [b_tab])
            cp("dve", gtmp[:, 8:16], Gb.rearrange("p a b -> p (a b)")[:, 8:16], [b_tab], [b_tab])
            cp("dve", Gf, fv(gtmp[:, 0:8], [[1, 8], [0, 128]]), [b_tab], [b_tab])
            cp("dve", Gb, fv(gtmp[:, 8:16], [[1, 8], [0, 128]]), [b_tab], [b_tab])

            rp = [carve([64], F32) for _ in range(2)]
            b_rp = [S.buf() for _ in range(2)]
            Sprev = carve([NT, 8, 128], BF16)
            b_sp = [S.buf() for _ in range(NT)]
            Sst = carve([8, 128], F32)
            b_S = S.buf()
            rkt = [carve([512], BF16) for _ in range(2)]
            rvt = [carve([D], BF16) for _ in range(2)]
            rqt = [carve([512], BF16) for _ in range(2)]
            sgt = [carve([D], BF16) for _ in range(2)]
            b_ld = [S.buf() for _ in range(2)]
            kr = carve([8, 64], F32)
            kw = carve([8, 64], BF16)
            krb = carve([8, 64], BF16)
            qrb = carve([8, 64], BF16)
            tA = carve([8, 32], F32)
            tB = carve([8, 32], F32)
            b_k = S.buf()
            b_kw = S.buf()
            b_q = S.buf()
            mset("dve", Sst[0:64], 0.0, [b_S])
            order = [1, 0] + list(range(NT - 1, 1, -1))
            for oi, n in enumerate(order):
                s = oi % 2
                cp("act", Sprev[0:64, n], Sst[0:64], [b_S], [b_sp[n]])
                S.dma("sp", rkt[s], z_tok[n * 128:(n + 1) * 128, 512:1024], [zb(n, "rk")], [b_ld[s]], b_ld[s])
                S.dma("sp", rvt[s], z_tok[n * 128:(n + 1) * 128, 1024:2048], [zb(n, "rv0"), zb(n, "rv1")], [b_ld[s]], b_ld[s])
                S.dma("sp", rp[s], rope_in[n * 128:(n + 1) * 128, :], [], [b_rp[s]], b_rp[s])
                ksrc = rkt[s].rearrange("p (h d) -> p h d", h=8)
                rope("pool", kr[:, :, 0:32], kr[:, :, 32:64], ksrc, 8, rp[s], (tA, tB), [b_ld[s], b_rp[s]], [b_k])
                tt("pool", kw, kr, fv(kwb, [[1, 8], [0, 64]]), ALU.mult, [b_k, b_tab], [b_kw])
                for h in range(8):
                    pk = 0 + h // 4
                    mm(psum[pk][0:64, (h % 4) * 128:(h % 4 + 1) * 128], kw[:, h, :], rvt[s][:, h * 128:(h + 1) * 128], True, True,
                       [b_kw, b_ld[s]], [bps[pk]])
                tt("dve", Sst[0:64], Sst[0:64], Gb[0:64], ALU.mult, [b_S, b_tab], [b_S])
                for hh in range(2):
                    tt("dve", Sst[0:64, hh * 4:(hh + 1) * 4, :], Sst[0:64, hh * 4:(hh + 1) * 4, :],
                       psum[hh][0:64, :].rearrange("p (a b) -> p a b", a=4), ALU.add, [b_S, bps[hh]], [b_S])
            qTs = carve([8, 128], BF16)
            kTs = carve([8, 128], BF16)
            b_qk = S.buf()
            scT = carve([8, 128], BF16)
            b_sc = S.buf()
            qwfq = carve([8, 128], BF16)
            qwbq = carve([8, 128], BF16)
            b_qw = S.buf()
            Sfb = carve([8, 128], BF16)
            b_sfb = S.buf()
            ysb = carve([8, 128], F32)
            ysq = carve([8, 128], F32)
            mst = carve([32], F32)
            b_y = S.buf()
            rtn = carve([D], BF16)
            b_rtn = S.buf()
            rTt = [carve([8, 128], BF16) for _ in range(2)]
            b_rTt = [S.buf() for _ in range(2)]
            b_retT = [S.buf() for _ in range(NT)]
            mset("dve", Sst[0:64], 0.0, [b_S])
            kwA = [carve([8, 64], BF16) for _ in range(2)]
            b_kwA = [S.buf() for _ in range(2)]
            scTA = [carve([8, 128], BF16) for _ in range(2)]
            b_scA = [S.buf() for _ in range(2)]
            qwfA = [carve([8, 128], BF16) for _ in range(2)]
            qwbA = [carve([8, 128], BF16) for _ in range(2)]
            b_qwA = [S.buf() for _ in range(2)]
            b_krb = S.buf()

            def front(n):
                s = n % 2
                S.dma("sp", rkt[s], z_tok[n * 128:(n + 1) * 128, 512:1024], [zb(n, "rk")], [b_ld[s]], b_ld[s])
                S.dma("sp", rvt[s], z_tok[n * 128:(n + 1) * 128, 1024:2048], [zb(n, "rv0"), zb(n, "rv1")], [b_ld[s]], b_ld[s])
                S.dma("sp", rqt[s], z_tok[n * 128:(n + 1) * 128, 0:512], [zb(n, "rq")], [b_ld[s]], b_ld[s])
                S.dma("sp", sgt[s], z_tok[n * 128:(n + 1) * 128, 2048:3072], [zb(n, "rg0"), zb(n, "rg1")], [b_ld[s]], b_ld[s])
                S.dma("sp", rp[s], rope_in[n * 128:(n + 1) * 128, :], [], [b_rp[s]], b_rp[s])
                ksrc = rkt[s].rearrange("p (h d) -> p h d", h=8)
                qsrc = rqt[s].rearrange("p (h d) -> p h d", h=8)
                rope("pool", kr[:, :, 0:32], kr[:, :, 32:64], ksrc, 8, rp[s], (tA, tB), [b_ld[s], b_rp[s]], [b_k])
                tt("pool", kwA[s], kr, fv(kwf, [[1, 8], [0, 64]]), ALU.mult, [b_k, b_tab], [b_kwA[s]])
                cp("pool", krb, kr, [b_k], [b_krb])
                rope("pool", qrb[:, :, 0:32], qrb[:, :, 32:64], qsrc, 8, rp[s], (tA, tB), [b_ld[s], b_rp[s]], [b_q])
                for h in range(8):
                    tr(psb(6)[0:64, h * 128:(h + 1) * 128], qrb[:, h, :], identb, [b_q, b_id], [bps[6]])
                for h in range(8):
                    tr(psb(7)[0:64, h * 128:(h + 1) * 128], krb[:, h, :], identb, [b_krb, b_id], [bps[7]])
                cp("act", qTs[0:64], psb(6)[0:64].rearrange("p (a b) -> p a b", a=8), [bps[6]], [b_qk])
                cp("act", kTs[0:64], psb(7)[0:64].rearrange("p (a b) -> p a b", a=8), [bps[7]], [b_qk])
                for h in range(8):
                    pk = 0 + h // 4
                    mm(psum[pk][:, (h % 4) * 128:(h % 4 + 1) * 128], kTs[0:64, h, :], qTs[0:64, h, :], True, True,
                       [b_qk], [bps[pk]])
                for hh in range(2):
                    tt("dve", scTA[s][:, hh * 4:(hh + 1) * 4, :], psum[hh][:, :].rearrange("p (a b) -> p a b", a=4),
                       Dtot[:, hh * 4:(hh + 1) * 4, :], ALU.mult, [bps[hh], b_tab], [b_scA[s]])
                tt("pool", qwfA[s][0:64], qTs[0:64], qwf[0:64], ALU.mult, [b_qk, b_tab], [b_qwA[s]])
                tt("pool", qwbA[s][0:64], qTs[0:64], qwb[0:64], ALU.mult, [b_qk, b_tab], [b_qwA[s]])

            def back(n):
                s = n % 2
                cp("act", Sfb[0:64], Sst[0:64], [b_S], [b_sfb])
                for h in range(8):
                    pk = 2 + h // 4
                    o_ = psum[pk][:, (h % 4) * 128:(h % 4 + 1) * 128]
                    mm(o_, scTA[s][:, h, :], rvt[s][:, h * 128:(h + 1) * 128], True, False, [b_scA[s], b_ld[s]], [bps[pk]])
                    mm(o_, qwfA[s][0:64, h, :], Sfb[0:64, h, :], False, False, [b_qwA[s], b_sfb], [bps[pk]])
                    mm(o_, qwbA[s][0:64, h, :], Sprev[0:64, n, h, :], False, True, [b_qwA[s], b_sp[n]], [bps[pk]])
                for h in range(8):
                    pk = 4 + h // 4
                    mm(psum[pk][0:64, (h % 4) * 128:(h % 4 + 1) * 128], kwA[s][:, h, :], rvt[s][:, h * 128:(h + 1) * 128], True, True,
                       [b_kwA[s], b_ld[s]], [bps[pk]])
                tt("dve", Sst[0:64], Sst[0:64], Gf[0:64], ALU.mult, [b_S, b_tab, b_sfb], [b_S])
                for hh in range(2):
                    tt("dve", Sst[0:64, hh * 4:(hh + 1) * 4, :], Sst[0:64, hh * 4:(hh + 1) * 4, :],
                       psum[4 + hh][0:64, :].rearrange("p (a b) -> p a b", a=4), ALU.add, [b_S, bps[4 + hh]], [b_S])
                for hh in range(2):
                    cp("act", ysb[:, hh * 4:(hh + 1) * 4, :], psum[2 + hh][:, :].rearrange("p (a b) -> p a b", a=4), [bps[2 + hh]], [b_y])
                rsum("dve", mst[:, 0:8], ysb, [b_y], [b_y])
                tt("pool", ysq, ysb, ysb, ALU.mult, [b_y], [b_y])
                rsum("dve", mst[:, 8:16], ysq, [b_y], [b_y])
                ts("dve", mst[:, 0:8], mst[:, 0:8], 1.0 / 128.0, None, ALU.mult, None, [b_y], [b_y])
                tt("dve", mst[:, 16:24], mst[:, 0:8], mst[:, 0:8], ALU.mult, [b_y], [b_y])
                stt("dve", mst[:, 8:16], mst[:, 8:16], 1.0 / 128.0, mst[:, 16:24], ALU.mult, ALU.subtract, [b_y], [b_y])
                rstd_from("dve", mst[:, 8:16], mst[:, 8:16], 1.0, [b_y], [b_y])
                tt("dve", ysb, ysb, fv(mst[:, 0:8], [[1, 8], [0, 128]]), ALU.subtract, [b_y], [b_y])
                tt("pool", ysb, ysb, fv(mst[:, 8:16], [[1, 8], [0, 128]]), ALU.mult, [b_y], [b_y])
                tt("pool", rtn, ysb.rearrange("p a b -> p (a b)"), sgt[s], ALU.mult, [b_y, b_ld[s]], [b_rtn])
                for kc in range(8):
                    tr(psb(6)[:, kc * 128:(kc + 1) * 128], rtn[:, kc * 128:(kc + 1) * 128], identb, [b_rtn, b_id], [bps[6]])
                cp("act", rTt[s], psb(6).rearrange("p (a b) -> p a b", a=8), [bps[6]], [b_rTt[s]])
                S.dma("sp", retT[:, :, n * 128:(n + 1) * 128], rTt[s], [b_rTt[s]], [b_retT[n]], b_rTt[s])

            front(0)
            for n in range(NT):
                if n + 1 < NT:
                    front(n + 1)
                back(n)
            S.barrier()
            if dbg and dbg.get("stop") == "P3":
                break

            st["off"] = PERSIST
            GP = 286 + 30 + 4096
            gp = carve([8, GP], BF16)
            b_gp = S.buf()
            mset("pool", gp.rearrange("p a b -> p (a b)"), 0.0, [b_gp])
            for cc in range(8):
                S.dma("sp", gp[:, cc, 15:271], gluT[cc, :, 0:256], [b_glu[cc]], [b_gp], b_gp)
                S.dma("sp", gp[:, cc, 301:301 + 4096], gluT[cc, :, 256:T], [b_glu[cc]], [b_gp], b_gp)
            cw = carve([D], F32, nparts=31)
            b_cw = S.buf()
            S.dma("sp", cw, conv_dw[l], [], [b_cw], b_cw)
            wdT = carve([8, 32], F32)
            for cc in range(8):
                tr(psum[0][:, cc * 32:cc * 32 + 31], cw[0:31, cc * 128:(cc + 1) * 128], identf[0:31, 0:31], [b_cw, b_id], [bps[0]])
            cp("dve", wdT[:, :, 0:31], psum[0][:, 0:256].rearrange("p (a b) -> p a b", a=8)[:, :, 0:31], [bps[0]], [b_cw])
            cv = carve([128], F32, nparts=24)
            S.dma("sp", cv, cvp[l], [], [b_cw], b_cw)
            cvT = carve([24], F32)
            tr(psum[1][:, 0:24], cv[0:24, :], identf[0:24, 0:24], [b_cw, b_id], [bps[1]])
            cp("dve", cvT, psum[1][:, 0:24], [bps[1]], [b_cw])
            dgw = carve([8, 31, 128], BF16)
            b_dg = S.buf()
            for cc in range(8):
                tt("pool", dgw[:, cc], fv(identf, [[0, 31], [1, 128]]), fv(wdT[:, cc, 0:31], [[1, 31], [0, 128]]), ALU.mult,
                   [b_cw, b_id], [b_dg])
            yf = carve([8, 512], F32)
            ysq2 = carve([8, 512], F32)
            b_yf = S.buf()
            mean = carve([512], F32)
            rstd = carve([512], F32)
            b_st = S.buf()
            cblk = [carve([8, 512], BF16) for _ in range(2)]
            b_cblk = [S.buf() for _ in range(2)]
            b_convT = [S.buf() for _ in range(9)]
            tmpc = carve([512], F32)
            b_tc = S.buf()
            cblocks = [(0, 256, 0)] + [(256 + 512 * j, 512, 286 + 512 * j) for j in range(8)]
            it = 0
            for bi, (t0, n, g0) in enumerate(cblocks):
                cb = cblk[bi % 2]
                b_cb = b_cblk[bi % 2]
                for cc in range(8):
                    p = it % 2
                    it += 1
                    for k in range(31):
                        mm(psum[p][:, 0:n], dgw[:, cc, k, :], gp[:, cc, g0 + k:g0 + k + n], k == 0, k == 30,
                           [b_dg, b_gp], [bps[p]])
                    act(yf[:, cc, 0:n], psum[p][:, 0:n], AF.Identity, [bps[p], b_cw], [b_yf], bias=cvT[:, cc:cc + 1])
                    tt("pool", ysq2[:, cc, 0:n], yf[:, cc, 0:n], yf[:, cc, 0:n], ALU.mult, [b_yf], [b_yf])
                for cc in range(8):
                    mm(psum[2][:, 0:n], onesm, yf[:, cc, 0:n], cc == 0, cc == 7, [b_yf, b_id], [bps[2]])
                for cc in range(8):
                    mm(psum[3][:, 0:n], onesm, ysq2[:, cc, 0:n], cc == 0, cc == 7, [b_yf, b_id], [bps[3]])
                cp("act", mean[:, 0:n], psum[2][:, 0:n], [bps[2]], [b_st])
                tt("dve", rstd[:, 0:n], mean[:, 0:n], mean[:, 0:n], ALU.mult, [b_st], [b_st])
                tt("dve", rstd[:, 0:n], psum[3][:, 0:n], rstd[:, 0:n], ALU.subtract, [b_st, bps[3]], [b_st])
                rstd_from("dve", rstd[:, 0:n], rstd[:, 0:n], 1.0, [b_st], [b_st])
                for cc in range(8):
                    tt("dve", tmpc[:, 0:n], yf[:, cc, 0:n], mean[:, 0:n], ALU.subtract, [b_yf, b_st], [b_tc])
                    tt("pool", tmpc[:, 0:n], tmpc[:, 0:n], rstd[:, 0:n], ALU.mult, [b_tc, b_st], [b_tc])
                    act(cb[:, cc, 0:n], tmpc[:, 0:n], AF.Silu, [b_tc, b_cw], [b_cb],
                        bias=cvT[:, 16 + cc:17 + cc], scale=cvT[:, 8 + cc:9 + cc])
                S.dma("sp", convT[:, :, t0:t0 + n], cb[:, :, 0:n], [b_cb], [b_convT[bi]], b_cb)
            S.barrier()
            if dbg and dbg.get("stop") == "P4":
                break

            st["off"] = PERSIST
            wbr = [carve([8, D], BF16) for _ in range(3)]
            b_wbr = S.buf()
            for k, wsrc in enumerate((w_ret_o, w_att_o, w_conv_o)):
                S.dma("pool", wbr[k], wsrc[l].rearrange("(kc p) n -> p kc n", p=128), [], [b_wbr], b_wbr)
            brt = [[carve([8, 128], BF16) for _ in range(3)] for _ in range(2)]
            sgl = [carve([3 * D], BF16) for _ in range(2)]
            b_in5 = [S.buf() for _ in range(2)]
            m0 = carve([D], F32)
            m1 = carve([D], F32)
            mgb = carve([D], BF16)
            b_m = S.buf()
            b_mg = S.buf()
            mTt = [carve([8, 128], BF16) for _ in range(2)]
            b_mTt = [S.buf() for _ in range(2)]
            b_mrg = [S.buf() for _ in range(NT)]
            for i in range(NT):
                s = i % 2
                bi_att = 0 if i < 2 else 1 + (i - 2) // 4
                for k, (src, bsrc) in enumerate(((retT, b_retT[i]), (attT, b_att[bi_att]), (convT, b_convT[bi_att]))):
                    S.dma("sp", brt[s][k], src[:, :, i * 128:(i + 1) * 128], [bsrc], [b_in5[s]], b_in5[s])
                S.dma("sp", sgl[s], z_tok[i * 128:(i + 1) * 128, 4608:7680], [zb(i, "sg%d" % k) for k in range(6)],
                      [b_in5[s]], b_in5[s])
                for k in range(3):
                    for hf in range(2):
                        p = 2 * k + hf
                        for kc in range(8):
                            mm(psum[p][:, :], brt[s][k][:, kc, :], wbr[k][:, kc, hf * 512:(hf + 1) * 512], kc == 0, kc == 7,
                               [b_in5[s], b_wbr], [bps[p]])
                for hf in range(2):
                    c0 = hf * 512
                    tt("dve", m0[:, c0:c0 + 512], psum[hf][:, :], sgl[s][:, c0:c0 + 512], ALU.mult, [bps[hf], b_in5[s]], [b_m])
                    tt("dve", m1[:, c0:c0 + 512], psum[2 + hf][:, :], sgl[s][:, D + c0:D + c0 + 512], ALU.mult, [bps[2 + hf], b_in5[s]], [b_m])
                tt("pool", m0, m0, m1, ALU.add, [b_m], [b_m])
                for hf in range(2):
                    c0 = hf * 512
                    tt("dve", m1[:, c0:c0 + 512], psum[4 + hf][:, :], sgl[s][:, 2 * D + c0:2 * D + c0 + 512], ALU.mult, [bps[4 + hf], b_in5[s]], [b_m])
                tt("pool", mgb, m0, m1, ALU.add, [b_m], [b_mg])
                for kc in range(8):
                    tr(psb(6 + s)[:, kc * 128:(kc + 1) * 128], mgb[:, kc * 128:(kc + 1) * 128], identb, [b_mg, b_id], [bps[6 + s]])
                cp("act", mTt[s], psb(6 + s).rearrange("p (a b) -> p a b", a=8), [bps[6 + s]], [b_mTt[s]])
                S.dma("sp", mrgT[:, :, i * 128:(i + 1) * 128], mTt[s], [b_mTt[s]], [b_mrg[i]], b_mTt[s])
            S.barrier()
            if dbg and dbg.get("stop") == "P5a":
                break

            st["off"] = PERSIST
            h2T = carve([8, T], BF16)
            b_h2T = [S.buf() for _ in range(NT)]
            Gall = carve([NT, 65], F32)
            b_G = [S.buf() for _ in range(NT)]
            b_Gi = S.buf()
            mset("pool", Gall.rearrange("p a b -> p (a b)"), 1.0, [b_Gi])
            MOE_BASE = st["off"]
            wo = carve([8, D], BF16)
            b_wo = S.buf()
            S.dma("pool", wo, w_out[l].rearrange("(kc p) n -> p kc n", p=128), [], [b_wo], b_wo)
            wr = carve([8, NE], F32)
            S.dma("sp", wr, w_router[l].rearrange("(kc p) n -> p kc n", p=128), [], [b_wo], b_wo)
            rb = carve([NE], F32)
            S.dma("sp", rb, pbc(rbias[l:l + 1, :], 128), [], [b_wo], b_wo)
            g1 = [carve([D], F32) for _ in range(2)]
            sc2 = [carve([D], F32) for _ in range(2)]
            sh2 = [carve([D], F32) for _ in range(2)]
            lg_ = carve([D], F32)
            lb_ = carve([D], F32)
            b_m5 = S.buf()
            for w_ in range(2):
                mod_bc(g1[w_], l, w_, 2, b_m5)
                mod_bc(sc2[w_], l, w_, 4, b_m5, plus1=True)
                mod_bc(sh2[w_], l, w_, 3, b_m5)
            vec_bc(lg_, ln1_g[l:l + 1, :], D, b_m5)
            vec_bc(lb_, ln1_b[l:l + 1, :], D, b_m5)
            mT5 = [carve([8, 128], BF16) for _ in range(2)]
            x5 = [carve([D], F32) for _ in range(2)]
            b_l5 = [S.buf() for _ in range(2)]
            u5 = carve([D], F32)
            v5 = carve([D], F32)
            st5 = carve([8], F32)
            b_u = S.buf()
            x1t = [carve([D], F32) for _ in range(2)]
            b_x1 = [S.buf() for _ in range(2)]
            h2f = carve([D], F32)
            b_h2 = S.buf()
            h2loT = carve([8, 128], BF16)
            b_h2Tf = S.buf()
            h2hi = carve([D], BF16)
            h2lo = carve([D], BF16)
            b_h2s = S.buf()
            wrh = carve([8, NE], BF16)
            wrl = carve([8, NE], BF16)
            wrt = carve([8, NE], F32)
            cp("dve", wrh, wr, [b_wo], [b_wo])
            cp("dve", wrt, wrh, [b_wo], [b_wo])
            tt("dve", wrl, wr, wrt, ALU.subtract, [b_wo], [b_wo])
            rs = carve([NE], F32)
            rsel = carve([NE], F32)
            rt1 = carve([NE], F32)
            rg8 = carve([32], F32)
            b_r = S.buf()

            def layer_norm(eng2, dst, u, gam, bet, reads, wb):
                rsum("dve", st5[:, 0:1], u, reads, [b_u])
                tt(eng2, v5, u, u, ALU.mult, reads, [b_u])
                rsum("dve", st5[:, 1:2], v5, [b_u], [b_u])
                ts("dve", st5[:, 0:1], st5[:, 0:1], 1.0 / D, None, ALU.mult, None, [b_u], [b_u])
                tt("dve", st5[:, 2:3], st5[:, 0:1], st5[:, 0:1], ALU.mult, [b_u], [b_u])
                stt("dve", st5[:, 1:2], st5[:, 1:2], 1.0 / D, st5[:, 2:3], ALU.mult, ALU.subtract, [b_u], [b_u])
                rstd_from("dve", st5[:, 1:2], st5[:, 1:2], 1.0, [b_u], [b_u])
                ts("dve", v5, u, st5[:, 0:1], st5[:, 1:2], ALU.subtract, ALU.mult, reads + [b_u], [b_u])
                tt(eng2, v5, v5, gam, ALU.mult, [b_u, b_m5], [b_u])
                tt(eng2, dst, v5, bet, ALU.add, [b_u, b_m5], wb)

            for i in range(NT):
                s = i % 2
                w_ = 1 if i < 2 else 0
                S.dma("sp", mT5[s], mrgT[:, :, i * 128:(i + 1) * 128], [b_mrg[i]], [b_l5[s]], b_l5[s])
                S.dma("sp", x5[s], xin(i), [b_xcur[i]], [b_l5[s]], b_l5[s])
                for hf in range(2):
                    for kc in range(8):
                        mm(psum[hf][:, :], mT5[s][:, kc, :], wo[:, kc, hf * 512:(hf + 1) * 512], kc == 0, kc == 7,
                           [b_l5[s], b_wo], [bps[hf]])
                for hf in range(2):
                    c0 = hf * 512
                    tt("dve", u5[:, c0:c0 + 512], psum[hf][:, :], g1[w_][:, c0:c0 + 512], ALU.mult, [bps[hf], b_m5], [b_u])
                stt("dve", u5, x5[s], ALPHA, u5, ALU.mult, ALU.add, [b_l5[s], b_u], [b_u])
                layer_norm("pool", x1t[s], u5, lg_, lb_, [b_u], [b_x1[s]])
                S.dma("sp", xres[i * 128:(i + 1) * 128, :], x1t[s], [b_x1[s]], [b_xres[i]], b_x1[s])
                tt("pool", h2f, x1t[s], sc2[w_], ALU.mult, [b_x1[s], b_m5], [b_h2])
                tt("pool", h2f, h2f, sh2[w_], ALU.add, [b_h2, b_m5], [b_h2])
                cp("pool", h2hi, h2f, [b_h2], [b_h2s])
                tt("pool", h2lo, h2f, h2hi, ALU.subtract, [b_h2, b_h2s], [b_h2s])
                for kc in range(8):
                    tr(psb(2)[:, kc * 128:(kc + 1) * 128], h2hi[:, kc * 128:(kc + 1) * 128], identb, [b_h2s, b_id], [bps[2]])
                for kc in range(8):
                    tr(psb(3)[:, kc * 128:(kc + 1) * 128], h2lo[:, kc * 128:(kc + 1) * 128], identb, [b_h2s, b_id], [bps[3]])
                cp("act", h2T[:, :, i * 128:(i + 1) * 128], psb(2).rearrange("p (a b) -> p a b", a=8), [bps[2]], [b_h2T[i]])
                cp("dve", h2loT, psb(3).rearrange("p (a b) -> p a b", a=8), [bps[3]], [b_h2Tf])
                for kc in range(8):
                    hT_i = h2T[:, kc, i * 128:(i + 1) * 128]
                    mm(psum[4][:, 0:NE], hT_i, wrh[:, kc, :], kc == 0, False, [b_h2T[i], b_wo], [bps[4]])
                    mm(psum[4][:, 0:NE], h2loT[:, kc, :], wrh[:, kc, :], False, False, [b_h2Tf, b_wo], [bps[4]])
                    mm(psum[4][:, 0:NE], hT_i, wrl[:, kc, :], False, kc == 7, [b_h2T[i], b_wo], [bps[4]])
                act(rs, psum[4][:, 0:NE], AF.Sigmoid, [bps[4]], [b_r])
                R = [b_r]
                if dbg and dbg.get("noroute"):
                    continue
                tt("dve", rsel, rs, rb, ALU.add, R + [b_wo], R)
                sel3 = rsel.rearrange("p (g e) -> p g e", g=8)
                S.op("dve", lambda e, sel3=sel3: e.tensor_reduce(rg8[:, 0:8], sel3, AX.X, ALU.max), R, R)
                t13 = rt1.rearrange("p (g e) -> p g e", g=8)
                tt("dve", t13, sel3, fv(rg8[:, 0:8], [[1, 8], [0, 8]]), ALU.is_ge, R, R)
                stt("dve", rt1, rt1, -1.0e4, rsel, ALU.mult, ALU.add, R, R)
                S.op("dve", lambda e, t13=t13: e.tensor_reduce(rg8[:, 8:16], t13, AX.X, ALU.max), R, R)
                tt("dve", rg8[:, 0:8], rg8[:, 0:8], rg8[:, 8:16], ALU.add, R, R)
                S.op("dve", lambda e: e.max(rg8[:, 16:24], rg8[:, 0:8]), R, R)
                ts("dve", rg8[:, 24:32], rg8[:, 0:8], rg8[:, 19:20], None, ALU.is_ge, None, R, R)
                ts("dve", rg8[:, 8:16], rg8[:, 24:32], 1.0e4, -1.0e4, ALU.mult, ALU.add, R, R)
                tt("dve", t13, sel3, fv(rg8[:, 24:32], [[1, 8], [0, 8]]), ALU.mult, R, R)
                tt("dve", t13, t13, fv(rg8[:, 8:16], [[1, 8], [0, 8]]), ALU.add, R, R)
                S.op("dve", lambda e: e.max(rg8[:, 16:24], rt1), R, R)
                ts("dve", rt1, rt1, rg8[:, 23:24], None, ALU.is_ge, None, R, R)
                tt("dve", rt1, rt1, rs, ALU.mult, R, R)
                rsum("dve", rg8[:, 0:1], rt1, R, R)
                S.op("dve", lambda e: e.reciprocal(rg8[:, 1:2], rg8[:, 0:1]), R, R)
                ts("dve", Gall[:, i, 0:NE], rt1, rg8[:, 1:2], 2.5, ALU.mult, ALU.mult, R + [b_Gi], [b_G[i]])
            if dbg and dbg.get("stop") == "P5b":
                dump("Gall", Gall.rearrange("p a b -> p (a b)"), b_G)
                S.barrier()
                break
            S.barrier()

            st["off"] = MOE_BASE
            SBT = [(0, 12), (12, 12), (24, 10)]
            acc = carve([12, D], F32)
            b_acc = [S.buf() for _ in range(12)]
            wg = [carve([8, 256], BF16) for _ in range(2)]
            wu = [carve([8, 256], BF16) for _ in range(2)]
            wd = [carve([2, D], BF16) for _ in range(2)]
            b_we = [S.buf() for _ in range(2)]
            sil = [carve([512], BF16) for _ in range(2)]
            b_sil = [S.buf() for _ in range(2)]
            hid = [carve([2, 512], BF16) for _ in range(2)]
            b_hid = [S.buf() for _ in range(2)]
            g2 = [carve([D], F32) for _ in range(2)]
            l2g = carve([D], F32)
            l2b = carve([D], F32)
            b_m6 = S.buf()
            for w_ in range(2):
                mod_bc(g2[w_], l, w_, 5, b_m6)
            vec_bc(l2g, ln2_g[l:l + 1, :], D, b_m6)
            vec_bc(l2b, ln2_b[l:l + 1, :], D, b_m6)
            x6 = [carve([D], F32) for _ in range(2)]
            b_x6 = [S.buf() for _ in range(2)]
            v5 = carve([D], F32)
            st5 = carve([8], F32)
            b_u = S.buf()
            b_m5 = b_m6
            ite = 0
            itb = 0
            itd = 0
            for (tile0, ntile) in SBT:
                blocks = []
                k = 0
                while k < ntile:
                    nb = min(4, ntile - k)
                    blocks.append((tile0 + k, nb))
                    k += nb
                witems = [(e, bk) for e in range(NE + 1) for bk in range(len(blocks))]
                wslot = {}

                def load_w(e):
                    nonlocal ite
                    s_ = ite % 2
                    ite += 1
                    wslot[e] = s_
                    if e < NE:
                        srcs = (w_eg[l, e], w_eu[l, e], w_ed[l, e])
                    else:
                        srcs = (w_sg[l], w_su[l], w_sd[l])
                    S.dma("pool", wg[s_], srcs[0].rearrange("(kc p) f -> p kc f", p=128), [], [b_we[s_]], b_we[s_])
                    S.dma("pool", wu[s_], srcs[1].rearrange("(kc p) f -> p kc f", p=128), [], [b_we[s_]], b_we[s_])
                    S.dma("pool", wd[s_], srcs[2].rearrange("(fc p) n -> p fc n", p=128), [], [b_we[s_]], b_we[s_])

                hslot = {}

                def gate_up(wi):
                    nonlocal itb
                    e, bk = witems[wi]
                    if bk == 0:
                        load_w(e)
                    s_ = wslot[e]
                    ti0, ntl = blocks[bk]
                    n = ntl * 128
                    t0 = ti0 * 128
                    hs = itb % 2
                    itb += 1
                    hslot[wi] = hs
                    hreads = [b_h2T[ti0 + k] for k in range(ntl)] + [b_we[s_]]
                    for fc in range(2):
                        pg, pu = fc, 2 + fc
                        for kc in range(8):
                            mm(psum[pg][:, 0:n], wg[s_][:, kc, fc * 128:(fc + 1) * 128], h2T[:, kc, t0:t0 + n], kc == 0, kc == 7,
                               hreads, [bps[pg]])
                        for kc in range(8):
                            mm(psum[pu][:, 0:n], wu[s_][:, kc, fc * 128:(fc + 1) * 128], h2T[:, kc, t0:t0 + n], kc == 0, kc == 7,
                               hreads, [bps[pu]])
                        act(sil[fc][:, 0:n], psum[pg][:, 0:n], AF.Silu, [bps[pg]], [b_sil[fc]])
                        tt("dve", hid[hs][:, fc, 0:n], psum[pu][:, 0:n], sil[fc][:, 0:n], ALU.mult, [bps[pu], b_sil[fc]], [b_hid[hs]])

                def down(wi):
                    nonlocal itd
                    e, bk = witems[wi]
                    s_ = wslot[e]
                    hs = hslot[wi]
                    ti0, ntl = blocks[bk]
                    for k in range(ntl):
                        ti = ti0 + k
                        la = ti - tile0
                        pd = 4 + 2 * (itd % 2)
                        itd += 1
                        for hf in range(2):
                            for fc in range(2):
                                mm(psum[pd + hf][:, :], hid[hs][:, fc, k * 128:(k + 1) * 128], wd[s_][:, fc, hf * 512:(hf + 1) * 512],
                                   fc == 0, fc == 1, [b_hid[hs], b_we[s_]], [bps[pd + hf]])
                        for hf in range(2):
                            c0 = hf * 512
                            if e == 0:
                                ts("dve", acc[:, la, c0:c0 + 512], psum[pd + hf][:, :], Gall[:, ti, e:e + 1], None, ALU.mult, None,
                                   [bps[pd + hf], b_G[ti]], [b_acc[la]])
                            else:
                                stt("dve", acc[:, la, c0:c0 + 512], psum[pd + hf][:, :], Gall[:, ti, e:e + 1], acc[:, la, c0:c0 + 512],
                                    ALU.mult, ALU.add, [bps[pd + hf], b_G[ti], b_acc[la]], [b_acc[la]])

                gate_up(0)
                for wi in range(len(witems)):
                    if wi + 1 < len(witems):
                        gate_up(wi + 1)
                    down(wi)
                for la in range(ntile):
                    ti = tile0 + la
                    s = la % 2
                    w_ = 1 if ti < 2 else 0
                    if last and ti < 2:
                        continue
                    S.dma("sp", x6[s], xres[ti * 128:(ti + 1) * 128, :], [b_xres[ti]], [b_x6[s]], b_x6[s])
                    tt("pool", acc[:, la, :], acc[:, la, :], g2[w_], ALU.mult, [b_acc[la], b_m6], [b_acc[la]])
                    stt("dve", acc[:, la, :], x6[s], ALPHA, acc[:, la, :], ALU.mult, ALU.add, [b_x6[s], b_acc[la]], [b_acc[la]])
                    layer_norm("pool", x6[s], acc[:, la, :], l2g, l2b, [b_acc[la]], [b_x6[s]])
                    if last:
                        S.dma("sp", out[(ti - 2) * 128:(ti - 1) * 128, :], x6[s], [b_x6[s]], [b_out], b_x6[s])
                    else:
                        S.dma("sp", xcur[ti * 128:(ti + 1) * 128, :], x6[s], [b_x6[s]], [b_xcur[ti]], b_x6[s])
            S.barrier()
        S.barrier()
        S.emit()
        print("ops", {e: len(S.ops[e]) for e in ENGS}, "dsems", len(S.dsems))
    return nc


def _consts():
    ident = np.eye(128, dtype=np.float32)
    n_freq = 16
    inv = (10000.0 ** (-np.arange(n_freq, dtype=np.float32) / n_freq)).astype(np.float32)
    t = np.arange(4096)
    row = (t // 64).astype(np.float32)
    col = (t % 64).astype(np.float32)
    ang = np.concatenate([row[:, None] * inv, col[:, None] * inv], axis=-1).astype(np.float32)
    rope = np.zeros((T, 64), np.float32)
    rope[:256, 0:32] = 1.0
    rope[256:, 0:32] = np.cos(ang)
    rope[256:, 32:64] = np.sin(ang)
    j = np.arange(128, dtype=np.float32)[:, None]
    i = np.arange(128, dtype=np.float32)[None, :]
    relt = np.concatenate([(i - j) + 0 * j, (i >= j).astype(np.float32), (i <= j).astype(np.float32)], axis=1).astype(np.float32)
    posc = np.zeros((128, 258), np.float32)
    posc[:, 0:128] = i + 1.0
    posc[:, 128:256] = 128.0 - i
    posc[:, 256] = 127.0 - j[:, 0]
    posc[:, 257] = j[:, 0]
    return ident, rope, relt, posc


_NC_CACHE = {}


def make_in_maps(inputs, NLW=4):
    ident, rope, relt, posc = _consts()
    f0 = lambda a: np.ascontiguousarray(np.asarray(a, dtype=np.float32))
    f = lambda a: f0(np.asarray(a)[:NLW])
    shared = {
        "w_ada": f(inputs["w_ada"]), "b_ada": f(inputs["b_ada"]), "w_in": f(inputs["w_in"]),
        "ret_decay_logit": f(inputs["ret_decay_logit"]).reshape(NLW, 16),
        "att_q_norm": f(inputs["att_q_norm"]), "att_k_norm": f(inputs["att_k_norm"]),
        "conv_dw": f(inputs["conv_dw"]),
        "conv_vecs": np.ascontiguousarray(np.concatenate(
            [f(inputs["conv_db"]).reshape(NLW, 8, 128), f(inputs["conv_ln_g"]).reshape(NLW, 8, 128),
             f(inputs["conv_ln_b"]).reshape(NLW, 8, 128)], axis=1)),
        "w_ret_o": f(inputs["w_ret_o"]), "w_att_o": f(inputs["w_att_o"]), "w_conv_o": f(inputs["w_conv_o"]),
        "w_out": f(inputs["w_out"]), "ln1_g": f(inputs["ln1_g"]), "ln1_b": f(inputs["ln1_b"]),
        "w_router": f(inputs["w_router"]), "router_bias": f(inputs["router_bias"]),
        "w_exp_gate": f(inputs["w_exp_gate"]), "w_exp_up": f(inputs["w_exp_up"]), "w_exp_down": f(inputs["w_exp_down"]),
        "w_sh_gate": f(inputs["w_sh_gate"]), "w_sh_up": f(inputs["w_sh_up"]), "w_sh_down": f(inputs["w_sh_down"]),
        "ln2_g": f(inputs["ln2_g"]), "ln2_b": f(inputs["ln2_b"]),
        "c_ident": ident, "c_rope": rope, "c_relt": relt, "c_posc": posc,
    }
    x = f0(inputs["x"])
    ctx = f0(inputs["ctx"])
    c = f0(inputs["c"])
    cc = f0(inputs["c_ctx"])
    maps = []
    for b in range(8):
        cv = np.zeros((128, 8, 2), np.float32)
        cv[:, :, 0] = c[b].reshape(8, 128).T
        cv[:, :, 1] = cc.reshape(8, 128).T
        m = dict(shared)
        m["xb"] = x[b]
        m["ctxb"] = ctx[b]
        m["cvec"] = np.ascontiguousarray(cv.reshape(128, 16))
        maps.append(m)
    return maps


def kernel(**inputs):
    if "nc" not in _NC_CACHE:
        _NC_CACHE["nc"] = build(4)
    nc = _NC_CACHE["nc"]
    maps = make_in_maps(inputs)
    res = run_bass_kernel_spmd(nc, maps, core_ids=list(range(8)))
    return np.stack([np.asarray(r["out"], dtype=np.float32) for r in res.results], axis=0)
```
